# Optimizing a Trainium2 kernel written in Bass

```python
import math
import jax
import jax.numpy as jnp
from jax import lax
import numpy as np

D_MODEL = 1024
BATCH = 8
SEQ = 2048
DEPTH = 2

GRID_W = 64
CTX_LEN = 256

NORM_EPS = 1e-6
N_BRANCH = 3
N_MOD = 6

DA_HEADS = 4
DA_HEAD_DIM = 64
DA_V_DIM = 2 * DA_HEAD_DIM
DA_QK_COLS = DA_HEADS * 2 * DA_HEAD_DIM
DA_WIDTH = DA_HEADS * DA_V_DIM
DA_SUBLN_EPS = 1e-5
Q_BLOCK = 128
ROPE_BASE = 10000.0

SC_WIDTH = 512
SC_KSIZE = 3

RW_HEADS = 8
RW_HEAD_DIM = 64
RW_WIDTH = RW_HEADS * RW_HEAD_DIM
RW_DECAY_LORA = 64
RW_AAA_LORA = 64
RW_GATE_LORA = 160
RW_DECAY_SCALE = 0.606531
RW_GN_EPS = 64e-5
N_DIR = 2

N_EXPERTS = 32
TOP_K = 4
D_FF = 1024
SWIGLU_LIMIT = 7.0
SWIGLU_ALPHA = 1.702
MOE_BLOCK = 256

BRANCH_WIDTH = DA_WIDTH
IN_SPLITS = (DA_QK_COLS, DA_QK_COLS, DA_WIDTH, SC_WIDTH, SC_WIDTH, SC_WIDTH, RW_WIDTH, RW_WIDTH, RW_WIDTH, N_BRANCH * D_MODEL)
IN_COLS = sum(IN_SPLITS)

kernel_name = 'hybrid_diffattn_conv_rwkv7_moe_dit'


def rms_norm(x, g, eps=NORM_EPS):
    xf = x.astype(jnp.float32)
    y = xf * lax.rsqrt(jnp.mean(xf * xf, axis=-1, keepdims=True) + eps)
    return (y * g.astype(jnp.float32)).astype(x.dtype)


def modulate(x, g, shift, scale):
    return rms_norm(x, g) * (1 + scale) + shift


def split_cols(t):
    offsets = np.cumsum(IN_SPLITS)[:-1].tolist()
    return jnp.split(t, offsets, axis=-1)


def to_heads(t, n_heads):
    return t.reshape(t.shape[:-1] + (n_heads, t.shape[-1] // n_heads))


def rope_1d(t, pos):
    half = t.shape[-1] // 2
    inv = jnp.power(ROPE_BASE, -jnp.arange(half, dtype=jnp.float32) / half)
    ang = pos.astype(jnp.float32)[:, None] * inv[None, :]
    cos = jnp.cos(ang)[None, :, None, :]
    sin = jnp.sin(ang)[None, :, None, :]
    t1 = t[..., :half].astype(jnp.float32)
    t2 = t[..., half:].astype(jnp.float32)
    return jnp.concatenate([t1 * cos - t2 * sin, t1 * sin + t2 * cos], axis=-1).astype(t.dtype)


def rope_2d(t, rows, cols):
    d = t.shape[-1] // 2
    return jnp.concatenate([rope_1d(t[..., :d], rows), rope_1d(t[..., d:], cols)], axis=-1)


def qk_pair(t):
    t = t.reshape(t.shape[:-1] + (DA_HEADS, 2, DA_HEAD_DIM))
    return t[..., 0, :], t[..., 1, :]


def diff_attend(q1, q2, k1, k2, v, lam):
    scale = DA_HEAD_DIM ** -0.5
    s1 = jnp.einsum('bqhd,bkhd->bhqk', q1, k1).astype(jnp.float32) * scale
    s2 = jnp.einsum('bqhd,bkhd->bhqk', q2, k2).astype(jnp.float32) * scale
    p = jax.nn.softmax(s1, axis=-1) - lam * jax.nn.softmax(s2, axis=-1)
    return jnp.einsum('bhqk,bkhe->bqhe', p.astype(v.dtype), v)


def blocked_diff_attend(q1, q2, k1, k2, v, lam):
    b, s, h, d = q1.shape
    nb = s // Q_BLOCK

    def to_blocks(t):
        return t.reshape(b, nb, Q_BLOCK, h, d).swapaxes(0, 1)

    def one_block(qs):
        return diff_attend(qs[0], qs[1], k1, k2, v, lam)

    out = lax.map(one_block, (to_blocks(q1), to_blocks(q2)))
    return out.swapaxes(0, 1).reshape(b, s, h, v.shape[-1])


def diff_attention_branch(q, k, v, qc, kc, vc, rows, cols, lam, lam_init, subln_g, need_ctx_out):
    q1, q2 = qk_pair(q)
    k1, k2 = qk_pair(k)
    q1, q2, k1, k2 = [rope_2d(t, rows, cols) for t in (q1, q2, k1, k2)]
    k1c, k2c = qk_pair(kc)
    vh = to_heads(v, DA_HEADS)
    vhc = to_heads(vc, DA_HEADS)
    o = blocked_diff_attend(q1, q2, jnp.concatenate([k1, k1c], axis=1), jnp.concatenate([k2, k2c], axis=1),
                            jnp.concatenate([vh, vhc], axis=1), lam)
    out = (rms_norm(o, subln_g, DA_SUBLN_EPS) * (1 - lam_init)).reshape(o.shape[:2] + (DA_WIDTH,))
    out_c = None
    if need_ctx_out:
        q1c, q2c = qk_pair(qc)
        oc = diff_attend(q1c, q2c, k1c, k2c, vhc, lam)
        out_c = (rms_norm(oc, subln_g, DA_SUBLN_EPS) * (1 - lam_init)).reshape(oc.shape[:2] + (DA_WIDTH,))
    return out, out_c


def depthwise_conv3(u, w):
    return lax.conv_general_dilated(u, w[:, None, :].astype(u.dtype), window_strides=(1,), padding=((1, 1),),
                                    dimension_numbers=('NWC', 'WIO', 'NWC'), feature_group_count=u.shape[-1])


def short_conv_branch(b_gate, c_gate, xin, w):
    return b_gate * depthwise_conv3(c_gate * xin, w)


def l2_normalize(t):
    tf = t.astype(jnp.float32)
    return (tf * lax.rsqrt(jnp.maximum(jnp.sum(tf * tf, axis=-1, keepdims=True), 1e-24))).astype(t.dtype)


def rwkv_direction_terms(h, k, p, d):
    decay = jnp.exp(-RW_DECAY_SCALE * jax.nn.sigmoid(p['rw_w0'][d] + jnp.tanh(h @ p['rw_w1'][d]) @ p['rw_w2'][d]))
    a = jax.nn.sigmoid(p['rw_a0'][d] + (h @ p['rw_a1'][d]) @ p['rw_a2'][d])
    k_eff = k * (1 + (a - 1) * p['rw_k_a'])
    return to_heads(decay, RW_HEADS), to_heads(a, RW_HEADS), to_heads(k_eff, RW_HEADS)


def rwkv_scan(state0, r, decay, k, v, kk, a, reverse, emit):
    xs = tuple(t.swapaxes(0, 1) for t in (r, decay, k, v, kk, a))

    def step(s, inp):
        r_t, w_t, k_t, v_t, kk_t, a_t = inp
        s = (s * w_t[:, :, None, :]
             - jnp.einsum('bhij,bhj->bhi', s, kk_t)[..., None] * (kk_t * a_t)[:, :, None, :]
             + v_t[..., None] * k_t[:, :, None, :])
        y = jnp.einsum('bhij,bhj->bhi', s, r_t) if emit else None
        return s, y

    s_final, ys = lax.scan(step, state0, xs, reverse=reverse)
    return s_final, (ys.swapaxes(0, 1) if emit else None)


def bonus_term(r, k, v, r_k):
    return jnp.sum(r * k * r_k, axis=-1, keepdims=True) * v


def rwkv_output(y, bonus, g, p):
    yf = y.astype(jnp.float32)
    mu = jnp.mean(yf, axis=-1, keepdims=True)
    var = jnp.mean(jnp.square(yf - mu), axis=-1, keepdims=True)
    yn = ((yf - mu) * lax.rsqrt(var + RW_GN_EPS)).reshape(y.shape[:2] + (RW_WIDTH,))
    yn = (yn * p['rw_lnx_g'].astype(jnp.float32) + p['rw_lnx_b'].astype(jnp.float32)).astype(y.dtype)
    return (yn + bonus.reshape(y.shape[:2] + (RW_WIDTH,))) * g


def rwkv_branch(h, r, k, v, hc, rc, kc, vc, p, need_ctx_out):
    b = h.shape[0]
    state0 = jnp.zeros((b, RW_HEADS, RW_HEAD_DIM, RW_HEAD_DIM), h.dtype)
    rh, vh = to_heads(r, RW_HEADS), to_heads(v, RW_HEADS)
    rch, vch = to_heads(rc, RW_HEADS), to_heads(vc, RW_HEADS)
    kk = l2_normalize(to_heads(k * p['rw_k_k'], RW_HEADS))
    kkc = l2_normalize(to_heads(kc * p['rw_k_k'], RW_HEADS))
    ys, bon, ycs, bonc = [], [], [], []
    for d in range(N_DIR):
        rev = d == 1
        dc, ac, kec = rwkv_direction_terms(hc, kc, p, d)
        s_ctx, yc_d = rwkv_scan(state0, rch, dc, kec, vch, kkc, ac, rev, need_ctx_out)
        dl, al, kel = rwkv_direction_terms(h, k, p, d)
        _, y_d = rwkv_scan(s_ctx, rh, dl, kel, vh, kk, al, rev, True)
        ys.append(y_d)
        bon.append(bonus_term(rh, kel, vh, p['rw_r_k']))
        if need_ctx_out:
            ycs.append(yc_d)
            bonc.append(bonus_term(rch, kec, vch, p['rw_r_k']))
    g = jax.nn.sigmoid(h @ p['rw_g1']) @ p['rw_g2']
    out = rwkv_output(ys[0] + ys[1], bon[0] + bon[1], g, p)
    out_c = None
    if need_ctx_out:
        gc = jax.nn.sigmoid(hc @ p['rw_g1']) @ p['rw_g2']
        out_c = rwkv_output(ycs[0] + ycs[1], bonc[0] + bonc[1], gc, p)
    return out, out_c


def merge_branches(branches, gate_logits, w_branch, w_out):
    stacked = jnp.stack(branches, axis=-2)
    proj = jnp.einsum('btne,ned->btnd', stacked, w_branch)
    gates = jax.nn.sigmoid(gate_logits.reshape(gate_logits.shape[:-1] + (N_BRANCH, D_MODEL)))
    return jnp.sum(gates * proj, axis=-2) @ w_out


def token_mixer(h, hc, rows, cols, lam, lam_init, p, need_ctx_out):
    q, k, v, sb, sg, sx, rr, rk, rv, gl = split_cols(h @ p['w_in'])
    qc, kc, vc, sbc, sgc, sxc, rrc, rkc, rvc, glc = split_cols(hc @ p['w_in'])
    o_da, o_da_c = diff_attention_branch(q, k, v, qc, kc, vc, rows, cols, lam, lam_init, p['da_subln_g'], need_ctx_out)
    o_rw, o_rw_c = rwkv_branch(h, rr, rk, rv, hc, rrc, rkc, rvc, p, need_ctx_out)
    o_sc = short_conv_branch(sb, sg, sx, p['conv_w'])
    y = merge_branches((o_da, o_sc, o_rw), gl, p['w_branch'], p['w_out'])
    yc = None
    if need_ctx_out:
        o_sc_c = short_conv_branch(sbc, sgc, sxc, p['conv_w'])
        yc = merge_branches((o_da_c, o_sc_c, o_rw_c), glc, p['w_branch'], p['w_out'])
    return y, yc


def moe_ffn(tokens, router_w, router_b, w1, b1, w2, b2):
    n_tok, d_model = tokens.shape
    logits = (tokens @ router_w + router_b).astype(jnp.float32)
    top_val, top_idx = lax.top_k(logits, TOP_K)
    gates = jax.nn.softmax(top_val, axis=-1)
    n_assign = n_tok * TOP_K
    expert_of = top_idx.reshape(-1).astype(jnp.int32)
    token_of = jnp.arange(n_assign, dtype=jnp.int32) // TOP_K
    order = jnp.argsort(expert_of)
    e_sorted = expert_of[order]
    counts = jnp.bincount(expert_of, length=N_EXPERTS)
    padded = (counts + MOE_BLOCK - 1) // MOE_BLOCK * MOE_BLOCK
    start = jnp.cumsum(counts) - counts
    end_padded = jnp.cumsum(padded)
    start_padded = end_padded - padded
    dest = start_padded[e_sorted] + jnp.arange(n_assign, dtype=jnp.int32) - start[e_sorted]
    n_rows = -(-n_assign // MOE_BLOCK) * MOE_BLOCK + N_EXPERTS * MOE_BLOCK
    n_blocks = n_rows // MOE_BLOCK
    row_token = jnp.full((n_rows,), n_tok, jnp.int32).at[dest].set(token_of[order])
    row_gate = jnp.zeros((n_rows,), jnp.float32).at[dest].set(gates.reshape(-1)[order])
    block_expert = jnp.minimum(jnp.searchsorted(end_padded, jnp.arange(n_blocks) * MOE_BLOCK, side='right'), N_EXPERTS - 1)
    tokens_pad = jnp.concatenate([tokens, jnp.zeros((1, d_model), tokens.dtype)], axis=0)
    xb = tokens_pad[row_token].reshape(n_blocks, MOE_BLOCK, d_model)

    def expert_block(args):
        xblk, e = args
        hid = xblk @ w1[e] + b1[e]
        glu = jnp.minimum(hid[:, :D_FF], SWIGLU_LIMIT)
        lin = jnp.clip(hid[:, D_FF:], -SWIGLU_LIMIT, SWIGLU_LIMIT)
        return (glu * jax.nn.sigmoid(SWIGLU_ALPHA * glu) * (lin + 1)) @ w2[e] + b2[e]

    yb = lax.map(expert_block, (xb, block_expert)).reshape(n_rows, d_model)
    yb = yb * row_gate[:, None].astype(yb.dtype)
    return jax.ops.segment_sum(yb, row_token, num_segments=n_tok + 1)[:n_tok]


def layer(x, xc, silu_c, silu_cc, rows, cols, li, need_ctx_out, p):
    b, s, _ = x.shape
    mod = (silu_c @ p['w_mod'] + p['b_mod']).reshape(b, 1, N_MOD, D_MODEL)
    modc = (silu_cc @ p['w_mod'] + p['b_mod']).reshape(N_MOD, D_MODEL)
    sh_a, sc_a, g_a, sh_f, sc_f, g_f = [mod[:, :, i] for i in range(N_MOD)]
    sh_ac, sc_ac, g_ac, sh_fc, sc_fc, g_fc = [modc[i] for i in range(N_MOD)]
    lam_init = 0.8 - 0.6 * math.exp(-0.3 * li)
    lv = p['da_lambda'].astype(jnp.float32)
    lam = jnp.exp(jnp.sum(lv[0] * lv[1])) - jnp.exp(jnp.sum(lv[2] * lv[3])) + lam_init
    h = modulate(x, p['norm1_g'], sh_a, sc_a)
    hc = modulate(xc, p['norm1_g'], sh_ac, sc_ac)
    y, yc = token_mixer(h, hc, rows, cols, lam, lam_init, p, need_ctx_out)
    x = x + g_a * y
    h2 = modulate(x, p['norm2_g'], sh_f, sc_f)
    moe_w = (p['router_w'], p['router_b'], p['exp_w1'], p['exp_b1'], p['exp_w2'], p['exp_b2'])
    if need_ctx_out:
        xc = xc + g_ac * yc
        h2c = modulate(xc, p['norm2_g'], sh_fc, sc_fc)
        f = moe_ffn(jnp.concatenate([h2.reshape(-1, D_MODEL), h2c.reshape(-1, D_MODEL)], axis=0), *moe_w)
        x = x + g_f * f[:b * s].reshape(b, s, D_MODEL)
        xc = xc + g_fc * f[b * s:].reshape(xc.shape)
    else:
        x = x + g_f * moe_ffn(h2.reshape(-1, D_MODEL), *moe_w).reshape(b, s, D_MODEL)
    return x, xc


def setup_inputs(seed: int = 0) -> dict:
    key = jax.random.key(seed)
    keys = jax.random.split(key, 40)
    D = D_MODEL

    def nrm(i, shape, scale):
        return scale * jax.random.normal(keys[i], shape, jnp.float32)

    return {
        'x': nrm(0, (BATCH, SEQ, D), 1.0),
        'c': nrm(1, (BATCH, D), 1.0),
        'ctx': nrm(2, (BATCH, CTX_LEN, D), 1.0),
        'c_ctx': nrm(3, (D,), 1.0),
        'w_mod': nrm(4, (DEPTH, D, N_MOD * D), 0.5 * D ** -0.5),
        'b_mod': nrm(5, (DEPTH, N_MOD * D), 0.02),
        'norm1_g': 1.0 + nrm(6, (DEPTH, D), 0.02),
        'norm2_g': 1.0 + nrm(7, (DEPTH, D), 0.02),
        'w_in': nrm(8, (DEPTH, D, IN_COLS), D ** -0.5),
        'da_lambda': nrm(9, (DEPTH, 4, DA_HEAD_DIM), 0.1),
        'da_subln_g': 1.0 + nrm(10, (DEPTH, DA_V_DIM), 0.02),
        'conv_w': nrm(11, (DEPTH, SC_KSIZE, SC_WIDTH), SC_KSIZE ** -0.5),
        'rw_w0': nrm(12, (DEPTH, N_DIR, RW_WIDTH), 0.5),
        'rw_w1': nrm(13, (DEPTH, N_DIR, D, RW_DECAY_LORA), D ** -0.5),
        'rw_w2': nrm(14, (DEPTH, N_DIR, RW_DECAY_LORA, RW_WIDTH), 0.5 * RW_DECAY_LORA ** -0.5),
        'rw_a0': nrm(15, (DEPTH, N_DIR, RW_WIDTH), 0.5),
        'rw_a1': nrm(16, (DEPTH, N_DIR, D, RW_AAA_LORA), D ** -0.5),
        'rw_a2': nrm(17, (DEPTH, N_DIR, RW_AAA_LORA, RW_WIDTH), 0.5 * RW_AAA_LORA ** -0.5),
        'rw_g1': nrm(18, (DEPTH, D, RW_GATE_LORA), D ** -0.5),
        'rw_g2': nrm(19, (DEPTH, RW_GATE_LORA, RW_WIDTH), RW_GATE_LORA ** -0.5),
        'rw_k_k': 0.85 + nrm(20, (DEPTH, RW_WIDTH), 0.05),
        'rw_k_a': 1.0 + nrm(21, (DEPTH, RW_WIDTH), 0.05),
        'rw_r_k': nrm(22, (DEPTH, RW_HEADS, RW_HEAD_DIM), 0.1),
        'rw_lnx_g': 1.0 + nrm(23, (DEPTH, RW_WIDTH), 0.02),
        'rw_lnx_b': nrm(24, (DEPTH, RW_WIDTH), 0.02),
        'w_branch': nrm(25, (DEPTH, N_BRANCH, BRANCH_WIDTH, D), BRANCH_WIDTH ** -0.5),
        'w_out': nrm(26, (DEPTH, D, D), D ** -0.5),
        'router_w': nrm(27, (DEPTH, D, N_EXPERTS), D ** -0.5),
        'router_b': nrm(28, (DEPTH, N_EXPERTS), 0.01),
        'exp_w1': nrm(29, (DEPTH, N_EXPERTS, D, 2 * D_FF), D ** -0.5),
        'exp_b1': nrm(30, (DEPTH, N_EXPERTS, 2 * D_FF), 0.01),
        'exp_w2': nrm(31, (DEPTH, N_EXPERTS, D_FF, D), D_FF ** -0.5),
        'exp_b2': nrm(32, (DEPTH, N_EXPERTS, D), 0.01),
        'final_g': 1.0 + nrm(33, (D,), 0.02),
    }


def reference(x, c, ctx, c_ctx, w_mod, b_mod, norm1_g, norm2_g, w_in, da_lambda, da_subln_g, conv_w,
              rw_w0, rw_w1, rw_w2, rw_a0, rw_a1, rw_a2, rw_g1, rw_g2, rw_k_k, rw_k_a, rw_r_k, rw_lnx_g, rw_lnx_b,
              w_branch, w_out, router_w, router_b, exp_w1, exp_b1, exp_w2, exp_b2, final_g):
    n_tokens = x.shape[1]
    grid_rows = n_tokens // GRID_W
    rows = jnp.repeat(jnp.arange(grid_rows, dtype=jnp.int32), GRID_W)
    cols = jnp.tile(jnp.arange(GRID_W, dtype=jnp.int32), grid_rows)
    silu_c = jax.nn.silu(c)
    silu_cc = jax.nn.silu(c_ctx)
    xc = ctx
    for li in range(DEPTH):
        p = {
            'w_mod': w_mod[li], 'b_mod': b_mod[li], 'norm1_g': norm1_g[li], 'norm2_g': norm2_g[li],
            'w_in': w_in[li], 'da_lambda': da_lambda[li], 'da_subln_g': da_subln_g[li], 'conv_w': conv_w[li],
            'rw_w0': rw_w0[li], 'rw_w1': rw_w1[li], 'rw_w2': rw_w2[li],
            'rw_a0': rw_a0[li], 'rw_a1': rw_a1[li], 'rw_a2': rw_a2[li],
            'rw_g1': rw_g1[li], 'rw_g2': rw_g2[li], 'rw_k_k': rw_k_k[li], 'rw_k_a': rw_k_a[li],
            'rw_r_k': rw_r_k[li], 'rw_lnx_g': rw_lnx_g[li], 'rw_lnx_b': rw_lnx_b[li],
            'w_branch': w_branch[li], 'w_out': w_out[li],
            'router_w': router_w[li], 'router_b': router_b[li],
            'exp_w1': exp_w1[li], 'exp_b1': exp_b1[li], 'exp_w2': exp_w2[li], 'exp_b2': exp_b2[li],
        }
        x, xc = layer(x, xc, silu_c, silu_cc, rows, cols, li, li < DEPTH - 1, p)
    return rms_norm(x, final_g)
```

```python
import math
import numpy as np
from contextlib import ExitStack
import concourse.bass as bass
import concourse.mybir as mybir
from concourse.bass_utils import run_bass_kernel_spmd

F32 = mybir.dt.float32
BF16 = mybir.dt.bfloat16
AF = mybir.ActivationFunctionType
ALU = mybir.AluOpType
AX = mybir.AxisListType

D = 1024
TL = 2048
TC = 256
T = TL + TC
NT = T // 128
DEPTH = 2
IN_COLS = 7680
TGS = [(0, 512), (512, 512), (1024, 512), (1536, 512), (2048, 256)]

ENGS = ("pe", "act", "dve", "pool", "sp")
NDMASEM = 24


class Tok:
    __slots__ = ("w", "rs", "name")

    def __init__(self, name=""):
        self.w = None
        self.rs = []
        self.name = name


def toks(n):
    return [Tok() for _ in range(n)]


class Ev:
    __slots__ = ("kind", "eng", "idx", "sem", "target", "op")

    def __init__(self, kind, eng, idx, sem=None, target=None, op=None):
        self.kind, self.eng, self.idx, self.sem, self.target, self.op = kind, eng, idx, sem, target, op


class Op:
    __slots__ = ("eng", "fn", "dma", "idx", "waits", "flag", "tick", "dsem", "dtarget", "prewait")


class Prog:
    def __init__(self):
        self.ops = {e: [] for e in ENGS}
        self.ndma = {e: 0 for e in ENGS}
        self.seen = {e: {} for e in ENGS}
        self.last = {e: None for e in ENGS}
        self.pend_dma = {}

    def op(self, eng, fn, r=(), w=(), dma=False, extra=()):
        o = Op()
        o.eng, o.fn, o.dma = eng, fn, dma
        o.idx = len(self.ops[eng])
        o.flag = False
        o.tick = None
        o.prewait = None
        waits = {}

        def need(ev, force=False):
            if ev is None:
                return
            if ev.kind == "dma":
                k = ("dma", ev.eng, ev.sem)
                if waits.get(k, (0, None))[0] < ev.target:
                    waits[k] = (ev.target, ev)
            else:
                if ev.eng == eng and not dma and not force:
                    if eng == "pe":
                        return
                    if o.idx - ev.idx > 2:
                        return
                k = ("eng", ev.eng)
                if waits.get(k, (-1, None))[0] < ev.idx:
                    waits[k] = (ev.idx, ev)

        for t in r:
            need(t.w)
        for t in w:
            need(t.w)
            for ev in t.rs:
                need(ev)
        for ev in extra:
            if not (ev.kind == "eng" and ev.eng == eng):
                need(ev, force=True)
        seen = self.seen[eng]
        fw = {}
        for k, (v, ev) in waits.items():
            if seen.get(k, -1) >= v:
                continue
            seen[k] = v
            fw[k] = (v, ev)
            if ev.kind == "eng":
                ev.op.flag = True
        o.waits = fw
        if dma:
            n = self.ndma[eng]
            self.ndma[eng] = n + 1
            o.dsem = n % NDMASEM
            o.dtarget = 16 * (n // NDMASEM + 1)
            if n >= NDMASEM:
                o.prewait = (o.dsem, o.dtarget - 16)
            ev = Ev("dma", eng, o.idx, o.dsem, o.dtarget, o)
            self.pend_dma[(eng, o.dsem)] = ev
        else:
            ev = Ev("eng", eng, o.idx, op=o)
            if fn is not None:
                self.last[eng] = ev
        for t in r:
            t.rs.append(ev)
        for t in w:
            t.w = ev
            t.rs = []
        self.ops[eng].append(o)
        return o

    def pe(self, fn, r=(), w=()):
        return self.op("pe", fn, r, w)

    def act(self, fn, r=(), w=()):
        return self.op("act", fn, r, w)

    def dve(self, fn, r=(), w=()):
        return self.op("dve", fn, r, w)

    def pool(self, fn, r=(), w=()):
        return self.op("pool", fn, r, w)

    def dma(self, q, fn, r=(), w=()):
        return self.op(q, fn, r, w, dma=True)

    def barrier(self):
        evs = [ev for ev in self.last.values() if ev is not None] + list(self.pend_dma.values())
        self.pend_dma = {}
        for e in ENGS:
            self.op(e, None, extra=evs)

    def emit(self, nc, stack):
        for e in ENGS:
            t = 0
            for o in self.ops[e]:
                if o.flag and not o.dma:
                    t += 1
                    o.tick = t
        esem = {e: stack.enter_context(nc.semaphore(f"s_{e}")) for e in ENGS}
        dsem = {e: [stack.enter_context(nc.semaphore(f"d_{e}{i}")) for i in range(NDMASEM)]
                for e in ENGS if self.ndma[e] > 0}
        block = stack.enter_context(nc.Block())
        prog = self

        def runner(ename):
            def f(eng):
                for o in prog.ops[ename]:
                    for k, (v, ev) in o.waits.items():
                        if k[0] == "dma":
                            eng.wait_ge(dsem[k[1]][k[2]], v)
                        else:
                            eng.wait_ge(esem[k[1]], ev.op.tick)
                    if o.dma and o.prewait is not None:
                        eng.wait_ge(dsem[ename][o.prewait[0]], o.prewait[1])
                    if o.fn is None:
                        continue
                    ins = o.fn(eng)
                    if o.dma:
                        ins.then_inc(dsem[ename][o.dsem], 16)
                    elif o.flag:
                        ins.then_inc(esem[ename], 1)
            return f

        block.sync(runner("sp"))
        block.tensor(runner("pe"))
        block.scalar(runner("act"))
        block.vector(runner("dve"))
        block.gpsimd(runner("pool"))


class Arena:
    def __init__(self, ap, n):
        self.ap, self.n, self.off = ap, n, 0

    def alloc(self, n, dt=F32):
        nf = n if dt == F32 else (n + 1) // 2
        nf = (nf + 7) // 8 * 8
        assert self.off + nf <= self.n, ("arena overflow", self.off, nf, self.n)
        a = self.ap[:, self.off:self.off + nf]
        self.off += nf
        if dt != F32:
            a = a.bitcast(dt)[:, 0:n]
        else:
            a = a[:, 0:n]
        return a

    def mark(self):
        return self.off

    def release(self, m):
        self.off = m


class Ring:
    def __init__(self, bufs):
        self.bufs = bufs
        self.toks = toks(len(bufs))
        self.i = 0

    def next(self):
        j = self.i % len(self.bufs)
        self.i += 1
        return self.bufs[j], self.toks[j]


def make_consts():
    c = {}
    c["ident"] = np.eye(128, dtype=np.float32)
    c["ones"] = np.ones((128, 128), dtype=np.float32)
    pos_r = (np.arange(TL) // 64).astype(np.float32)
    pos_c = (np.arange(TL) % 64).astype(np.float32)
    inv = np.power(10000.0, -np.arange(16, dtype=np.float32) / 16).astype(np.float32)
    cosT = np.zeros((128, TL), np.float32)
    sinT = np.zeros((128, TL), np.float32)
    pm = np.zeros((128, 128), np.float32)
    for p in range(128):
        d = p % 64
        pos = pos_r if d < 32 else pos_c
        ang = pos * inv[d % 16]
        cosT[p] = np.cos(ang)
        sinT[p] = np.sin(ang)
        if (d % 32) < 16:
            pm[p + 16, p] = -1.0
        else:
            pm[p - 16, p] = 1.0
    c["cosT"], c["sinT"], c["pm"] = cosT, sinT, pm
    s = np.arange(128)[:, None]
    t = np.arange(128)[None, :]
    c["m_f_strict"] = (s < t).astype(np.float32)
    c["m_f_incl"] = (s <= t).astype(np.float32)
    c["m_r_strict"] = (s > t).astype(np.float32)
    c["m_r_incl"] = (s >= t).astype(np.float32)
    bd = np.zeros((128, 128), np.float32)
    bd[:64, :64] = 1
    bd[64:, 64:] = 1
    c["blockdiag"] = bd
    return c


CONST_NAMES = ["ident", "ones", "cosT", "sinT", "pm", "m_f_strict", "m_f_incl", "m_r_strict", "m_r_incl", "blockdiag"]

WEIGHT_SPECS = [
    ("w_mod", [DEPTH, D, 6 * D]), ("b_mod", [DEPTH, 6 * D]), ("norm1_g", [DEPTH, D]), ("norm2_g", [DEPTH, D]),
    ("w_in", [DEPTH, D, IN_COLS]), ("da_lambda", [DEPTH, 4, 64]), ("da_subln_g", [DEPTH, 128]),
    ("conv_w", [DEPTH, 3, 512]), ("rw_w0", [DEPTH, 2, 512]), ("rw_w1", [DEPTH, 2, D, 64]),
    ("rw_w2", [DEPTH, 2, 64, 512]), ("rw_a0", [DEPTH, 2, 512]), ("rw_a1", [DEPTH, 2, D, 64]),
    ("rw_a2", [DEPTH, 2, 64, 512]), ("rw_g1", [DEPTH, D, 160]), ("rw_g2", [DEPTH, 160, 512]),
    ("rw_k_k", [DEPTH, 512]), ("rw_k_a", [DEPTH, 512]), ("rw_r_k", [DEPTH, 8, 64]),
    ("rw_lnx_g", [DEPTH, 512]), ("rw_lnx_b", [DEPTH, 512]), ("w_branch", [DEPTH, 3, 512, D]),
    ("w_out", [DEPTH, D, D]), ("router_w", [DEPTH, D, 32]), ("router_b", [DEPTH, 32]),
    ("exp_w1", [DEPTH, 32, D, 2 * D]), ("exp_b1", [DEPTH, 32, 2 * D]), ("exp_w2", [DEPTH, 32, D, D]),
    ("exp_b2", [DEPTH, 32, D]), ("final_g", [D]),
]


def I_copy(o, i):
    return lambda e: e.tensor_copy(out=o, in_=i)


def I_acopy(o, i):
    return lambda e: e.activation(out=o, in_=i, func=AF.Copy)


def I_act(o, i, func, bias=None, scale=None):
    kw = {}
    if bias is not None:
        kw["bias"] = bias
    if scale is not None:
        kw["scale"] = scale
    return lambda e: e.activation(out=o, in_=i, func=func, **kw)


def I_mm(o, l, r, start=True, stop=True):
    return lambda e: e.matmul(o, lhsT=l, rhs=r, start=start, stop=stop)


def I_tr(o, i, ident):
    return lambda e: e.transpose(out=o, in_=i, identity=ident)


def I_tt(o, a, b, op):
    return lambda e: e.tensor_tensor(out=o, in0=a, in1=b, op=op)


def I_ts(o, a, s1, op0, s2=None, op1=None):
    if op1 is None:
        return lambda e: e.tensor_scalar(out=o, in0=a, scalar1=s1, scalar2=None, op0=op0)
    return lambda e: e.tensor_scalar(out=o, in0=a, scalar1=s1, scalar2=s2, op0=op0, op1=op1)


def I_stt(o, in0, scalar, in1, op0, op1):
    return lambda e: e.scalar_tensor_tensor(out=o, in0=in0, scalar=scalar, in1=in1, op0=op0, op1=op1)


def I_red(o, i, op=ALU.add):
    return lambda e: e.tensor_reduce(out=o, in_=i, axis=AX.X, op=op)


def I_dma(o, i):
    return lambda e: e.dma_start(out=o, in_=i)


def I_memset(o, v):
    return lambda e: e.memset(o, v)


ARENA_F32 = 52224


class MK:
    def __init__(self, n_layers=DEPTH, dbg=(), stop=None, skip=(), dbg_br=False):
        self.skip = set(skip)
        self.n_exp_dbg = 1
        self.dbg_br = dbg_br
        self.n_layers = n_layers
        self.dbg_names = set(dbg)
        self.stop = stop
        nc = bass.Bass("TRN2", target_bir_lowering=False)
        self.nc = nc
        self.st = ExitStack()
        self.P = Prog()
        self.x_d = nc.dram_tensor("x", [TL, D], F32, kind="ExternalInput").ap()
        self.ctx_d = nc.dram_tensor("ctx", [TC, D], F32, kind="ExternalInput").ap()
        self.c2_d = nc.dram_tensor("c2", [2, D], F32, kind="ExternalInput").ap()
        self.W = {n: nc.dram_tensor(n, s, F32, kind="ExternalInput").ap() for n, s in WEIGHT_SPECS}
        cs = make_consts()
        self.Cd = {n: nc.dram_tensor("k_" + n, list(cs[n].shape), F32, kind="ExternalInput").ap() for n in CONST_NAMES}
        self.out_d = nc.dram_tensor("out", [TL, D], F32, kind="ExternalOutput").ap()
        self.brT_d = nc.dram_tensor("brT", [3, 4, 128, T], BF16).ap()
        self.yscr_d = nc.dram_tensor("yscr", [2, NT, 128, 1024], F32).ap()
        self.gt_d = nc.dram_tensor("gtscr", [32, T], F32).ap()
        self.dbg_out = {}
        self.out_toks = []
        arena_t = self.st.enter_context(nc.sbuf_tensor("arena", [128, ARENA_F32], F32))
        self.A = Arena(arena_t[:], ARENA_F32)
        banks = [self.st.enter_context(nc.psum_tensor(f"psb{i}", [128, 512], F32))[:] for i in range(8)]
        self.PS = Ring(banks[0:4])
        self.PSacc = Ring(banks[4:8])
        self.t_br = toks(3)
        self.xscr_d = nc.dram_tensor("xscr", [128, 8 * T], F32).ap()

    def dbg(self, name, ap, tok, shape, dt=F32):
        if name not in self.dbg_names:
            return
        d = self.nc.dram_tensor("dbg_" + name, list(shape), dt, kind="ExternalOutput").ap()
        t = Tok()
        self.P.dma("sp", I_dma(d, ap), r=tok if isinstance(tok, (list, tuple)) else [tok], w=[t])
        self.out_toks.append(t)
        self.dbg_out[name] = "dbg_" + name

    def load_w_bf16(self, dst3, src2d, tok, kc=8):
        self.P.dma("pool", I_dma(dst3, src2d.rearrange("(k p) c -> p k c", p=128)), w=[tok])

    def rows_to_cols(self, dst, rows, n, tok_rows, tok_dst, scale=None):
        P = self.P
        ps, pt = self.PS.next()
        for j in range(n):
            P.pe(I_mm(ps[:, j:j + 1], rows[0:1, j * 128:(j + 1) * 128], self.ones[0:1, 0:1]), r=[tok_rows, self.t_c], w=[pt])
        P.dve(I_copy(dst, ps[:, 0:n]), r=[pt], w=[tok_dst])

    def bcast_rows(self, dst, row, n, tok_row, tok_dst, scale=None, ones=None):
        P = self.P
        ps, pt = self.PS.next()
        P.pe(I_mm(ps[:, 0:n], (self.ones if ones is None else ones)[0:1, 0:128], row[0:1, 0:n]), r=[tok_row, self.t_c], w=[pt])
        if scale is None:
            P.dve(I_copy(dst, ps[:, 0:n]), r=[pt], w=[tok_dst])
        else:
            P.dve(I_ts(dst, ps[:, 0:n], float(scale), ALU.mult), r=[pt], w=[tok_dst])

    def rsqrt(self, out, in_, mult, eps, r, tok):
        self.P.act(I_act(out, in_, AF.Ln, bias=self.epsc[:, self.eps_idx[eps]:self.eps_idx[eps] + 1][0:out.shape[0]], scale=float(mult)), r=list(r) + [self.t_c], w=[tok])
        self.P.act(I_act(out, out, AF.Exp, scale=-0.5), r=[tok], w=[tok])

    def AL(self, n, dt=F32):
        try:
            return self.B.alloc(n, dt)
        except AssertionError:
            return self.A.alloc(n, dt)

    def spill_x(self):
        P = self.P
        self.t_xs = Tok()
        for k in range(8):
            P.dma("sp" if k % 2 == 0 else "act", I_dma(self.xscr_d[:, k * T:(k + 1) * T], self.xT[:, k * T:(k + 1) * T]), r=self.t_x, w=[self.t_xs])
        P.barrier()
        self.B = Arena(self.xT, 8 * T)

    def restore_x(self):
        P = self.P
        P.barrier()
        for k in range(8):
            P.dma("sp" if k % 2 == 0 else "act", I_dma(self.xT[:, k * T:(k + 1) * T], self.xscr_d[:, k * T:(k + 1) * T]), r=[self.t_xs], w=self.t_x)
        P.barrier()

    def setup(self):
        P, A = self.P, self.A
        self.t_c = Tok()
        self.ident = A.alloc(128)
        self.ones = A.alloc(128)
        self.ident_b = A.alloc(128, BF16)
        self.ones_b = A.alloc(128, BF16)
        P.dma("sp", I_dma(self.ident, self.Cd["ident"]), w=[self.t_c])
        P.dma("sp", I_dma(self.ones, self.Cd["ones"]), w=[self.t_c])
        P.dma("pool", I_dma(self.ident_b, self.Cd["ident"]), w=[self.t_c])
        P.dma("pool", I_dma(self.ones_b, self.Cd["ones"]), w=[self.t_c])
        self.epsc = A.alloc(8)
        self.eps_idx = {1e-6: 0, 1e-5: 1, 64e-5: 2, 1e-24: 3}
        for v, i in self.eps_idx.items():
            P.dve(I_memset(self.epsc[:, i:i + 1], v), w=[self.t_c])
        self.xT = A.alloc(8 * T)
        self.xT3 = self.xT.rearrange("p (k t) -> p k t", k=8)
        self.t_x = toks(5)
        self.hT = A.alloc(8 * T, BF16)
        self.hT3 = self.hT.rearrange("p (k t) -> p k t", k=8)
        self.t_h = toks(5)
        self.csT = A.alloc(16)
        self.t_cs = Tok()
        self.eff = A.alloc(96)
        self.eff4 = self.eff.rearrange("p (n k c) -> p n k c", n=6, k=8)
        self.t_eff = Tok()
        self.gvec = A.alloc(24)
        self.t_gvec = Tok()

    def load_x(self):
        P, A = self.P, self.A
        m = A.mark()
        stage = Ring([A.alloc(1024) for _ in range(2)])
        for i in range(NT):
            sb, stt = stage.next()
            src = self.x_d[i * 128:(i + 1) * 128, :] if i < 16 else self.ctx_d[(i - 16) * 128:(i - 15) * 128, :]
            P.dma("sp", I_dma(sb, src), w=[stt])
            for half in range(2):
                ps, pt = self.PS.next()
                for j in range(4):
                    k = half * 4 + j
                    P.pe(I_tr(ps[:, j * 128:(j + 1) * 128], sb[:, k * 128:(k + 1) * 128], self.ident), r=[stt, self.t_c], w=[pt])
                fn = I_copy(self.xT3[:, half * 4:half * 4 + 4, i * 128:(i + 1) * 128], ps.rearrange("p (a b) -> p a b", a=4))
                P.dve(fn, r=[pt], w=[self.t_x[min(i // 4, 4)]])
        c2s = A.alloc(1024)
        tcs = Tok()
        P.dma("sp", I_dma(c2s[0:2, :], self.c2_d), w=[tcs])
        P.act(I_act(c2s[0:2, :], c2s[0:2, :], AF.Silu), r=[tcs], w=[tcs])
        ps, pt = self.PS.next()
        for k in range(8):
            P.pe(I_tr(ps[:, k * 2:k * 2 + 2], c2s[0:2, k * 128:(k + 1) * 128], self.ident[0:2, 0:2]), r=[tcs, self.t_c], w=[pt])
        P.dve(I_copy(self.csT, ps[:, 0:16]), r=[pt], w=[self.t_cs])
        P.barrier()
        A.release(m)

    def mod_phase(self, li):
        P, A = self.P, self.A
        m = A.mark()
        brow = A.alloc(6 * D)
        grow = A.alloc(3 * D)
        tb, tg = Tok(), Tok()
        P.dma("sp", I_dma(brow[0:1, :], self.W["b_mod"][li:li + 1, :]), w=[tb])
        P.dma("sp", I_dma(grow[0:1, 0:D], self.W["norm1_g"][li:li + 1, :]), w=[tg])
        P.dma("sp", I_dma(grow[0:1, D:2 * D], self.W["norm2_g"][li:li + 1, :]), w=[tg])
        P.dma("sp", I_dma(grow[0:1, 2 * D:3 * D], self.W["final_g"].rearrange("(o d) -> o d", o=1)), w=[tg])
        self.rows_to_cols(self.gvec, grow, 24, tg, self.t_gvec)
        wring = Ring([A.alloc(8 * 512) for _ in range(2)])
        psm, ptm = self.PS.next()
        for piece in range(12):
            wp, wt = wring.next()
            wp3 = wp.rearrange("p (k c) -> p k c", k=8)
            src = self.W["w_mod"][li][:, piece * 512:(piece + 1) * 512].rearrange("(k p) c -> p k c", p=128)
            P.dma("sp" if piece % 2 == 0 else "act", I_dma(wp3, src), w=[wt])
            for jj in range(4):
                j = piece * 4 + jj
                for kc in range(8):
                    P.pe(I_mm(psm[:, j * 2:j * 2 + 2], wp3[:, kc, jj * 128:(jj + 1) * 128], self.csT[:, kc * 2:kc * 2 + 2],
                              start=(kc == 0), stop=False), r=[wt, self.t_cs], w=[ptm])
                P.pe(I_mm(psm[:, j * 2:j * 2 + 2], brow[0:1, j * 128:(j + 1) * 128], self.ones[0:1, 0:2], start=False, stop=True),
                     r=[tb, self.t_c], w=[ptm])
        P.dve(I_copy(self.eff, psm[:, 0:96]), r=[ptm], w=[self.t_eff])
        for n, gi in ((1, 0), (4, 1)):
            P.dve(I_stt(self.eff4[:, n], self.eff4[:, n], 1.0,
                        self.gvec[:, gi * 8:(gi + 1) * 8].unsqueeze(2).to_broadcast([128, 8, 2]), ALU.add, ALU.mult),
                  r=[self.t_eff, self.t_gvec], w=[self.t_eff])
        P.barrier()
        A.release(m)

    def norm_phase(self, which, li, router=None):
        P, A = self.P, self.A
        m = A.mark()
        n_sh, n_sc = (0, 1) if which == 0 else (3, 4)
        sq = A.alloc(8 * 512)
        sq3 = sq.rearrange("p (k t) -> p k t", k=8)
        tsq = Tok()
        rstd_r = Ring([A.alloc(512) for _ in range(2)])
        tmp_r = Ring([A.alloc(512) for _ in range(3)])
        if router is not None:
            h2f = A.alloc(8 * 512)
            h2f3 = h2f.rearrange("p (k t) -> p k t", k=8)
            th2f = Tok()
        for tg, (s, n) in enumerate(TGS):
            col = 0 if tg < 4 else 1
            P.act(I_act(sq3[:, :, 0:n], self.xT3[:, :, s:s + n], AF.Square), r=[self.t_x[tg]], w=[tsq])
            ps, pt = self.PS.next()
            for k in range(8):
                P.pe(I_mm(ps[:, 0:n], self.ones, sq3[:, k, 0:n], start=(k == 0), stop=(k == 7)), r=[tsq, self.t_c], w=[pt])
            rstd, trs = rstd_r.next()
            self.rsqrt(rstd[:, 0:n], ps[:, 0:n], 1.0 / D, 1e-6, [pt], trs)
            for k in range(8):
                tmp, tt = tmp_r.next()
                P.op("dve",
                     I_stt(tmp[:, 0:n], self.xT3[:, k, s:s + n], self.eff4[:, n_sc, k, col:col + 1], rstd[:, 0:n], ALU.mult, ALU.mult),
                     r=[self.t_x[tg], self.t_eff, trs], w=[tt])
                if router is None:
                    P.act(I_act(self.hT3[:, k, s:s + n], tmp[:, 0:n], AF.Identity, bias=self.eff4[:, n_sh, k, col:col + 1]),
                          r=[tt, self.t_eff], w=[self.t_h[tg]])
                else:
                    P.act(I_act(h2f3[:, k, 0:n], tmp[:, 0:n], AF.Identity, bias=self.eff4[:, n_sh, k, col:col + 1]),
                          r=[tt, self.t_eff], w=[th2f])
                    P.pool(I_copy(self.hT3[:, k, s:s + n], h2f3[:, k, 0:n]), r=[th2f], w=[self.t_h[tg]])
            if router is not None:
                router(tg, s, n, h2f3, th2f)
        P.barrier()
        A.release(m)


    def attn_phase(self, li):
        P, A = self.P, self.A
        m = A.mark()
        self.B.off = 0
        lam_init = 0.8 - 0.6 * math.exp(-0.3 * li)
        W = self.W["w_in"][li]
        wq = self.AL(8 * 512, BF16).rearrange("p (k c) -> p k c", k=8)
        wk = self.AL(8 * 512, BF16).rearrange("p (k c) -> p k c", k=8)
        wv = self.AL(8 * 512, BF16).rearrange("p (k c) -> p k c", k=8)
        tw = toks(3)
        self.load_w_bf16(wq, W[:, 0:512], tw[0])
        self.load_w_bf16(wk, W[:, 512:1024], tw[1])
        self.load_w_bf16(wv, W[:, 1024:1536], tw[2])
        cosT = self.AL(TL)
        sinT = self.AL(TL)
        pm = self.AL(128, BF16)
        trope = Tok()
        P.dma("sp", I_dma(cosT, self.Cd["cosT"]), w=[trope])
        P.dma("act", I_dma(sinT, self.Cd["sinT"]), w=[trope])
        P.dma("pool", I_dma(pm, self.Cd["pm"]), w=[trope])
        qT = self.AL(4 * T, BF16).rearrange("p (h t) -> p h t", h=4)
        kT = self.AL(4 * T, BF16).rearrange("p (h t) -> p h t", h=4)
        vaug = self.AL(NT * 4 * 130, BF16).rearrange("p (i h e) -> p i h e", i=NT, h=4)
        t_q, t_k, t_v = toks(5), toks(5), toks(NT)
        tv1 = Tok()
        P.pool(I_memset(vaug[:, :, :, 128:129], 1.0), w=t_v)
        rows = self.AL(512)
        trow = Tok()
        P.dma("sp", I_dma(rows[0:1, 0:256], self.W["da_lambda"][li:li + 1].rearrange("o a b -> o (a b)")), w=[trow])
        P.dma("sp", I_dma(rows[0:1, 256:384], self.W["da_subln_g"][li:li + 1, :]), w=[trow])
        lsm = self.AL(16)
        tl = Tok()
        P.dve(I_tt(rows[0:1, 384:448], rows[0:1, 0:64], rows[0:1, 64:128], ALU.mult), r=[trow], w=[trow])
        P.dve(I_tt(rows[0:1, 448:512], rows[0:1, 128:192], rows[0:1, 192:256], ALU.mult), r=[trow], w=[trow])
        P.dve(I_red(lsm[0:1, 0:2], rows[0:1, 384:512].rearrange("o (a b) -> o a b", a=2)), r=[trow], w=[tl])
        P.act(I_act(lsm[0:1, 2:4], lsm[0:1, 0:2], AF.Exp), r=[tl], w=[tl])
        P.dve(I_tt(lsm[0:1, 4:5], lsm[0:1, 3:4], lsm[0:1, 2:3], ALU.subtract), r=[tl], w=[tl])
        P.dve(I_ts(lsm[0:1, 4:5], lsm[0:1, 4:5], -lam_init, ALU.add), r=[tl], w=[tl])
        neglam = self.AL(8)
        gsub = self.AL(128)
        tng = Tok()
        self.bcast_rows(neglam[:, 0:1], lsm[:, 4:5], 1, tl, tng)
        self.bcast_rows(gsub, rows[:, 256:384], 128, trow, tng, scale=(1.0 - lam_init))
        raw_r = Ring([self.AL(512, BF16) for _ in range(2)])
        t1_r = Ring([self.AL(512) for _ in range(2)])
        t2_r = Ring([self.AL(512) for _ in range(2)])
        for (w3, twk, dstT, tdst) in ((wq, tw[0], qT, t_q), (wk, tw[1], kT, t_k)):
            for h in range(4):
                for tg, (s, n) in enumerate(TGS):
                    ps, pt = self.PS.next()
                    for kc in range(8):
                        P.pe(I_mm(ps[:, 0:n], w3[:, kc, h * 128:(h + 1) * 128], self.hT3[:, kc, s:s + n], start=(kc == 0), stop=(kc == 7)),
                             r=[twk, self.t_h[tg]], w=[pt])
                    if tg == 4:
                        P.act(I_acopy(dstT[:, h, s:s + n], ps[:, 0:n]), r=[pt], w=[tdst[tg]])
                        continue
                    raw, tr = raw_r.next()
                    P.act(I_acopy(raw[:, 0:n], ps[:, 0:n]), r=[pt], w=[tr])
                    ps2, pt2 = self.PS.next()
                    P.pe(I_mm(ps2[:, 0:n], pm, raw[:, 0:n]), r=[trope, tr], w=[pt2])
                    a1, ta1 = t1_r.next()
                    a2, ta2 = t2_r.next()
                    P.pool(I_tt(a1[:, 0:n], raw[:, 0:n], cosT[:, s:s + n], ALU.mult), r=[tr, trope], w=[ta1])
                    P.dve(I_tt(a2[:, 0:n], ps2[:, 0:n], sinT[:, s:s + n], ALU.mult), r=[pt2, trope], w=[ta2])
                    P.pool(I_tt(dstT[:, h, s:s + n], a1[:, 0:n], a2[:, 0:n], ALU.add), r=[ta1, ta2], w=[tdst[tg]])
        for i in range(NT):
            ps, pt = self.PS.next()
            for kc in range(8):
                P.pe(I_mm(ps, self.hT3[:, kc, i * 128:(i + 1) * 128], wv[:, kc, :], start=(kc == 0), stop=(kc == 7)),
                     r=[tw[2], self.t_h[min(i // 4, 4)]], w=[pt])
            P.act(I_acopy(vaug[:, i, :, 0:128], ps.rearrange("p (h e) -> p h e", h=4)), r=[pt], w=[t_v[i]])
        self.dbg("qT", qT.rearrange("p h t -> p (h t)"), t_q, [128, 4 * T], BF16)
        self.dbg("kT", kT.rearrange("p h t -> p (h t)"), t_k, [128, 4 * T], BF16)
        if self.stop == f"attnproj_{li}":
            P.barrier()
            A.release(m)
            return
        Ea_r = Ring([self.AL(NT * 256, BF16) for _ in range(2)])
        osb = [self.AL(4 * 130) for _ in range(2)]
        tosb = toks(2)
        oda = self.AL(4 * 512)
        oda3 = oda.rearrange("p (j c) -> p j c", j=4)
        toda = Tok()
        sm_r = Ring([self.AL(8) for _ in range(2)])
        o1_r = Ring([self.AL(128) for _ in range(2)])
        o2_r = Ring([self.AL(128) for _ in range(2)])
        stg_r = Ring([self.AL(512, BF16) for _ in range(2)])
        groups = [(s, 256, list(range(NT)), s // 512) for s in range(0, TL, 256)] + [(2048, 256, [16, 17], 4)]
        for (qs0, qn, ktiles, tg) in groups:
            nq = qn // 128
            for h in range(4):
                for sm in range(2):
                    R_ = slice(sm * 64, (sm + 1) * 64)
                    Ea, tea = Ea_r.next()
                    Ea3 = Ea.rearrange("p (k q) -> p k q", k=NT)
                    for idx, kt in enumerate(ktiles):
                        ps, pt = self.PS.next()
                        P.pe(I_mm(ps[:, 0:qn], kT[R_, h, kt * 128:(kt + 1) * 128], qT[R_, h, qs0:qs0 + qn]),
                             r=[t_k[min(kt // 4, 4)], t_q[tg]], w=[pt])
                        P.act(I_act(Ea3[:, idx, 0:qn], ps[:, 0:qn], AF.Exp, scale=0.125), r=[pt], w=[tea])
                    for j in range(nq):
                        ob, tob = self.PSacc.next()
                        for idx, kt in enumerate(ktiles):
                            P.pe(I_mm(ob[:, 0:129], Ea3[:, idx, j * 128:(j + 1) * 128], vaug[:, kt, h, 0:129],
                                      start=(idx == 0), stop=(idx == len(ktiles) - 1)), r=[tea, t_v[kt]], w=[tob])
                        P.dve(I_copy(osb[sm][:, j * 130:j * 130 + 129], ob[:, 0:129]), r=[tob], w=[tosb[sm]])
                for j in range(nq):
                    s8, ts8 = sm_r.next()
                    o1, to1 = o1_r.next()
                    o2, to2 = o2_r.next()
                    P.dve(lambda e, o=s8[:, 0:1], i=osb[0][:, j * 130 + 128:j * 130 + 129]: e.reciprocal(out=o, in_=i), r=[tosb[0]], w=[ts8])
                    P.dve(lambda e, o=s8[:, 1:2], i=osb[1][:, j * 130 + 128:j * 130 + 129]: e.reciprocal(out=o, in_=i), r=[tosb[1]], w=[ts8])
                    P.dve(I_tt(s8[:, 2:3], s8[:, 1:2], neglam[:, 0:1], ALU.mult), r=[ts8, tng], w=[ts8])
                    P.dve(I_ts(o1, osb[0][:, j * 130:j * 130 + 128], s8[:, 0:1], ALU.mult), r=[tosb[0], ts8], w=[to1])
                    P.dve(I_stt(o1, osb[1][:, j * 130:j * 130 + 128], s8[:, 2:3], o1, ALU.mult, ALU.add), r=[tosb[1], ts8, to1], w=[to1])
                    P.pool(I_tt(o2, o1, o1, ALU.mult), r=[to1], w=[to2])
                    P.dve(I_red(s8[:, 3:4], o2), r=[to2], w=[ts8])
                    self.rsqrt(s8[:, 4:5], s8[:, 3:4], 1.0 / 128, 1e-5, [ts8], ts8)
                    P.dve(I_stt(oda3[:, j, h * 128:(h + 1) * 128], o1, s8[:, 4:5], gsub, ALU.mult, ALU.mult), r=[to1, ts8, tng], w=[toda])
            for j in range(nq):
                ti = qs0 // 128 + j
                ps, pt = self.PS.next()
                for h in range(4):
                    P.pe(I_tr(ps[:, h * 128:(h + 1) * 128], oda3[:, j, h * 128:(h + 1) * 128], self.ident), r=[toda, self.t_c], w=[pt])
                stg, tst = stg_r.next()
                P.act(I_acopy(stg, ps), r=[pt], w=[tst])
                P.dma("sp", I_dma(self.brT_d[0, :, :, ti * 128:(ti + 1) * 128].rearrange("k p t -> p k t"),
                                  stg.rearrange("p (k t) -> p k t", k=4)), r=[tst], w=[self.t_br[0]])
        P.barrier()
        A.release(m)

    def conv_phase(self, li):
        P, A = self.P, self.A
        m = A.mark()
        self.B.off = 0
        W = self.W["w_in"][li]
        ws = [self.AL(8 * 512, BF16).rearrange("p (k c) -> p k c", k=8) for _ in range(3)]
        tw = toks(3)
        for i in range(3):
            self.load_w_bf16(ws[i], W[:, 1536 + i * 512:2048 + i * 512], tw[i])
        rows = self.AL(1536)
        trow = Tok()
        P.dma("sp", I_dma(rows[0:1, :], self.W["conv_w"][li:li + 1].rearrange("o a b -> o (a b)")), w=[trow])
        cw = self.AL(16)
        tcw = Tok()
        self.rows_to_cols(cw[:, 0:12], rows, 12, trow, tcw)
        ubuf = self.AL(2312)
        tub = Tok()
        P.pool(I_memset(ubuf, 0.0), w=[tub])
        sbT = self.AL(T, BF16)
        tsb = Tok()
        sg_r = Ring([self.AL(512) for _ in range(2)])
        c1 = self.AL(TL)
        tc1 = Tok()
        osc = self.AL(T, BF16)
        tosc = Tok()
        for ch in range(4):
            for tg, (s, n) in enumerate(TGS):
                pss = [self.PS.next() for _ in range(3)]
                for i in range(3):
                    for kc in range(8):
                        P.pe(I_mm(pss[i][0][:, 0:n], ws[i][:, kc, ch * 128:(ch + 1) * 128], self.hT3[:, kc, s:s + n],
                                  start=(kc == 0), stop=(kc == 7)), r=[tw[i], self.t_h[tg]], w=[pss[i][1]])
                off = 1 + s if tg < 4 else 2051
                sg, tsg = sg_r.next()
                P.act(I_acopy(sbT[:, s:s + n], pss[0][0][:, 0:n]), r=[pss[0][1]], w=[tsb])
                P.act(I_acopy(sg[:, 0:n], pss[1][0][:, 0:n]), r=[pss[1][1]], w=[tsg])
                P.dve(I_tt(ubuf[:, off:off + n], pss[2][0][:, 0:n], sg[:, 0:n], ALU.mult), r=[pss[2][1], tsg], w=[tub])
            for (o0, u0, n) in ((0, 0, TL), (TL, 2050, TC)):
                P.dve(I_ts(c1[:, 0:n], ubuf[:, u0:u0 + n], cw[:, ch:ch + 1], ALU.mult), r=[tub, tcw], w=[tc1])
                P.dve(I_stt(c1[:, 0:n], ubuf[:, u0 + 1:u0 + 1 + n], cw[:, 4 + ch:5 + ch], c1[:, 0:n], ALU.mult, ALU.add), r=[tub, tcw], w=[tc1])
                P.dve(I_stt(c1[:, 0:n], ubuf[:, u0 + 2:u0 + 2 + n], cw[:, 8 + ch:9 + ch], c1[:, 0:n], ALU.mult, ALU.add), r=[tub, tcw], w=[tc1])
                P.pool(I_tt(osc[:, o0:o0 + n], c1[:, 0:n], sbT[:, o0:o0 + n], ALU.mult), r=[tc1, tsb], w=[tosc])
            P.dma("sp", I_dma(self.brT_d[1, ch], osc), r=[tosc], w=[self.t_br[1]])
        P.barrier()
        A.release(m)

    def merge_phase(self, li):
        P, A = self.P, self.A
        m = A.mark()
        W = self.W["w_in"][li]
        wg = A.alloc(8 * 1024, BF16).rearrange("p (k c) -> p k c", k=8)
        wb = A.alloc(4 * 1024, BF16).rearrange("p (k c) -> p k c", k=4)
        twg, twb = Tok(), Tok()
        mg = A.alloc(8 * T, BF16).rearrange("p (k t) -> p k t", k=8)
        tmg = toks(5)
        br_r = Ring([A.alloc(4 * 512, BF16) for _ in range(2)])
        gs_r = Ring([A.alloc(512) for _ in range(2)])
        for n_ in range(3):
            self.load_w_bf16(wg, W[:, 4608 + n_ * 1024:4608 + (n_ + 1) * 1024], twg)
            self.load_w_bf16(wb, self.W["w_branch"][li, n_], twb, kc=4)
            for tg, (s, n) in enumerate(TGS):
                br, tbr = br_r.next()
                br3 = br.rearrange("p (k t) -> p k t", k=4)
                P.dma("sp", I_dma(br3[:, :, 0:n], self.brT_d[n_, :, :, s:s + n].rearrange("k p t -> p k t")), r=[self.t_br[n_]], w=[tbr])
                for cc in range(8):
                    psg, ptg = self.PS.next()
                    for kc in range(8):
                        P.pe(I_mm(psg[:, 0:n], wg[:, kc, cc * 128:(cc + 1) * 128], self.hT3[:, kc, s:s + n], start=(kc == 0), stop=(kc == 7)),
                             r=[twg, self.t_h[tg]], w=[ptg])
                    gs, tgs = gs_r.next()
                    P.act(I_act(gs[:, 0:n], psg[:, 0:n], AF.Sigmoid), r=[ptg], w=[tgs])
                    psp, ptp = self.PS.next()
                    for kc in range(4):
                        P.pe(I_mm(psp[:, 0:n], wb[:, kc, cc * 128:(cc + 1) * 128], br3[:, kc, 0:n], start=(kc == 0), stop=(kc == 3)),
                             r=[twb, tbr], w=[ptp])
                    if n_ == 0:
                        P.dve(I_tt(mg[:, cc, s:s + n], psp[:, 0:n], gs[:, 0:n], ALU.mult), r=[ptp, tgs], w=[tmg[tg]])
                    else:
                        P.dve(I_tt(gs[:, 0:n], psp[:, 0:n], gs[:, 0:n], ALU.mult), r=[ptp, tgs], w=[tgs])
                        P.pool(I_tt(mg[:, cc, s:s + n], mg[:, cc, s:s + n], gs[:, 0:n], ALU.add), r=[tmg[tg], tgs], w=[tmg[tg]])
        wo = wg
        self.load_w_bf16(wo, self.W["w_out"][li], twg)
        for tg, (s, n) in enumerate(TGS):
            col = 0 if tg < 4 else 1
            for cc in range(8):
                ps, pt = self.PS.next()
                for kc in range(8):
                    P.pe(I_mm(ps[:, 0:n], wo[:, kc, cc * 128:(cc + 1) * 128], mg[:, kc, s:s + n], start=(kc == 0), stop=(kc == 7)),
                         r=[twg, tmg[tg]], w=[pt])
                P.dve(I_stt(self.xT3[:, cc, s:s + n], ps[:, 0:n], self.eff4[:, 2, cc, col:col + 1], self.xT3[:, cc, s:s + n], ALU.mult, ALU.add),
                      r=[pt, self.t_eff, self.t_x[tg]], w=[self.t_x[tg]])
        P.barrier()
        A.release(m)

    def final_phase(self):
        P, A = self.P, self.A
        m = A.mark()
        sq = A.alloc(8 * 512)
        sq3 = sq.rearrange("p (k t) -> p k t", k=8)
        tsq = Tok()
        rstd_r = Ring([A.alloc(512) for _ in range(2)])
        y_r = Ring([A.alloc(8 * 512) for _ in range(2)])
        o_r = Ring([A.alloc(1024) for _ in range(3)])
        for tg, (s, n) in enumerate(TGS[:4]):
            P.act(I_act(sq3[:, :, 0:n], self.xT3[:, :, s:s + n], AF.Square), r=[self.t_x[tg]], w=[tsq])
            ps, pt = self.PS.next()
            for k in range(8):
                P.pe(I_mm(ps[:, 0:n], self.ones, sq3[:, k, 0:n], start=(k == 0), stop=(k == 7)), r=[tsq, self.t_c], w=[pt])
            rstd, trs = rstd_r.next()
            self.rsqrt(rstd[:, 0:n], ps[:, 0:n], 1.0 / D, 1e-6, [pt], trs)
            y, ty = y_r.next()
            y3 = y.rearrange("p (k t) -> p k t", k=8)
            for k in range(8):
                P.dve(I_stt(y3[:, k, 0:n], self.xT3[:, k, s:s + n], self.gvec[:, 16 + k:17 + k], rstd[:, 0:n], ALU.mult, ALU.mult),
                      r=[self.t_x[tg], self.t_gvec, trs], w=[ty])
            for j in range(n // 128):
                ti = s // 128 + j
                ob, tob = o_r.next()
                for half in range(2):
                    ps, pt = self.PS.next()
                    for q in range(4):
                        k = half * 4 + q
                        P.pe(I_tr(ps[:, q * 128:(q + 1) * 128], y3[:, k, j * 128:(j + 1) * 128], self.ident), r=[ty, self.t_c], w=[pt])
                    P.act(I_acopy(ob[:, half * 512:(half + 1) * 512], ps), r=[pt], w=[tob])
                to = Tok()
                P.dma("sp", I_dma(self.out_d[ti * 128:(ti + 1) * 128, :], ob), r=[tob], w=[to])
                self.out_toks.append(to)
        P.barrier()
        A.release(m)

    def rwkv_phase(self, li):
        P, A = self.P, self.A
        m = A.mark()
        W = self.W["w_in"][li]
        DS = 0.606531
        self.B.off = 0
        B = self.B
        AL = self.AL
        a512 = lambda dt=F32: AL(512, dt)
        wrkv = [A.alloc(8 * 512, BF16).rearrange("p (k c) -> p k c", k=8) for _ in range(3)]
        tw = toks(3)
        for i in range(3):
            self.load_w_bf16(wrkv[i], W[:, 3072 + i * 512:3584 + i * 512], tw[i])
        lo1 = A.alloc(8 * 416, BF16).rearrange("p (k c) -> p k c", k=8)
        tlo = Tok()
        for d in range(2):
            self.load_w_bf16(lo1[:, :, d * 64:(d + 1) * 64], self.W["rw_w1"][li, d], tlo)
            self.load_w_bf16(lo1[:, :, 128 + d * 64:128 + (d + 1) * 64], self.W["rw_a1"][li, d], tlo)
        self.load_w_bf16(lo1[:, :, 256:416], self.W["rw_g1"][li], tlo)
        lo2 = A.alloc(6 * 512, BF16).rearrange("p (k c) -> p k c", k=6)
        for d in range(2):
            P.dma("pool", I_dma(lo2[0:64, d, :], self.W["rw_w2"][li, d]), w=[tlo])
            P.dma("pool", I_dma(lo2[0:64, 2 + d, :], self.W["rw_a2"][li, d]), w=[tlo])
        P.dma("pool", I_dma(lo2[:, 4, :], self.W["rw_g2"][li, 0:128, :]), w=[tlo])
        P.dma("pool", I_dma(lo2[0:32, 5, :], self.W["rw_g2"][li, 128:160, :]), w=[tlo])
        brow = A.alloc(4 * 512, BF16)
        P.dma("pool", I_dma(brow[0:1, 0:1024], self.W["rw_w0"][li:li + 1].rearrange("o a b -> o (a b)")), w=[tlo])
        P.dma("pool", I_dma(brow[0:1, 1024:2048], self.W["rw_a0"][li:li + 1].rearrange("o a b -> o (a b)")), w=[tlo])
        rows = A.alloc(5 * 512, BF16)
        trow = Tok()
        for i, nm in enumerate(["rw_k_k", "rw_k_a", "rw_lnx_g", "rw_lnx_b"]):
            P.dma("pool", I_dma(rows[0:1, i * 512:(i + 1) * 512], self.W[nm][li:li + 1, :]), w=[trow])
        P.dma("pool", I_dma(rows[0:1, 2048:2560], self.W["rw_r_k"][li:li + 1].rearrange("o a b -> o (a b)")), w=[trow])
        bc = [A.alloc(512) for _ in range(5)]
        tbc = Tok()
        for i in range(5):
            self.bcast_rows(bc[i], rows[:, i * 512:(i + 1) * 512], 512, trow, tbc, ones=self.ones_b)
        kk_bc, ka_bc, lg_bc, lb_bc, rk_bc = bc
        A.release(A.mark())
        msk = {}
        tmk = Tok()
        for d, (nS, nI, nST) in enumerate((("m_f_strict", "m_f_incl", "m_r_strict"), ("m_r_strict", "m_r_incl", "m_f_strict"))):
            mc1, mc2, mn = A.alloc(256, BF16), A.alloc(256, BF16), A.alloc(128, BF16)
            tri = A.alloc(128)
            P.dma("pool", I_dma(mc2[:, 0:128], self.Cd[nS]), w=[tmk])
            P.dma("pool", I_dma(mc2[:, 128:256], self.Cd[nI]), w=[tmk])
            P.dma("pool", I_dma(mn, self.Cd[nST]), w=[tmk])
            P.dma("sp", I_dma(tri, self.Cd[nI]), w=[tmk])
            P.dve(I_ts(mc1[:, 0:128], mc2[:, 0:128], -1.0, ALU.mult), r=[tmk], w=[tmk])
            P.dve(I_copy(mc1[:, 128:256], mc2[:, 128:256]), r=[tmk], w=[tmk])
            P.dve(I_ts(mn, mn, -1.0, ALU.mult), r=[tmk], w=[tmk])
            P.dve(I_ts(tri, tri, -DS, ALU.mult), r=[tmk], w=[tmk])
            msk[d] = (mc1, mc2, mn, tri)
        allneg = A.alloc(128)
        P.dve(I_ts(allneg, self.ones, -DS, ALU.mult), r=[self.t_c], w=[tmk])
        Mst = [[A.alloc(512) for _ in range(2)] for _ in range(2)]
        tM = [toks(2) for _ in range(2)]
        for d in range(2):
            P.pool(I_memset(Mst[d][0], 0.0), w=[tM[d][0]])
        mpar = [0, 0]
        r_s, k_s, v_s, sig, a_s, kx, kk, b_s, kE, tA, tB, cum_s = [a512() for _ in range(12)]
        tk_ = {n: Tok() for n in "r k v sig a kx kk b kE tA tB cum l1 la e1 e2 e3 e4 kt bt k2 rt kh bh vb ktb rtb fT pc dg".split()}
        l1 = AL(128, BF16)
        la = AL(128, BF16)
        e1, e2, e3, e4 = [a512(BF16) for _ in range(4)]
        kt_, bt_, k2_, rt_ = [a512(BF16) for _ in range(4)]
        kh_, bh_, vb_ = [a512(BF16) for _ in range(3)]
        red8 = A.alloc(16)
        tred = Tok()
        pcb = a512()
        dg = a512()
        fT = [AL(4 * 128, BF16).rearrange("p (q t) -> p q t", q=4) for _ in range(4)]
        tfT = toks(4)
        hb = []
        for h in range(8):
            d_ = {}
            for nm, sz in (("X0", 128), ("X1", 128), ("N0", 128), ("N1", 128), ("P0", 128), ("P1", 128), ("A1", 256), ("A2", 256), ("KAV", 128), ("WU", 128)):
                d_[nm] = AL(sz, BF16)
            d_["t"] = {nm: Tok() for nm in ("X0", "X1", "N0", "N1", "P0", "P1", "A1", "A2", "KAV", "WU")}
            hb.append(d_)
        PhiT = AL(512)
        Gall = AL(512)
        QT = AL(8 * 128).rearrange("p (h t) -> p h t", h=8)
        Y0s = a512()
        yout = AL(1024)
        tPhi, tG, tQT, tY0, tyo = Tok(), Tok(), Tok(), Tok(), Tok()
        tys = [[Tok() for _ in range(NT)] for _ in range(2)]
        order = [[16, 17] + list(range(16)), [17, 16] + list(range(15, -1, -1))]
        nsteps = NT if self.stop != f"rw1_{li}" else 3
        for step in range(nsteps):
            for d in range(2):
                i = order[d][step]
                mc1, mc2, mn, tri = msk[d]
                tgi = min(i // 4, 4)
                th = self.t_h[tgi]
                cols = slice(i * 128, (i + 1) * 128)
                for q, (dst, tn) in enumerate(((r_s, "r"), (k_s, "k"), (v_s, "v"))):
                    ps, pt = self.PS.next()
                    for kc in range(8):
                        P.pe(I_mm(ps, self.hT3[:, kc, cols], wrkv[q][:, kc, :], start=(kc == 0), stop=(kc == 7)), r=[tw[q], th], w=[pt])
                    P.act(I_acopy(dst, ps), r=[pt], w=[tk_[tn]])
                P.pool(I_copy(vb_, v_s), r=[tk_["v"]], w=[tk_["vb"]])
                for (c0, dst, tn, fn) in ((d * 64, l1, "l1", AF.Tanh), (128 + d * 64, la, "la", AF.Copy)):
                    ps, pt = self.PS.next()
                    for kc in range(8):
                        P.pe(I_mm(ps[0:64, 0:128], lo1[:, kc, c0:c0 + 64], self.hT3[:, kc, cols], start=(kc == 0), stop=(kc == 7)), r=[tlo, th], w=[pt])
                    P.act(I_act(dst[0:64, :], ps[0:64, 0:128], fn), r=[pt], w=[tk_[tn]])
                for (src, tn_s, wi, bi, dst, tn) in ((l1, "l1", d, d, sig, "sig"), (la, "la", 2 + d, 2 + d, a_s, "a")):
                    ps, pt = self.PS.next()
                    P.pe(I_mm(ps, src[0:64, :], lo2[0:64, wi, :], start=True, stop=False), r=[tk_[tn_s], tlo], w=[pt])
                    P.pe(I_mm(ps, self.ones_b[0:1, 0:128], brow[0:1, bi * 512:(bi + 1) * 512], start=False, stop=True), r=[tlo, self.t_c], w=[pt])
                    P.act(I_act(dst, ps, AF.Sigmoid), r=[pt], w=[tk_[tn]])
                P.pool(I_tt(kx, k_s, kk_bc, ALU.mult), r=[tk_["k"], tbc], w=[tk_["kx"]])
                P.pool(I_tt(tA, kx, kx, ALU.mult), r=[tk_["kx"]], w=[tk_["tA"]])
                P.dve(I_red(red8[:, 0:8], tA.rearrange("p (h j) -> p h j", h=8)), r=[tk_["tA"]], w=[tred])
                P.dve(I_ts(red8[:, 0:8], red8[:, 0:8], 1e-24, ALU.max), r=[tred], w=[tred])
                self.rsqrt(red8[:, 0:8], red8[:, 0:8], 1.0, 1e-24, [tred], tred)
                P.dve(I_tt(kk.rearrange("p (h j) -> p h j", h=8), kx.rearrange("p (h j) -> p h j", h=8),
                           red8[:, 0:8].unsqueeze(2).to_broadcast([128, 8, 64]), ALU.mult), r=[tk_["kx"], tred], w=[tk_["kk"]])
                P.pool(I_tt(b_s, kk, a_s, ALU.mult), r=[tk_["kk"], tk_["a"]], w=[tk_["b"]])
                P.dve(I_stt(tB, a_s, -1.0, ka_bc, ALU.add, ALU.mult), r=[tk_["a"], tbc], w=[tk_["tB"]])
                P.dve(I_stt(kE, tB, 1.0, k_s, ALU.add, ALU.mult), r=[tk_["tB"], tk_["k"]], w=[tk_["kE"]])
                P.pool(I_tt(tA, r_s, kE, ALU.mult), r=[tk_["r"], tk_["kE"], tred], w=[tk_["tA"]])
                P.pool(I_tt(tA, tA, rk_bc, ALU.mult), r=[tk_["tA"], tbc], w=[tk_["tA"]])
                P.dve(I_red(red8[:, 8:16], tA.rearrange("p (h j) -> p h j", h=8)), r=[tk_["tA"]], w=[tred])
                P.dve(I_tt(yout[:, 512:1024].rearrange("p (h j) -> p h j", h=8), v_s.rearrange("p (h j) -> p h j", h=8),
                           red8[:, 8:16].unsqueeze(2).to_broadcast([128, 8, 64]), ALU.mult), r=[tk_["v"], tred], w=[tyo])
                psc, ptc = self.PS.next()
                psC, ptC = self.PS.next()
                P.pe(I_mm(psc, tri, sig), r=[tmk, tk_["sig"]], w=[ptc])
                P.pe(I_mm(psC, allneg, sig), r=[tmk, tk_["sig"]], w=[ptC])
                P.act(I_acopy(cum_s, psc), r=[ptc], w=[tk_["cum"]])
                P.dve(I_stt(tB, sig, DS, psc, ALU.mult, ALU.add), r=[tk_["sig"], ptc, tk_["kE"]], w=[tk_["tB"]])
                P.act(I_act(e1, tB, AF.Exp), r=[tk_["tB"]], w=[tk_["e1"]])
                P.act(I_act(e2, psc, AF.Exp, scale=-1.0), r=[ptc], w=[tk_["e2"]])
                P.act(I_act(e3, psc, AF.Exp), r=[ptc], w=[tk_["e3"]])
                P.dve(I_tt(tA, psC, cum_s, ALU.subtract), r=[ptC, tk_["cum"], tred], w=[tk_["tA"]])
                P.act(I_act(e4, tA, AF.Exp), r=[tk_["tA"]], w=[tk_["e4"]])
                P.act(I_act(pcb, psC, AF.Exp), r=[ptC], w=[tk_["pc"]])
                P.pool(I_tt(dg[0:64, :].rearrange("p (h j) -> p h j", h=8), pcb[0:64, :].rearrange("p (h j) -> p h j", h=8),
                            self.ident[0:64, 0:64].unsqueeze(1).to_broadcast([64, 8, 64]), ALU.mult), r=[tk_["pc"], self.t_c], w=[tk_["dg"]])
                P.dve(I_tt(kt_, kk, e1, ALU.mult), r=[tk_["kk"], tk_["e1"]], w=[tk_["kt"]])
                P.pool(I_tt(bt_, b_s, e2, ALU.mult), r=[tk_["b"], tk_["e2"]], w=[tk_["bt"]])
                P.dve(I_tt(k2_, kE, e2, ALU.mult), r=[tk_["kE"], tk_["e2"]], w=[tk_["k2"]])
                P.pool(I_tt(rt_, r_s, e3, ALU.mult), r=[tk_["r"], tk_["e3"]], w=[tk_["rt"]])
                P.dve(I_tt(kh_, kE, e4, ALU.mult), r=[tk_["kE"], tk_["e4"]], w=[tk_["kh"]])
                P.pool(I_tt(bh_, b_s, e4, ALU.mult), r=[tk_["b"], tk_["e4"]], w=[tk_["bh"]])
                for g in range(4):
                    ps, pt = self.PS.next()
                    psb = ps.bitcast(BF16)
                    for q, (src, tn) in enumerate(((kt_, "kt"), (rt_, "rt"), (bt_, "bt"), (k2_, "k2"))):
                        P.pe(I_tr(psb[:, q * 128:(q + 1) * 128], src[:, g * 128:(g + 1) * 128], self.ident_b), r=[tk_[tn], self.t_c], w=[pt])
                    P.op("act" if g % 2 == 0 else "dve", (I_acopy if g % 2 == 0 else I_copy)(fT[g].rearrange("p q t -> p (q t)"), psb[:, 0:512]), r=[pt], w=[tfT[g]])
                for h in range(8):
                    g, R_ = h // 2, slice((h % 2) * 64, (h % 2) * 64 + 64)
                    H = hb[h]
                    f = fT[g]
                    ps1, pt1 = self.PS.next()
                    P.pe(I_mm(ps1[:, 0:256], f[R_, 2, :], f[R_, 0:2, :].rearrange("p q t -> p (q t)")), r=[tfT[g]], w=[pt1])
                    P.dve(I_tt(H["A1"], ps1[:, 0:256], mc1, ALU.mult), r=[pt1, tmk], w=[H["t"]["A1"]])
                    ps2, pt2 = self.PS.next()
                    P.pe(I_mm(ps2[:, 0:256], f[R_, 3, :], f[R_, 0:2, :].rearrange("p q t -> p (q t)")), r=[tfT[g]], w=[pt2])
                    P.dve(I_tt(H["A2"], ps2[:, 0:256], mc2, ALU.mult), r=[pt2, tmk], w=[H["t"]["A2"]])
                    ps3, pt3 = self.PS.next()
                    P.pe(I_mm(ps3[:, 0:128], f[R_, 0, :], f[R_, 2, :]), r=[tfT[g]], w=[pt3])
                    P.dve(I_tt(H["N0"], ps3[:, 0:128], mn, ALU.mult), r=[pt3, tmk], w=[H["t"]["N0"]])
                    P.pool(I_copy(H["X0"], H["A1"][:, 0:128]), r=[H["t"]["A1"]], w=[H["t"]["X0"]])
                    P.pool(I_tt(H["P0"], H["A1"][:, 0:128], self.ident_b, ALU.add), r=[H["t"]["A1"], self.t_c], w=[H["t"]["P0"]])
                cur = 0
                for lvl in range(1, 7):
                    nxt = 1 - cur
                    for h in range(8):
                        H = hb[h]
                        Xc, Nc, Pc = H[f"X{cur}"], H[f"N{cur}"], H[f"P{cur}"]
                        Xn, Nn, Pn = H[f"X{nxt}"], H[f"N{nxt}"], H[f"P{nxt}"]
                        tXc, tNc, tPc = H["t"][f"X{cur}"], H["t"][f"N{cur}"], H["t"][f"P{cur}"]
                        tXn, tNn, tPn = H["t"][f"X{nxt}"], H["t"][f"N{nxt}"], H["t"][f"P{nxt}"]
                        psN, ptN = self.PS.next()
                        P.pe(I_mm(psN[:, 0:128], Xc, Nc), r=[tXc, tNc], w=[ptN])
                        P.act(I_acopy(Nn, psN[:, 0:128]), r=[ptN], w=[tNn])
                        if lvl < 6:
                            psX, ptX = self.PS.next()
                            P.pe(I_mm(psX[:, 0:128], Nc, Xc), r=[tXc, tNc], w=[ptX])
                            P.dve(I_copy(Xn, psX[:, 0:128]), r=[ptX], w=[tXn])
                        psP, ptP = self.PS.next()
                        P.pe(I_mm(psP[:, 0:128], Nn, Pc), r=[tNn, tPc], w=[ptP])
                        P.dve(I_tt(Pn, psP[:, 0:128], Pc, ALU.add), r=[ptP, tPc], w=[tPn])
                    cur = nxt
                psY0, ptY0 = self.PSacc.next()
                psPhi, ptPhi = self.PSacc.next()
                psG, ptG = self.PSacc.next()
                for h in range(8):
                    g, R_ = h // 2, slice((h % 2) * 64, (h % 2) * 64 + 64)
                    hc = slice(h * 64, (h + 1) * 64)
                    H = hb[h]
                    TT, tTT = H[f"P{cur}"], H["t"][f"P{cur}"]
                    P.pool(I_copy(H["KAV"][:, 0:64], kt_[:, hc]), r=[tk_["kt"]], w=[H["t"]["KAV"]])
                    ps, pt = self.PS.next()
                    P.pe(I_mm(ps[:, 0:64], H["A2"][:, 0:128], vb_[:, hc]), r=[H["t"]["A2"], tk_["vb"]], w=[pt])
                    P.act(I_acopy(H["KAV"][:, 64:128], ps[:, 0:64]), r=[pt], w=[H["t"]["KAV"]])
                    ps, pt = self.PS.next()
                    P.pe(I_mm(ps[:, 0:128], TT, H["KAV"]), r=[tTT, H["t"]["KAV"]], w=[pt])
                    P.dve(I_ts(H["WU"], ps[:, 0:128], -1.0, ALU.mult), r=[pt], w=[H["t"]["WU"]])
                    tWU = H["t"]["WU"]
                    ps, pt = self.PS.next()
                    P.pe(I_mm(ps[0:64, 0:128], H["WU"][:, 0:64], H["A1"][:, 128:256], start=True, stop=False), r=[tWU, H["t"]["A1"]], w=[pt])
                    P.pe(I_mm(ps[0:64, 0:128], rt_[:, hc], self.ident_b, start=False, stop=True), r=[tk_["rt"], self.t_c], w=[pt])
                    P.act(I_acopy(QT[0:64, h, :], ps[0:64, 0:128]), r=[pt], w=[tQT])
                    P.pe(I_mm(psY0[:, hc], H["A1"][:, 128:256], H["WU"][:, 64:128], start=True, stop=False), r=[tWU, H["t"]["A1"]], w=[ptY0])
                    P.pe(I_mm(psY0[:, hc], H["A2"][:, 128:256], vb_[:, hc], start=False, stop=True), r=[H["t"]["A2"], tk_["vb"]], w=[ptY0])
                    P.pe(I_mm(psPhi[0:64, hc], H["WU"][:, 0:64], bh_[:, hc]), r=[tWU, tk_["bh"]], w=[ptPhi])
                    P.pe(I_mm(psG[0:64, hc], bh_[:, hc], H["WU"][:, 64:128], start=True, stop=False), r=[tWU, tk_["bh"]], w=[ptG])
                    P.pe(I_mm(psG[0:64, hc], kh_[:, hc], vb_[:, hc], start=False, stop=True), r=[tk_["kh"], tk_["vb"]], w=[ptG])
                P.dve(I_copy(Y0s, psY0), r=[ptY0], w=[tY0])
                P.dve(I_tt(PhiT[0:64, :], psPhi[0:64, :], dg[0:64, :], ALU.add), r=[ptPhi, tk_["dg"]], w=[tPhi])
                P.act(I_acopy(Gall[0:64, :], psG[0:64, :]), r=[ptG], w=[tG])
                pc_, pn_ = mpar[d], 1 - mpar[d]
                Mc, Mn = Mst[d][pc_], Mst[d][pn_]
                tMc, tMn = tM[d][pc_], tM[d][pn_]
                psY, ptY = self.PSacc.next()
                for h in range(8):
                    hc = slice(h * 64, (h + 1) * 64)
                    P.pe(I_mm(psY[:, hc], QT[0:64, h, :], Mc[0:64, hc]), r=[tQT, tMc], w=[ptY])
                P.dve(I_tt(yout[:, 0:512], psY, Y0s, ALU.add), r=[ptY, tY0], w=[tyo])
                psM, ptM = self.PS.next()
                for h in range(8):
                    hc = slice(h * 64, (h + 1) * 64)
                    P.pe(I_mm(psM[0:64, hc], PhiT[0:64, hc], Mc[0:64, hc]), r=[tPhi, tMc], w=[ptM])
                P.dve(I_tt(Mn[0:64, :], psM[0:64, :], Gall[0:64, :], ALU.add), r=[ptM, tG], w=[tMn])
                mpar[d] = pn_
                P.dma("sp", I_dma(self.yscr_d[d, i], yout), r=[tyo], w=[tys[d][i]])
        P.barrier()
        B.off = 0
        ya_r = Ring([AL(1024) for _ in range(2)])
        yb_r = Ring([AL(1024) for _ in range(2)])
        gs1 = AL(128, BF16)
        gs2 = AL(128, BF16)
        tgs = Tok()
        stg_r = Ring([AL(512, BF16) for _ in range(2)])
        yc = a512()
        tyc = Tok()
        for i in range(NT):
            if nsteps < NT and i not in (16, 17):
                continue
            cols = slice(i * 128, (i + 1) * 128)
            th = self.t_h[min(i // 4, 4)]
            ya, tya = ya_r.next()
            yb, tyb = yb_r.next()
            P.dma("sp", I_dma(ya, self.yscr_d[0, i]), r=[tys[0][i]], w=[tya])
            P.dma("act", I_dma(yb, self.yscr_d[1, i]), r=[tys[1][i]], w=[tyb])
            P.pool(I_tt(ya, ya, yb, ALU.add), r=[tya, tyb], w=[tya])
            y3 = ya[:, 0:512].rearrange("p (h j) -> p h j", h=8)
            P.dve(I_red(red8[:, 0:8], y3), r=[tya], w=[tred])
            P.dve(I_ts(red8[:, 0:8], red8[:, 0:8], 1.0 / 64, ALU.mult), r=[tred], w=[tred])
            P.dve(I_tt(yc.rearrange("p (h j) -> p h j", h=8), y3, red8[:, 0:8].unsqueeze(2).to_broadcast([128, 8, 64]), ALU.subtract),
                  r=[tya, tred], w=[tyc])
            P.pool(I_tt(yb[:, 0:512], yc, yc, ALU.mult), r=[tyc], w=[tyb])
            P.dve(I_red(red8[:, 8:16], yb[:, 0:512].rearrange("p (h j) -> p h j", h=8)), r=[tyb], w=[tred])
            self.rsqrt(red8[:, 8:16], red8[:, 8:16], 1.0 / 64, 64e-5, [tred], tred)
            P.dve(I_tt(yc.rearrange("p (h j) -> p h j", h=8), yc.rearrange("p (h j) -> p h j", h=8),
                       red8[:, 8:16].unsqueeze(2).to_broadcast([128, 8, 64]), ALU.mult), r=[tyc, tred], w=[tyc])
            P.pool(I_tt(yc, yc, lg_bc, ALU.mult), r=[tyc, tbc], w=[tyc])
            P.pool(I_tt(yc, yc, lb_bc, ALU.add), r=[tyc, tbc], w=[tyc])
            P.pool(I_tt(yc, yc, ya[:, 512:1024], ALU.add), r=[tyc, tya], w=[tyc])
            ps, pt = self.PS.next()
            for kc in range(8):
                P.pe(I_mm(ps[:, 0:128], lo1[:, kc, 256:384], self.hT3[:, kc, cols], start=(kc == 0), stop=(kc == 7)), r=[tlo, th], w=[pt])
            P.act(I_act(gs1, ps[:, 0:128], AF.Sigmoid), r=[pt], w=[tgs])
            ps, pt = self.PS.next()
            for kc in range(8):
                P.pe(I_mm(ps[0:32, 0:128], lo1[:, kc, 384:416], self.hT3[:, kc, cols], start=(kc == 0), stop=(kc == 7)), r=[tlo, th], w=[pt])
            P.act(I_act(gs2[0:32, :], ps[0:32, 0:128], AF.Sigmoid), r=[pt], w=[tgs])
            ps, pt = self.PS.next()
            P.pe(I_mm(ps, gs1, lo2[:, 4, :], start=True, stop=False), r=[tgs, tlo], w=[pt])
            P.pe(I_mm(ps, gs2[0:32, :], lo2[0:32, 5, :], start=False, stop=True), r=[tgs, tlo], w=[pt])
            P.dve(I_tt(yc, yc, ps, ALU.mult), r=[tyc, pt], w=[tyc])
            ps, pt = self.PS.next()
            for g in range(4):
                P.pe(I_tr(ps[:, g * 128:(g + 1) * 128], yc[:, g * 128:(g + 1) * 128], self.ident), r=[tyc, self.t_c], w=[pt])
            stg, tst = stg_r.next()
            P.act(I_acopy(stg, ps), r=[pt], w=[tst])
            P.dma("sp", I_dma(self.brT_d[2, :, :, cols].rearrange("k p t -> p k t"), stg.rearrange("p (k t) -> p k t", k=4)),
                  r=[tst], w=[self.t_br[2]])
        P.barrier()
        A.release(m)

    def moe_router_setup(self, li):
        P, A = self.P, self.A
        self.wr = A.alloc(8 * 32).rearrange("p (k c) -> p k c", k=8)
        self.t_wr = Tok()
        P.dma("sp", I_dma(self.wr, self.W["router_w"][li].rearrange("(k p) c -> p k c", p=128)), w=[self.t_wr])
        self.rbrow = A.alloc(32)
        P.dma("sp", I_dma(self.rbrow[0:1, :], self.W["router_b"][li:li + 1, :]), w=[self.t_wr])
        self.GT = A.alloc(T)
        self.t_GT = Tok()
        self.rt_r = Ring([A.alloc(128) for _ in range(2)])

    def router_cb(self, tg, s, n, h2f3, th2f):
        P = self.P
        for j in range(n // 128):
            c0 = s + j * 128
            ps, pt = self.PS.next()
            for kc in range(8):
                P.pe(I_mm(ps[:, 0:32], h2f3[:, kc, j * 128:(j + 1) * 128], self.wr[:, kc, :], start=(kc == 0), stop=False),
                     r=[th2f, self.t_wr], w=[pt])
            P.pe(I_mm(ps[:, 0:32], self.ones[0:1, 0:128], self.rbrow[0:1, 0:32], start=False, stop=True), r=[self.t_wr, self.t_c], w=[pt])
            wk, twk = self.rt_r.next()
            lg, t8, msk, e_, sm = wk[:, 0:32], wk[:, 32:40], wk[:, 40:72], wk[:, 72:104], wk[:, 104:108]
            P.dve(I_copy(lg, ps[:, 0:32]), r=[pt], w=[twk])
            P.dve(lambda e, o=t8, i=lg: e.max(out=o, in_=i), r=[twk], w=[twk])
            P.dve(I_ts(msk, lg, t8[:, 3:4], ALU.is_ge), r=[twk], w=[twk])
            P.dve(I_ts(sm[:, 0:1], t8[:, 0:1], -1.0, ALU.mult), r=[twk], w=[twk])
            P.act(I_act(e_, lg, AF.Exp, bias=sm[:, 0:1]), r=[twk], w=[twk])
            P.dve(I_tt(e_, e_, msk, ALU.mult), r=[twk], w=[twk])
            P.dve(I_red(sm[:, 1:2], e_), r=[twk], w=[twk])
            P.dve(lambda e, o=sm[:, 2:3], i=sm[:, 1:2]: e.reciprocal(out=o, in_=i), r=[twk], w=[twk])
            P.dve(I_ts(e_, e_, sm[:, 2:3], ALU.mult), r=[twk], w=[twk])
            ps2, pt2 = self.PS.next()
            P.pe(I_tr(ps2[0:32, 0:128], e_, self.ident), r=[twk, self.t_c], w=[pt2])
            P.dve(I_copy(self.GT[0:32, c0:c0 + 128], ps2[0:32, 0:128]), r=[pt2], w=[self.t_GT])

    def moe_phase(self, li):
        P, A = self.P, self.A
        m = A.mark()
        b1T = A.alloc(16 * 32).rearrange("p (c e) -> p c e", c=16)
        tb1T = Tok()
        m2 = A.mark()
        self.moe_router_setup(li)
        self.norm_phase(1, li, router=self.router_cb)
        self.dbg(f"GT{li}", self.GT[0:32, :], self.t_GT, [32, T])
        tgt = Tok()
        P.dma("sp", I_dma(self.gt_d, self.GT[0:32, :]), r=[self.t_GT], w=[tgt])
        b1s = A.alloc(2048)
        tb1 = Tok()
        P.dma("act", I_dma(b1s[0:32, :], self.W["exp_b1"][li]), w=[tb1])
        for c in range(16):
            ps, pt = self.PS.next()
            P.pe(I_tr(ps[:, 0:32], b1s[0:32, c * 128:(c + 1) * 128], self.ident[0:32, 0:32]), r=[tb1, self.t_c], w=[pt])
            P.dve(I_copy(b1T[:, c, :], ps[:, 0:32]), r=[pt], w=[tb1T])
        b2s = A.alloc(1024)
        tb2 = Tok()
        P.dma("act", I_dma(b2s[0:32, :], self.W["exp_b2"][li]), w=[tb2])
        for tg, (s, n) in enumerate(TGS):
            col = 0 if tg < 4 else 1
            for cc in range(8):
                ps, pt = self.PS.next()
                P.pe(I_mm(ps[:, 0:n], b2s[0:32, cc * 128:(cc + 1) * 128], self.GT[0:32, s:s + n]), r=[tb2, self.t_GT], w=[pt])
                P.dve(I_stt(self.xT3[:, cc, s:s + n], ps[:, 0:n], self.eff4[:, 5, cc, col:col + 1], self.xT3[:, cc, s:s + n], ALU.mult, ALU.add),
                      r=[pt, self.t_eff, self.t_x[tg]], w=[self.t_x[tg]])
        P.barrier()
        A.release(m2)
        w1_r = Ring([A.alloc(8 * 256, BF16) for _ in range(4)])
        w2_r = Ring([A.alloc(8 * 128, BF16) for _ in range(4)])
        gate_r = Ring([A.alloc(T, BF16) for _ in range(2)])
        aT3 = A.alloc(8 * T, BF16).rearrange("p (c t) -> p c t", c=8)
        taT = toks(5)
        tmp_r = Ring([A.alloc(512) for _ in range(2)])
        g_r = Ring([A.alloc(512) for _ in range(3)])
        s_r = Ring([A.alloc(512, BF16) for _ in range(2)])
        l_r = Ring([A.alloc(512, BF16) for _ in range(2)])
        a_r = Ring([A.alloc(512, BF16) for _ in range(2)])
        W1, W2 = self.W["exp_w1"][li], self.W["exp_w2"][li]
        n_exp = 32 if self.stop != f"moe1_{li}" else self.n_exp_dbg
        loads, comps = [], []
        bufmap = {}

        def mk_gate(e):
            def f():
                gate, tgate = gate_r.next()
                P.dma("pool", I_dma(gate, self.gt_d[e:e + 1, :].to_broadcast([128, T])), r=[tgt], w=[tgate])
                bufmap[("g", e)] = (gate, tgate)
            return f

        def mk_w1(e, c):
            def f():
                wp, twp = w1_r.next()
                wp3 = wp.rearrange("p (k c) -> p k c", k=8)
                P.dma("pool", I_dma(wp3[:, :, 0:128], W1[e][:, c * 128:(c + 1) * 128].rearrange("(k p) c -> p k c", p=128)), w=[twp])
                P.dma("pool", I_dma(wp3[:, :, 128:256], W1[e][:, 1024 + c * 128:1024 + (c + 1) * 128].rearrange("(k p) c -> p k c", p=128)), w=[twp])
                bufmap[("w1", e, c)] = (wp3, twp)
            return f

        def mk_w2(e, cc):
            def f():
                w2p, tw2 = w2_r.next()
                w2p3 = w2p.rearrange("p (k c) -> p k c", k=8)
                P.dma("pool", I_dma(w2p3, W2[e][:, cc * 128:(cc + 1) * 128].rearrange("(k p) c -> p k c", p=128)), w=[tw2])
                bufmap[("w2", e, cc)] = (w2p3, tw2)
            return f

        def mk_hid(e, c):
            def f():
                wp3, twp = bufmap[("w1", e, c)]
                for tg, (s, n) in enumerate(TGS):
                    psg, ptg = self.PS.next()
                    psl, ptl = self.PS.next()
                    for kc in range(8):
                        P.pe(I_mm(psg[:, 0:n], wp3[:, kc, 0:128], self.hT3[:, kc, s:s + n], start=(kc == 0), stop=(kc == 7)),
                             r=[twp, self.t_h[tg]], w=[ptg])
                    for kc in range(8):
                        P.pe(I_mm(psl[:, 0:n], wp3[:, kc, 128:256], self.hT3[:, kc, s:s + n], start=(kc == 0), stop=(kc == 7)),
                             r=[twp, self.t_h[tg]], w=[ptl])
                    g_, tg_ = g_r.next()
                    s_, ts_ = s_r.next()
                    l_, tl_ = l_r.next()
                    a_, ta_ = a_r.next()
                    P.dve(I_ts(g_[:, 0:n], psg[:, 0:n], b1T[:, c, e:e + 1], ALU.add, 7.0, ALU.min), r=[ptg, tb1T], w=[tg_])
                    P.act(I_act(s_[:, 0:n], g_[:, 0:n], AF.Sigmoid, scale=1.702), r=[tg_], w=[ts_])
                    P.act(I_act(l_[:, 0:n], psl[:, 0:n], AF.Identity, bias=b1T[:, 8 + c, e:e + 1]), r=[ptl, tb1T], w=[tl_])
                    P.dve(I_ts(l_[:, 0:n], l_[:, 0:n], 7.0, ALU.min, -7.0, ALU.max), r=[tl_], w=[tl_])
                    P.dve(I_tt(a_[:, 0:n], g_[:, 0:n], s_[:, 0:n], ALU.mult), r=[tg_, ts_], w=[ta_])
                    P.dve(I_stt(aT3[:, c, s:s + n], l_[:, 0:n], 1.0, a_[:, 0:n], ALU.add, ALU.mult), r=[ta_, tl_], w=[taT[tg]])
            return f

        def mk_y(e, cc):
            def f():
                gate, tgate = bufmap[("g", e)]
                w2p3, tw2 = bufmap[("w2", e, cc)]
                for tg, (s, n) in enumerate(TGS):
                    col = 0 if tg < 4 else 1
                    ps, pt = self.PS.next()
                    for c in range(8):
                        P.pe(I_mm(ps[:, 0:n], w2p3[:, c, :], aT3[:, c, s:s + n], start=(c == 0), stop=(c == 7)), r=[tw2, taT[tg]], w=[pt])
                    tmp, ttmp = tmp_r.next()
                    P.dve(I_tt(tmp[:, 0:n], ps[:, 0:n], gate[:, s:s + n], ALU.mult), r=[pt, tgate], w=[ttmp])
                    P.dve(I_stt(self.xT3[:, cc, s:s + n], tmp[:, 0:n], self.eff4[:, 5, cc, col:col + 1], self.xT3[:, cc, s:s + n], ALU.mult, ALU.add),
                          r=[ttmp, self.t_eff, self.t_x[tg]], w=[self.t_x[tg]])
            return f

        for e in range(n_exp):
            loads.append(mk_gate(e))
            for c in range(8):
                loads.append(mk_w1(e, c))
                comps.append((len(loads) - 1, mk_hid(e, c)))
            for cc in range(8):
                loads.append(mk_w2(e, cc))
                comps.append((len(loads) - 1, mk_y(e, cc)))
        LOOK = 3
        ptr = 0
        for need, f in comps:
            tgt_ptr = min(len(loads), need + 1 + LOOK)
            while ptr < tgt_ptr:
                loads[ptr]()
                ptr += 1
            f()
        P.barrier()
        A.release(m)

    def build(self):
        P = self.P
        self.setup()
        self.load_x()
        for li in range(self.n_layers):
            self.mod_phase(li)
            self.norm_phase(0, li)
            self.dbg(f"hT{li}", self.hT, self.t_h, [128, 8 * T], BF16)
            self.dbg(f"eff{li}", self.eff, self.t_eff, [128, 96])
            if self.stop == f"norm1_{li}":
                break
            self.spill_x()
            if "attn" not in self.skip:
                self.attn_phase(li)
            if self.stop in (f"attn_{li}", f"attnproj_{li}"):
                break
            if "conv" not in self.skip:
                self.conv_phase(li)
            if self.stop == f"conv_{li}":
                break
            if "rw" not in self.skip:
                self.rwkv_phase(li)
            if self.stop in (f"rw_{li}", f"rw1_{li}"):
                break
            self.restore_x()
            self.merge_phase(li)
            self.dbg(f"xmid{li}", self.xT, self.t_x, [128, 8 * T])
            if self.stop == f"merge_{li}":
                break
            self.moe_phase(li)
            self.dbg(f"xout{li}", self.xT, self.t_x, [128, 8 * T])
            if self.stop in (f"moe_{li}", f"moe1_{li}"):
                break
        if self.stop is None:
            self.final_phase()
        if self.dbg_br:
            t = Tok()
            d = self.nc.dram_tensor("dbg_brT", [3, 4, 128, T], BF16, kind="ExternalOutput").ap()
            P.dma("sp", I_dma(d, self.brT_d), r=self.t_br, w=[t])
            self.out_toks.append(t)
            self.dbg_out["brT"] = "dbg_brT"
        P.op("sp", None, r=self.out_toks)
        P.emit(self.nc, self.st)
        self.st.close()
        return self.nc


_CONSTS = None


def make_in_maps(inputs):
    global _CONSTS
    if _CONSTS is None:
        _CONSTS = make_consts()
    shared = {n: np.ascontiguousarray(np.asarray(inputs[n], dtype=np.float32)) for n, _ in WEIGHT_SPECS}
    for n in CONST_NAMES:
        shared["k_" + n] = _CONSTS[n]
    x = np.asarray(inputs["x"], dtype=np.float32)
    ctx = np.asarray(inputs["ctx"], dtype=np.float32)
    c = np.asarray(inputs["c"], dtype=np.float32)
    c_ctx = np.asarray(inputs["c_ctx"], dtype=np.float32)
    maps = []
    for b in range(8):
        m = dict(shared)
        m["x"] = np.ascontiguousarray(x[b])
        m["ctx"] = np.ascontiguousarray(ctx[b])
        m["c2"] = np.ascontiguousarray(np.stack([c[b], c_ctx], 0))
        maps.append(m)
    return maps


def kernel(**inputs):
    mk = MK()
    nc = mk.build()
    maps = make_in_maps(inputs)
    res = run_bass_kernel_spmd(nc, maps, core_ids=list(range(8)))
    return np.stack([np.asarray(res.results[b]["out"], dtype=np.float32) for b in range(8)], 0)
```

```python
import math
import numpy as np
from contextlib import ExitStack
import concourse.bass as bass
import concourse.mybir as mybir
from concourse.bass_utils import run_bass_kernel_spmd

F32 = mybir.dt.float32
BF16 = mybir.dt.bfloat16
AF = mybir.ActivationFunctionType
ALU = mybir.AluOpType
AX = mybir.AxisListType

D = 1024
TL = 2048
TC = 256
T = TL + TC
NT = T // 128
DEPTH = 2
IN_COLS = 7680
TGS = [(0, 512), (512, 512), (1024, 512), (1536, 512), (2048, 256)]

ENGS = ("pe", "act", "dve", "pool", "sp")
NDMASEM = 24


class Tok:
    __slots__ = ("w", "rs", "name")

    def __init__(self, name=""):
        self.w = None
        self.rs = []
        self.name = name


def toks(n):
    return [Tok() for _ in range(n)]


class Ev:
    __slots__ = ("kind", "eng", "idx", "sem", "target", "op")

    def __init__(self, kind, eng, idx, sem=None, target=None, op=None):
        self.kind, self.eng, self.idx, self.sem, self.target, self.op = kind, eng, idx, sem, target, op


class Op:
    __slots__ = ("eng", "fn", "dma", "idx", "waits", "flag", "tick", "dsem", "dtarget", "prewait")


class Prog:
    def __init__(self):
        self.ops = {e: [] for e in ENGS}
        self.ndma = {e: 0 for e in ENGS}
        self.seen = {e: {} for e in ENGS}
        self.last = {e: None for e in ENGS}
        self.pend_dma = {}

    def op(self, eng, fn, r=(), w=(), dma=False, extra=()):
        o = Op()
        o.eng, o.fn, o.dma = eng, fn, dma
        o.idx = len(self.ops[eng])
        o.flag = False
        o.tick = None
        o.prewait = None
        waits = {}

        def need(ev, force=False):
            if ev is None:
                return
            if ev.kind == "dma":
                k = ("dma", ev.eng, ev.sem)
                if waits.get(k, (0, None))[0] < ev.target:
                    waits[k] = (ev.target, ev)
            else:
                if ev.eng == eng and not dma and not force:
                    if eng == "pe":
                        return
                    if o.idx - ev.idx > 2:
                        return
                k = ("eng", ev.eng)
                if waits.get(k, (-1, None))[0] < ev.idx:
                    waits[k] = (ev.idx, ev)

        for t in r:
            need(t.w)
        for t in w:
            need(t.w)
            for ev in t.rs:
                need(ev)
        for ev in extra:
            if not (ev.kind == "eng" and ev.eng == eng):
                need(ev, force=True)
        seen = self.seen[eng]
        fw = {}
        for k, (v, ev) in waits.items():
            if seen.get(k, -1) >= v:
                continue
            seen[k] = v
            fw[k] = (v, ev)
            if ev.kind == "eng":
                ev.op.flag = True
        o.waits = fw
        if dma:
            n = self.ndma[eng]
            self.ndma[eng] = n + 1
            o.dsem = n % NDMASEM
            o.dtarget = 16 * (n // NDMASEM + 1)
            if n >= NDMASEM:
                o.prewait = (o.dsem, o.dtarget - 16)
            ev = Ev("dma", eng, o.idx, o.dsem, o.dtarget, o)
            self.pend_dma[(eng, o.dsem)] = ev
        else:
            ev = Ev("eng", eng, o.idx, op=o)
            if fn is not None:
                self.last[eng] = ev
        for t in r:
            t.rs.append(ev)
        for t in w:
            t.w = ev
            t.rs = []
        self.ops[eng].append(o)
        return o

    def pe(self, fn, r=(), w=()):
        return self.op("pe", fn, r, w)

    def act(self, fn, r=(), w=()):
        return self.op("act", fn, r, w)

    def dve(self, fn, r=(), w=()):
        return self.op("dve", fn, r, w)

    def pool(self, fn, r=(), w=()):
        return self.op("pool", fn, r, w)

    def dma(self, q, fn, r=(), w=()):
        return self.op(q, fn, r, w, dma=True)

    def barrier(self):
        evs = [ev for ev in self.last.values() if ev is not None] + list(self.pend_dma.values())
        self.pend_dma = {}
        for e in ENGS:
            self.op(e, None, extra=evs)

    def emit(self, nc, stack):
        for e in ENGS:
            t = 0
            for o in self.ops[e]:
                if o.flag and not o.dma:
                    t += 1
                    o.tick = t
        esem = {e: stack.enter_context(nc.semaphore(f"s_{e}")) for e in ENGS}
        dsem = {e: [stack.enter_context(nc.semaphore(f"d_{e}{i}")) for i in range(NDMASEM)]
                for e in ENGS if self.ndma[e] > 0}
        block = stack.enter_context(nc.Block())
        prog = self

        def runner(ename):
            def f(eng):
                for o in prog.ops[ename]:
                    for k, (v, ev) in o.waits.items():
                        if k[0] == "dma":
                            eng.wait_ge(dsem[k[1]][k[2]], v)
                        else:
                            eng.wait_ge(esem[k[1]], ev.op.tick)
                    if o.dma and o.prewait is not None:
                        eng.wait_ge(dsem[ename][o.prewait[0]], o.prewait[1])
                    if o.fn is None:
                        continue
                    ins = o.fn(eng)
                    if o.dma:
                        ins.then_inc(dsem[ename][o.dsem], 16)
                    elif o.flag:
                        ins.then_inc(esem[ename], 1)
            return f

        block.sync(runner("sp"))
        block.tensor(runner("pe"))
        block.scalar(runner("act"))
        block.vector(runner("dve"))
        block.gpsimd(runner("pool"))


class Arena:
    def __init__(self, ap, n):
        self.ap, self.n, self.off = ap, n, 0

    def alloc(self, n, dt=F32):
        nf = n if dt == F32 else (n + 1) // 2
        nf = (nf + 7) // 8 * 8
        assert self.off + nf <= self.n, ("arena overflow", self.off, nf, self.n)
        a = self.ap[:, self.off:self.off + nf]
        self.off += nf
        if dt != F32:
            a = a.bitcast(dt)[:, 0:n]
        else:
            a = a[:, 0:n]
        return a

    def mark(self):
        return self.off

    def release(self, m):
        self.off = m


class Ring:
    def __init__(self, bufs):
        self.bufs = bufs
        self.toks = toks(len(bufs))
        self.i = 0

    def next(self):
        j = self.i % len(self.bufs)
        self.i += 1
        return self.bufs[j], self.toks[j]


def make_consts():
    c = {}
    c["ident"] = np.eye(128, dtype=np.float32)
    c["ones"] = np.ones((128, 128), dtype=np.float32)
    pos_r = (np.arange(TL) // 64).astype(np.float32)
    pos_c = (np.arange(TL) % 64).astype(np.float32)
    inv = np.power(10000.0, -np.arange(16, dtype=np.float32) / 16).astype(np.float32)
    cosT = np.zeros((128, TL), np.float32)
    sinT = np.zeros((128, TL), np.float32)
    pm = np.zeros((128, 128), np.float32)
    for p in range(128):
        d = p % 64
        pos = pos_r if d < 32 else pos_c
        ang = pos * inv[d % 16]
        cosT[p] = np.cos(ang)
        sinT[p] = np.sin(ang)
        if (d % 32) < 16:
            pm[p + 16, p] = -1.0
        else:
            pm[p - 16, p] = 1.0
    c["cosT"], c["sinT"], c["pm"] = cosT, sinT, pm
    s = np.arange(128)[:, None]
    t = np.arange(128)[None, :]
    c["m_f_strict"] = (s < t).astype(np.float32)
    c["m_f_incl"] = (s <= t).astype(np.float32)
    c["m_r_strict"] = (s > t).astype(np.float32)
    c["m_r_incl"] = (s >= t).astype(np.float32)
    bd = np.zeros((128, 128), np.float32)
    bd[:64, :64] = 1
    bd[64:, 64:] = 1
    c["blockdiag"] = bd
    return c


CONST_NAMES = ["ident", "ones", "cosT", "sinT", "pm", "m_f_strict", "m_f_incl", "m_r_strict", "m_r_incl", "blockdiag"]

WEIGHT_SPECS = [
    ("w_mod", [DEPTH, D, 6 * D]), ("b_mod", [DEPTH, 6 * D]), ("norm1_g", [DEPTH, D]), ("norm2_g", [DEPTH, D]),
    ("w_in", [DEPTH, D, IN_COLS]), ("da_lambda", [DEPTH, 4, 64]), ("da_subln_g", [DEPTH, 128]),
    ("conv_w", [DEPTH, 3, 512]), ("rw_w0", [DEPTH, 2, 512]), ("rw_w1", [DEPTH, 2, D, 64]),
    ("rw_w2", [DEPTH, 2, 64, 512]), ("rw_a0", [DEPTH, 2, 512]), ("rw_a1", [DEPTH, 2, D, 64]),
    ("rw_a2", [DEPTH, 2, 64, 512]), ("rw_g1", [DEPTH, D, 160]), ("rw_g2", [DEPTH, 160, 512]),
    ("rw_k_k", [DEPTH, 512]), ("rw_k_a", [DEPTH, 512]), ("rw_r_k", [DEPTH, 8, 64]),
    ("rw_lnx_g", [DEPTH, 512]), ("rw_lnx_b", [DEPTH, 512]), ("w_branch", [DEPTH, 3, 512, D]),
    ("w_out", [DEPTH, D, D]), ("router_w", [DEPTH, D, 32]), ("router_b", [DEPTH, 32]),
    ("exp_w1", [DEPTH, 32, D, 2 * D]), ("exp_b1", [DEPTH, 32, 2 * D]), ("exp_w2", [DEPTH, 32, D, D]),
    ("exp_b2", [DEPTH, 32, D]), ("final_g", [D]),
]


def I_copy(o, i):
    return lambda e: e.tensor_copy(out=o, in_=i)


def I_acopy(o, i):
    return lambda e: e.activation(out=o, in_=i, func=AF.Copy)


def I_act(o, i, func, bias=None, scale=None):
    kw = {}
    if bias is not None:
        kw["bias"] = bias
    if scale is not None:
        kw["scale"] = scale
    return lambda e: e.activation(out=o, in_=i, func=func, **kw)


def I_mm(o, l, r, start=True, stop=True):
    return lambda e: e.matmul(o, lhsT=l, rhs=r, start=start, stop=stop)


def I_tr(o, i, ident):
    return lambda e: e.transpose(out=o, in_=i, identity=ident)


def I_tt(o, a, b, op):
    return lambda e: e.tensor_tensor(out=o, in0=a, in1=b, op=op)


def I_ts(o, a, s1, op0, s2=None, op1=None):
    if op1 is None:
        return lambda e: e.tensor_scalar(out=o, in0=a, scalar1=s1, scalar2=None, op0=op0)
    return lambda e: e.tensor_scalar(out=o, in0=a, scalar1=s1, scalar2=s2, op0=op0, op1=op1)


def I_stt(o, in0, scalar, in1, op0, op1):
    return lambda e: e.scalar_tensor_tensor(out=o, in0=in0, scalar=scalar, in1=in1, op0=op0, op1=op1)


def I_red(o, i, op=ALU.add):
    return lambda e: e.tensor_reduce(out=o, in_=i, axis=AX.X, op=op)


def I_dma(o, i):
    return lambda e: e.dma_start(out=o, in_=i)


def I_memset(o, v):
    return lambda e: e.memset(o, v)


ARENA_F32 = 52224


class MK:
    def __init__(self, n_layers=DEPTH, dbg=(), stop=None, skip=(), dbg_br=False):
        self.skip = set(skip)
        self.n_exp_dbg = 1
        self.dbg_br = dbg_br
        self.n_layers = n_layers
        self.dbg_names = set(dbg)
        self.stop = stop
        nc = bass.Bass("TRN2", target_bir_lowering=False)
        self.nc = nc
        self.st = ExitStack()
        self.P = Prog()
        self.x_d = nc.dram_tensor("x", [TL, D], F32, kind="ExternalInput").ap()
        self.ctx_d = nc.dram_tensor("ctx", [TC, D], F32, kind="ExternalInput").ap()
        self.c2_d = nc.dram_tensor("c2", [2, D], F32, kind="ExternalInput").ap()
        self.W = {n: nc.dram_tensor(n, s, F32, kind="ExternalInput").ap() for n, s in WEIGHT_SPECS}
        cs = make_consts()
        self.Cd = {n: nc.dram_tensor("k_" + n, list(cs[n].shape), F32, kind="ExternalInput").ap() for n in CONST_NAMES}
        self.out_d = nc.dram_tensor("out", [TL, D], F32, kind="ExternalOutput").ap()
        self.brT_d = nc.dram_tensor("brT", [3, 4, 128, T], BF16).ap()
        self.yscr_d = nc.dram_tensor("yscr", [2, NT, 128, 1024], F32).ap()
        self.gt_d = nc.dram_tensor("gtscr", [32, T], F32).ap()
        self.dbg_out = {}
        self.out_toks = []
        arena_t = self.st.enter_context(nc.sbuf_tensor("arena", [128, ARENA_F32], F32))
        self.A = Arena(arena_t[:], ARENA_F32)
        banks = [self.st.enter_context(nc.psum_tensor(f"psb{i}", [128, 512], F32))[:] for i in range(8)]
        self.PS = Ring(banks[0:4])
        self.PSacc = Ring(banks[4:8])
        self.PS8 = Ring(banks)
        self.PS8.toks = self.PS.toks + self.PSacc.toks
        self.t_br = toks(3)
        self.xscr_d = nc.dram_tensor("xscr", [128, 8 * T], F32).ap()

    def dbg(self, name, ap, tok, shape, dt=F32):
        if name not in self.dbg_names:
            return
        d = self.nc.dram_tensor("dbg_" + name, list(shape), dt, kind="ExternalOutput").ap()
        t = Tok()
        self.P.dma("sp", I_dma(d, ap), r=tok if isinstance(tok, (list, tuple)) else [tok], w=[t])
        self.out_toks.append(t)
        self.dbg_out[name] = "dbg_" + name

    def load_w_bf16(self, dst3, src2d, tok, kc=8):
        self.P.dma("pool", I_dma(dst3, src2d.rearrange("(k p) c -> p k c", p=128)), w=[tok])

    def rows_to_cols(self, dst, rows, n, tok_rows, tok_dst, scale=None):
        P = self.P
        ps, pt = self.PS.next()
        for j in range(n):
            P.pe(I_mm(ps[:, j:j + 1], rows[0:1, j * 128:(j + 1) * 128], self.ones[0:1, 0:1]), r=[tok_rows, self.t_c], w=[pt])
        P.dve(I_copy(dst, ps[:, 0:n]), r=[pt], w=[tok_dst])

    def bcast_rows(self, dst, row, n, tok_row, tok_dst, scale=None, ones=None):
        P = self.P
        ps, pt = self.PS.next()
        P.pe(I_mm(ps[:, 0:n], (self.ones if ones is None else ones)[0:1, 0:128], row[0:1, 0:n]), r=[tok_row, self.t_c], w=[pt])
        if scale is None:
            P.dve(I_copy(dst, ps[:, 0:n]), r=[pt], w=[tok_dst])
        else:
            P.dve(I_ts(dst, ps[:, 0:n], float(scale), ALU.mult), r=[pt], w=[tok_dst])

    def rsqrt(self, out, in_, mult, eps, r, tok):
        self.P.act(I_act(out, in_, AF.Ln, bias=self.epsc[:, self.eps_idx[eps]:self.eps_idx[eps] + 1][0:out.shape[0]], scale=float(mult)), r=list(r) + [self.t_c], w=[tok])
        self.P.act(I_act(out, out, AF.Exp, scale=-0.5), r=[tok], w=[tok])

    def AL(self, n, dt=F32):
        try:
            return self.B.alloc(n, dt)
        except AssertionError:
            return self.A.alloc(n, dt)

    def spill_x(self):
        P = self.P
        self.t_xs = Tok()
        for k in range(8):
            P.dma("sp" if k % 2 == 0 else "act", I_dma(self.xscr_d[:, k * T:(k + 1) * T], self.xT[:, k * T:(k + 1) * T]), r=self.t_x, w=[self.t_xs])
        P.barrier()
        self.B = Arena(self.xT, 8 * T)

    def restore_x(self):
        P = self.P
        P.barrier()
        for k in range(8):
            P.dma("sp" if k % 2 == 0 else "act", I_dma(self.xT[:, k * T:(k + 1) * T], self.xscr_d[:, k * T:(k + 1) * T]), r=[self.t_xs], w=self.t_x)
        P.barrier()

    def setup(self):
        P, A = self.P, self.A
        self.t_c = Tok()
        self.ident = A.alloc(128)
        self.ones = A.alloc(128)
        self.ident_b = A.alloc(128, BF16)
        self.ones_b = A.alloc(128, BF16)
        P.dma("sp", I_dma(self.ident, self.Cd["ident"]), w=[self.t_c])
        P.dma("sp", I_dma(self.ones, self.Cd["ones"]), w=[self.t_c])
        P.dma("pool", I_dma(self.ident_b, self.Cd["ident"]), w=[self.t_c])
        P.dma("pool", I_dma(self.ones_b, self.Cd["ones"]), w=[self.t_c])
        self.epsc = A.alloc(8)
        self.eps_idx = {1e-6: 0, 1e-5: 1, 64e-5: 2, 1e-24: 3}
        for v, i in self.eps_idx.items():
            P.dve(I_memset(self.epsc[:, i:i + 1], v), w=[self.t_c])
        self.xT = A.alloc(8 * T)
        self.xT3 = self.xT.rearrange("p (k t) -> p k t", k=8)
        self.t_x = toks(5)
        self.hT = A.alloc(8 * T, BF16)
        self.hT3 = self.hT.rearrange("p (k t) -> p k t", k=8)
        self.t_h = toks(5)
        self.csT = A.alloc(16)
        self.t_cs = Tok()
        self.eff = A.alloc(96)
        self.eff4 = self.eff.rearrange("p (n k c) -> p n k c", n=6, k=8)
        self.t_eff = Tok()
        self.gvec = A.alloc(24)
        self.t_gvec = Tok()

    def load_x(self):
        P, A = self.P, self.A
        m = A.mark()
        stage = Ring([A.alloc(1024) for _ in range(2)])
        for i in range(NT):
            sb, stt = stage.next()
            src = self.x_d[i * 128:(i + 1) * 128, :] if i < 16 else self.ctx_d[(i - 16) * 128:(i - 15) * 128, :]
            P.dma("sp", I_dma(sb, src), w=[stt])
            for half in range(2):
                ps, pt = self.PS.next()
                for j in range(4):
                    k = half * 4 + j
                    P.pe(I_tr(ps[:, j * 128:(j + 1) * 128], sb[:, k * 128:(k + 1) * 128], self.ident), r=[stt, self.t_c], w=[pt])
                fn = I_copy(self.xT3[:, half * 4:half * 4 + 4, i * 128:(i + 1) * 128], ps.rearrange("p (a b) -> p a b", a=4))
                P.dve(fn, r=[pt], w=[self.t_x[min(i // 4, 4)]])
        c2s = A.alloc(1024)
        tcs = Tok()
        P.dma("sp", I_dma(c2s[0:2, :], self.c2_d), w=[tcs])
        P.act(I_act(c2s[0:2, :], c2s[0:2, :], AF.Silu), r=[tcs], w=[tcs])
        ps, pt = self.PS.next()
        for k in range(8):
            P.pe(I_tr(ps[:, k * 2:k * 2 + 2], c2s[0:2, k * 128:(k + 1) * 128], self.ident[0:2, 0:2]), r=[tcs, self.t_c], w=[pt])
        P.dve(I_copy(self.csT, ps[:, 0:16]), r=[pt], w=[self.t_cs])
        P.barrier()
        A.release(m)

    def mod_phase(self, li):
        P, A = self.P, self.A
        m = A.mark()
        brow = A.alloc(6 * D)
        grow = A.alloc(3 * D)
        tb, tg = Tok(), Tok()
        P.dma("sp", I_dma(brow[0:1, :], self.W["b_mod"][li:li + 1, :]), w=[tb])
        P.dma("sp", I_dma(grow[0:1, 0:D], self.W["norm1_g"][li:li + 1, :]), w=[tg])
        P.dma("sp", I_dma(grow[0:1, D:2 * D], self.W["norm2_g"][li:li + 1, :]), w=[tg])
        P.dma("sp", I_dma(grow[0:1, 2 * D:3 * D], self.W["final_g"].rearrange("(o d) -> o d", o=1)), w=[tg])
        self.rows_to_cols(self.gvec, grow, 24, tg, self.t_gvec)
        wring = Ring([A.alloc(8 * 512) for _ in range(2)])
        psm, ptm = self.PS.next()
        for piece in range(12):
            wp, wt = wring.next()
            wp3 = wp.rearrange("p (k c) -> p k c", k=8)
            src = self.W["w_mod"][li][:, piece * 512:(piece + 1) * 512].rearrange("(k p) c -> p k c", p=128)
            P.dma("sp" if piece % 2 == 0 else "act", I_dma(wp3, src), w=[wt])
            for jj in range(4):
                j = piece * 4 + jj
                for kc in range(8):
                    P.pe(I_mm(psm[:, j * 2:j * 2 + 2], wp3[:, kc, jj * 128:(jj + 1) * 128], self.csT[:, kc * 2:kc * 2 + 2],
                              start=(kc == 0), stop=False), r=[wt, self.t_cs], w=[ptm])
                P.pe(I_mm(psm[:, j * 2:j * 2 + 2], brow[0:1, j * 128:(j + 1) * 128], self.ones[0:1, 0:2], start=False, stop=True),
                     r=[tb, self.t_c], w=[ptm])
        P.dve(I_copy(self.eff, psm[:, 0:96]), r=[ptm], w=[self.t_eff])
        for n, gi in ((1, 0), (4, 1)):
            P.dve(I_stt(self.eff4[:, n], self.eff4[:, n], 1.0,
                        self.gvec[:, gi * 8:(gi + 1) * 8].unsqueeze(2).to_broadcast([128, 8, 2]), ALU.add, ALU.mult),
                  r=[self.t_eff, self.t_gvec], w=[self.t_eff])
        P.barrier()
        A.release(m)

    def norm_phase(self, which, li, router=None):
        P, A = self.P, self.A
        m = A.mark()
        n_sh, n_sc = (0, 1) if which == 0 else (3, 4)
        sq = A.alloc(8 * 512)
        sq3 = sq.rearrange("p (k t) -> p k t", k=8)
        tsq = Tok()
        rstd_r = Ring([A.alloc(512) for _ in range(2)])
        tmp_r = Ring([A.alloc(512) for _ in range(3)])
        if router is not None:
            h2f = A.alloc(8 * 512)
            h2f3 = h2f.rearrange("p (k t) -> p k t", k=8)
            th2f = Tok()
        for tg, (s, n) in enumerate(TGS):
            col = 0 if tg < 4 else 1
            P.act(I_act(sq3[:, :, 0:n], self.xT3[:, :, s:s + n], AF.Square), r=[self.t_x[tg]], w=[tsq])
            ps, pt = self.PS.next()
            for k in range(8):
                P.pe(I_mm(ps[:, 0:n], self.ones, sq3[:, k, 0:n], start=(k == 0), stop=(k == 7)), r=[tsq, self.t_c], w=[pt])
            rstd, trs = rstd_r.next()
            self.rsqrt(rstd[:, 0:n], ps[:, 0:n], 1.0 / D, 1e-6, [pt], trs)
            for k in range(8):
                tmp, tt = tmp_r.next()
                P.op("dve",
                     I_stt(tmp[:, 0:n], self.xT3[:, k, s:s + n], self.eff4[:, n_sc, k, col:col + 1], rstd[:, 0:n], ALU.mult, ALU.mult),
                     r=[self.t_x[tg], self.t_eff, trs], w=[tt])
                if router is None:
                    P.act(I_act(self.hT3[:, k, s:s + n], tmp[:, 0:n], AF.Identity, bias=self.eff4[:, n_sh, k, col:col + 1]),
                          r=[tt, self.t_eff], w=[self.t_h[tg]])
                else:
                    P.act(I_act(h2f3[:, k, 0:n], tmp[:, 0:n], AF.Identity, bias=self.eff4[:, n_sh, k, col:col + 1]),
                          r=[tt, self.t_eff], w=[th2f])
                    P.pool(I_copy(self.hT3[:, k, s:s + n], h2f3[:, k, 0:n]), r=[th2f], w=[self.t_h[tg]])
            if router is not None:
                router(tg, s, n, h2f3, th2f)
        P.barrier()
        A.release(m)


    def attn_phase(self, li):
        P, A = self.P, self.A
        m = A.mark()
        self.B.off = 0
        lam_init = 0.8 - 0.6 * math.exp(-0.3 * li)
        W = self.W["w_in"][li]
        wq = self.AL(8 * 512, BF16).rearrange("p (k c) -> p k c", k=8)
        wk = self.AL(8 * 512, BF16).rearrange("p (k c) -> p k c", k=8)
        wv = self.AL(8 * 512, BF16).rearrange("p (k c) -> p k c", k=8)
        tw = toks(3)
        self.load_w_bf16(wq, W[:, 0:512], tw[0])
        self.load_w_bf16(wk, W[:, 512:1024], tw[1])
        self.load_w_bf16(wv, W[:, 1024:1536], tw[2])
        cosT = self.AL(TL)
        sinT = self.AL(TL)
        pm = self.AL(128, BF16)
        trope = Tok()
        P.dma("sp", I_dma(cosT, self.Cd["cosT"]), w=[trope])
        P.dma("act", I_dma(sinT, self.Cd["sinT"]), w=[trope])
        P.dma("pool", I_dma(pm, self.Cd["pm"]), w=[trope])
        qT = self.AL(4 * T, BF16).rearrange("p (h t) -> p h t", h=4)
        kT = self.AL(4 * T, BF16).rearrange("p (h t) -> p h t", h=4)
        vaug = self.AL(NT * 4 * 130, BF16).rearrange("p (i h e) -> p i h e", i=NT, h=4)
        t_q, t_k, t_v = toks(5), toks(5), toks(NT)
        tv1 = Tok()
        P.pool(I_memset(vaug[:, :, :, 128:129], 1.0), w=t_v)
        rows = self.AL(512)
        trow = Tok()
        P.dma("sp", I_dma(rows[0:1, 0:256], self.W["da_lambda"][li:li + 1].rearrange("o a b -> o (a b)")), w=[trow])
        P.dma("sp", I_dma(rows[0:1, 256:384], self.W["da_subln_g"][li:li + 1, :]), w=[trow])
        lsm = self.AL(16)
        tl = Tok()
        P.dve(I_tt(rows[0:1, 384:448], rows[0:1, 0:64], rows[0:1, 64:128], ALU.mult), r=[trow], w=[trow])
        P.dve(I_tt(rows[0:1, 448:512], rows[0:1, 128:192], rows[0:1, 192:256], ALU.mult), r=[trow], w=[trow])
        P.dve(I_red(lsm[0:1, 0:2], rows[0:1, 384:512].rearrange("o (a b) -> o a b", a=2)), r=[trow], w=[tl])
        P.act(I_act(lsm[0:1, 2:4], lsm[0:1, 0:2], AF.Exp), r=[tl], w=[tl])
        P.dve(I_tt(lsm[0:1, 4:5], lsm[0:1, 3:4], lsm[0:1, 2:3], ALU.subtract), r=[tl], w=[tl])
        P.dve(I_ts(lsm[0:1, 4:5], lsm[0:1, 4:5], -lam_init, ALU.add), r=[tl], w=[tl])
        neglam = self.AL(8)
        gsub = self.AL(128)
        tng = Tok()
        self.bcast_rows(neglam[:, 0:1], lsm[:, 4:5], 1, tl, tng)
        self.bcast_rows(gsub, rows[:, 256:384], 128, trow, tng, scale=(1.0 - lam_init))
        raw_r = Ring([self.AL(512, BF16) for _ in range(2)])
        t1_r = Ring([self.AL(512) for _ in range(2)])
        t2_r = Ring([self.AL(512) for _ in range(2)])
        for (w3, twk, dstT, tdst) in ((wq, tw[0], qT, t_q), (wk, tw[1], kT, t_k)):
            for h in range(4):
                for tg, (s, n) in enumerate(TGS):
                    ps, pt = self.PS.next()
                    for kc in range(8):
                        P.pe(I_mm(ps[:, 0:n], w3[:, kc, h * 128:(h + 1) * 128], self.hT3[:, kc, s:s + n], start=(kc == 0), stop=(kc == 7)),
                             r=[twk, self.t_h[tg]], w=[pt])
                    if tg == 4:
                        P.act(I_acopy(dstT[:, h, s:s + n], ps[:, 0:n]), r=[pt], w=[tdst[tg]])
                        continue
                    raw, tr = raw_r.next()
                    P.act(I_acopy(raw[:, 0:n], ps[:, 0:n]), r=[pt], w=[tr])
                    ps2, pt2 = self.PS.next()
                    P.pe(I_mm(ps2[:, 0:n], pm, raw[:, 0:n]), r=[trope, tr], w=[pt2])
                    a1, ta1 = t1_r.next()
                    a2, ta2 = t2_r.next()
                    P.pool(I_tt(a1[:, 0:n], raw[:, 0:n], cosT[:, s:s + n], ALU.mult), r=[tr, trope], w=[ta1])
                    P.dve(I_tt(a2[:, 0:n], ps2[:, 0:n], sinT[:, s:s + n], ALU.mult), r=[pt2, trope], w=[ta2])
                    P.pool(I_tt(dstT[:, h, s:s + n], a1[:, 0:n], a2[:, 0:n], ALU.add), r=[ta1, ta2], w=[tdst[tg]])
        for i in range(NT):
            ps, pt = self.PS.next()
            for kc in range(8):
                P.pe(I_mm(ps, self.hT3[:, kc, i * 128:(i + 1) * 128], wv[:, kc, :], start=(kc == 0), stop=(kc == 7)),
                     r=[tw[2], self.t_h[min(i // 4, 4)]], w=[pt])
            P.act(I_acopy(vaug[:, i, :, 0:128], ps.rearrange("p (h e) -> p h e", h=4)), r=[pt], w=[t_v[i]])
        self.dbg("qT", qT.rearrange("p h t -> p (h t)"), t_q, [128, 4 * T], BF16)
        self.dbg("kT", kT.rearrange("p h t -> p (h t)"), t_k, [128, 4 * T], BF16)
        if self.stop == f"attnproj_{li}":
            P.barrier()
            A.release(m)
            return
        Ea_r = Ring([self.AL(NT * 256, BF16) for _ in range(2)])
        osb = [self.AL(4 * 130) for _ in range(2)]
        tosb = toks(2)
        oda = self.AL(4 * 512)
        oda3 = oda.rearrange("p (j c) -> p j c", j=4)
        toda = Tok()
        sm_r = Ring([self.AL(8) for _ in range(2)])
        o1_r = Ring([self.AL(128) for _ in range(2)])
        o2_r = Ring([self.AL(128) for _ in range(2)])
        stg_r = Ring([self.AL(512, BF16) for _ in range(2)])
        groups = [(s, 256, list(range(NT)), s // 512) for s in range(0, TL, 256)] + [(2048, 256, [16, 17], 4)]
        for (qs0, qn, ktiles, tg) in groups:
            nq = qn // 128
            for h in range(4):
                for sm in range(2):
                    R_ = slice(sm * 64, (sm + 1) * 64)
                    Ea, tea = Ea_r.next()
                    Ea3 = Ea.rearrange("p (k q) -> p k q", k=NT)
                    for idx, kt in enumerate(ktiles):
                        ps, pt = self.PS.next()
                        P.pe(I_mm(ps[:, 0:qn], kT[R_, h, kt * 128:(kt + 1) * 128], qT[R_, h, qs0:qs0 + qn]),
                             r=[t_k[min(kt // 4, 4)], t_q[tg]], w=[pt])
                        P.act(I_act(Ea3[:, idx, 0:qn], ps[:, 0:qn], AF.Exp, scale=0.125), r=[pt], w=[tea])
                    for j in range(nq):
                        ob, tob = self.PSacc.next()
                        for idx, kt in enumerate(ktiles):
                            P.pe(I_mm(ob[:, 0:129], Ea3[:, idx, j * 128:(j + 1) * 128], vaug[:, kt, h, 0:129],
                                      start=(idx == 0), stop=(idx == len(ktiles) - 1)), r=[tea, t_v[kt]], w=[tob])
                        P.dve(I_copy(osb[sm][:, j * 130:j * 130 + 129], ob[:, 0:129]), r=[tob], w=[tosb[sm]])
                for j in range(nq):
                    s8, ts8 = sm_r.next()
                    o1, to1 = o1_r.next()
                    o2, to2 = o2_r.next()
                    P.dve(lambda e, o=s8[:, 0:1], i=osb[0][:, j * 130 + 128:j * 130 + 129]: e.reciprocal(out=o, in_=i), r=[tosb[0]], w=[ts8])
                    P.dve(lambda e, o=s8[:, 1:2], i=osb[1][:, j * 130 + 128:j * 130 + 129]: e.reciprocal(out=o, in_=i), r=[tosb[1]], w=[ts8])
                    P.dve(I_tt(s8[:, 2:3], s8[:, 1:2], neglam[:, 0:1], ALU.mult), r=[ts8, tng], w=[ts8])
                    P.dve(I_ts(o1, osb[0][:, j * 130:j * 130 + 128], s8[:, 0:1], ALU.mult), r=[tosb[0], ts8], w=[to1])
                    P.dve(I_stt(o1, osb[1][:, j * 130:j * 130 + 128], s8[:, 2:3], o1, ALU.mult, ALU.add), r=[tosb[1], ts8, to1], w=[to1])
                    P.pool(I_tt(o2, o1, o1, ALU.mult), r=[to1], w=[to2])
                    P.dve(I_red(s8[:, 3:4], o2), r=[to2], w=[ts8])
                    self.rsqrt(s8[:, 4:5], s8[:, 3:4], 1.0 / 128, 1e-5, [ts8], ts8)
                    P.dve(I_stt(oda3[:, j, h * 128:(h + 1) * 128], o1, s8[:, 4:5], gsub, ALU.mult, ALU.mult), r=[to1, ts8, tng], w=[toda])
            for j in range(nq):
                ti = qs0 // 128 + j
                ps, pt = self.PS.next()
                for h in range(4):
                    P.pe(I_tr(ps[:, h * 128:(h + 1) * 128], oda3[:, j, h * 128:(h + 1) * 128], self.ident), r=[toda, self.t_c], w=[pt])
                stg, tst = stg_r.next()
                P.act(I_acopy(stg, ps), r=[pt], w=[tst])
                P.dma("sp", I_dma(self.brT_d[0, :, :, ti * 128:(ti + 1) * 128].rearrange("k p t -> p k t"),
                                  stg.rearrange("p (k t) -> p k t", k=4)), r=[tst], w=[self.t_br[0]])
        P.barrier()
        A.release(m)

    def conv_phase(self, li):
        P, A = self.P, self.A
        m = A.mark()
        self.B.off = 0
        W = self.W["w_in"][li]
        ws = [self.AL(8 * 512, BF16).rearrange("p (k c) -> p k c", k=8) for _ in range(3)]
        tw = toks(3)
        for i in range(3):
            self.load_w_bf16(ws[i], W[:, 1536 + i * 512:2048 + i * 512], tw[i])
        rows = self.AL(1536)
        trow = Tok()
        P.dma("sp", I_dma(rows[0:1, :], self.W["conv_w"][li:li + 1].rearrange("o a b -> o (a b)")), w=[trow])
        cw = self.AL(16)
        tcw = Tok()
        self.rows_to_cols(cw[:, 0:12], rows, 12, trow, tcw)
        ubuf = self.AL(2312)
        tub = Tok()
        P.pool(I_memset(ubuf, 0.0), w=[tub])
        sbT = self.AL(T, BF16)
        tsb = Tok()
        sg_r = Ring([self.AL(512) for _ in range(2)])
        c1 = self.AL(TL)
        tc1 = Tok()
        osc = self.AL(T, BF16)
        tosc = Tok()
        for ch in range(4):
            for tg, (s, n) in enumerate(TGS):
                pss = [self.PS.next() for _ in range(3)]
                for i in range(3):
                    for kc in range(8):
                        P.pe(I_mm(pss[i][0][:, 0:n], ws[i][:, kc, ch * 128:(ch + 1) * 128], self.hT3[:, kc, s:s + n],
                                  start=(kc == 0), stop=(kc == 7)), r=[tw[i], self.t_h[tg]], w=[pss[i][1]])
                off = 1 + s if tg < 4 else 2051
                sg, tsg = sg_r.next()
                P.act(I_acopy(sbT[:, s:s + n], pss[0][0][:, 0:n]), r=[pss[0][1]], w=[tsb])
                P.act(I_acopy(sg[:, 0:n], pss[1][0][:, 0:n]), r=[pss[1][1]], w=[tsg])
                P.dve(I_tt(ubuf[:, off:off + n], pss[2][0][:, 0:n], sg[:, 0:n], ALU.mult), r=[pss[2][1], tsg], w=[tub])
            for (o0, u0, n) in ((0, 0, TL), (TL, 2050, TC)):
                P.dve(I_ts(c1[:, 0:n], ubuf[:, u0:u0 + n], cw[:, ch:ch + 1], ALU.mult), r=[tub, tcw], w=[tc1])
                P.dve(I_stt(c1[:, 0:n], ubuf[:, u0 + 1:u0 + 1 + n], cw[:, 4 + ch:5 + ch], c1[:, 0:n], ALU.mult, ALU.add), r=[tub, tcw], w=[tc1])
                P.dve(I_stt(c1[:, 0:n], ubuf[:, u0 + 2:u0 + 2 + n], cw[:, 8 + ch:9 + ch], c1[:, 0:n], ALU.mult, ALU.add), r=[tub, tcw], w=[tc1])
                P.pool(I_tt(osc[:, o0:o0 + n], c1[:, 0:n], sbT[:, o0:o0 + n], ALU.mult), r=[tc1, tsb], w=[tosc])
            P.dma("sp", I_dma(self.brT_d[1, ch], osc), r=[tosc], w=[self.t_br[1]])
        P.barrier()
        A.release(m)

    def merge_phase(self, li):
        P, A = self.P, self.A
        m = A.mark()
        W = self.W["w_in"][li]
        wg = A.alloc(8 * 1024, BF16).rearrange("p (k c) -> p k c", k=8)
        wb = A.alloc(4 * 1024, BF16).rearrange("p (k c) -> p k c", k=4)
        twg, twb = Tok(), Tok()
        mg = A.alloc(8 * T, BF16).rearrange("p (k t) -> p k t", k=8)
        tmg = toks(5)
        br_r = Ring([A.alloc(4 * 512, BF16) for _ in range(2)])
        gs_r = Ring([A.alloc(512) for _ in range(2)])
        for n_ in range(3):
            self.load_w_bf16(wg, W[:, 4608 + n_ * 1024:4608 + (n_ + 1) * 1024], twg)
            self.load_w_bf16(wb, self.W["w_branch"][li, n_], twb, kc=4)
            for tg, (s, n) in enumerate(TGS):
                br, tbr = br_r.next()
                br3 = br.rearrange("p (k t) -> p k t", k=4)
                P.dma("sp", I_dma(br3[:, :, 0:n], self.brT_d[n_, :, :, s:s + n].rearrange("k p t -> p k t")), r=[self.t_br[n_]], w=[tbr])
                for cc in range(8):
                    psg, ptg = self.PS.next()
                    for kc in range(8):
                        P.pe(I_mm(psg[:, 0:n], wg[:, kc, cc * 128:(cc + 1) * 128], self.hT3[:, kc, s:s + n], start=(kc == 0), stop=(kc == 7)),
                             r=[twg, self.t_h[tg]], w=[ptg])
                    gs, tgs = gs_r.next()
                    P.act(I_act(gs[:, 0:n], psg[:, 0:n], AF.Sigmoid), r=[ptg], w=[tgs])
                    psp, ptp = self.PS.next()
                    for kc in range(4):
                        P.pe(I_mm(psp[:, 0:n], wb[:, kc, cc * 128:(cc + 1) * 128], br3[:, kc, 0:n], start=(kc == 0), stop=(kc == 3)),
                             r=[twb, tbr], w=[ptp])
                    if n_ == 0:
                        P.dve(I_tt(mg[:, cc, s:s + n], psp[:, 0:n], gs[:, 0:n], ALU.mult), r=[ptp, tgs], w=[tmg[tg]])
                    else:
                        P.dve(I_tt(gs[:, 0:n], psp[:, 0:n], gs[:, 0:n], ALU.mult), r=[ptp, tgs], w=[tgs])
                        P.pool(I_tt(mg[:, cc, s:s + n], mg[:, cc, s:s + n], gs[:, 0:n], ALU.add), r=[tmg[tg], tgs], w=[tmg[tg]])
        wo = wg
        self.load_w_bf16(wo, self.W["w_out"][li], twg)
        for tg, (s, n) in enumerate(TGS):
            col = 0 if tg < 4 else 1
            for cc in range(8):
                ps, pt = self.PS.next()
                for kc in range(8):
                    P.pe(I_mm(ps[:, 0:n], wo[:, kc, cc * 128:(cc + 1) * 128], mg[:, kc, s:s + n], start=(kc == 0), stop=(kc == 7)),
                         r=[twg, tmg[tg]], w=[pt])
                P.dve(I_stt(self.xT3[:, cc, s:s + n], ps[:, 0:n], self.eff4[:, 2, cc, col:col + 1], self.xT3[:, cc, s:s + n], ALU.mult, ALU.add),
                      r=[pt, self.t_eff, self.t_x[tg]], w=[self.t_x[tg]])
        P.barrier()
        A.release(m)

    def final_phase(self):
        P, A = self.P, self.A
        m = A.mark()
        sq = A.alloc(8 * 512)
        sq3 = sq.rearrange("p (k t) -> p k t", k=8)
        tsq = Tok()
        rstd_r = Ring([A.alloc(512) for _ in range(2)])
        y_r = Ring([A.alloc(8 * 512) for _ in range(2)])
        o_r = Ring([A.alloc(1024) for _ in range(3)])
        for tg, (s, n) in enumerate(TGS[:4]):
            P.act(I_act(sq3[:, :, 0:n], self.xT3[:, :, s:s + n], AF.Square), r=[self.t_x[tg]], w=[tsq])
            ps, pt = self.PS.next()
            for k in range(8):
                P.pe(I_mm(ps[:, 0:n], self.ones, sq3[:, k, 0:n], start=(k == 0), stop=(k == 7)), r=[tsq, self.t_c], w=[pt])
            rstd, trs = rstd_r.next()
            self.rsqrt(rstd[:, 0:n], ps[:, 0:n], 1.0 / D, 1e-6, [pt], trs)
            y, ty = y_r.next()
            y3 = y.rearrange("p (k t) -> p k t", k=8)
            for k in range(8):
                P.dve(I_stt(y3[:, k, 0:n], self.xT3[:, k, s:s + n], self.gvec[:, 16 + k:17 + k], rstd[:, 0:n], ALU.mult, ALU.mult),
                      r=[self.t_x[tg], self.t_gvec, trs], w=[ty])
            for j in range(n // 128):
                ti = s // 128 + j
                ob, tob = o_r.next()
                for half in range(2):
                    ps, pt = self.PS.next()
                    for q in range(4):
                        k = half * 4 + q
                        P.pe(I_tr(ps[:, q * 128:(q + 1) * 128], y3[:, k, j * 128:(j + 1) * 128], self.ident), r=[ty, self.t_c], w=[pt])
                    P.act(I_acopy(ob[:, half * 512:(half + 1) * 512], ps), r=[pt], w=[tob])
                to = Tok()
                P.dma("sp", I_dma(self.out_d[ti * 128:(ti + 1) * 128, :], ob), r=[tob], w=[to])
                self.out_toks.append(to)
        P.barrier()
        A.release(m)

    def rwkv_phase(self, li):
        P, A = self.P, self.A
        m = A.mark()
        W = self.W["w_in"][li]
        DS = 0.606531
        self.B.off = 0
        B = self.B
        AL = self.AL
        a512 = lambda dt=F32: AL(512, dt)
        wrkv = [A.alloc(8 * 512, BF16).rearrange("p (k c) -> p k c", k=8) for _ in range(3)]
        tw = toks(3)
        for i in range(3):
            self.load_w_bf16(wrkv[i], W[:, 3072 + i * 512:3584 + i * 512], tw[i])
        lo1 = A.alloc(8 * 416, BF16).rearrange("p (k c) -> p k c", k=8)
        tlo = Tok()
        for d in range(2):
            self.load_w_bf16(lo1[:, :, d * 64:(d + 1) * 64], self.W["rw_w1"][li, d], tlo)
            self.load_w_bf16(lo1[:, :, 128 + d * 64:128 + (d + 1) * 64], self.W["rw_a1"][li, d], tlo)
        self.load_w_bf16(lo1[:, :, 256:416], self.W["rw_g1"][li], tlo)
        lo2 = A.alloc(6 * 512, BF16).rearrange("p (k c) -> p k c", k=6)
        for d in range(2):
            P.dma("pool", I_dma(lo2[0:64, d, :], self.W["rw_w2"][li, d]), w=[tlo])
            P.dma("pool", I_dma(lo2[0:64, 2 + d, :], self.W["rw_a2"][li, d]), w=[tlo])
        P.dma("pool", I_dma(lo2[:, 4, :], self.W["rw_g2"][li, 0:128, :]), w=[tlo])
        P.dma("pool", I_dma(lo2[0:32, 5, :], self.W["rw_g2"][li, 128:160, :]), w=[tlo])
        brow = A.alloc(4 * 512, BF16)
        P.dma("pool", I_dma(brow[0:1, 0:1024], self.W["rw_w0"][li:li + 1].rearrange("o a b -> o (a b)")), w=[tlo])
        P.dma("pool", I_dma(brow[0:1, 1024:2048], self.W["rw_a0"][li:li + 1].rearrange("o a b -> o (a b)")), w=[tlo])
        rows = A.alloc(5 * 512, BF16)
        trow = Tok()
        for i, nm in enumerate(["rw_k_k", "rw_k_a", "rw_lnx_g", "rw_lnx_b"]):
            P.dma("pool", I_dma(rows[0:1, i * 512:(i + 1) * 512], self.W[nm][li:li + 1, :]), w=[trow])
        P.dma("pool", I_dma(rows[0:1, 2048:2560], self.W["rw_r_k"][li:li + 1].rearrange("o a b -> o (a b)")), w=[trow])
        bc = [A.alloc(512) for _ in range(5)]
        tbc = Tok()
        for i in range(5):
            self.bcast_rows(bc[i], rows[:, i * 512:(i + 1) * 512], 512, trow, tbc, ones=self.ones_b)
        kk_bc, ka_bc, lg_bc, lb_bc, rk_bc = bc
        A.release(A.mark())
        msk = {}
        tmk = Tok()
        for d, (nS, nI, nST) in enumerate((("m_f_strict", "m_f_incl", "m_r_strict"), ("m_r_strict", "m_r_incl", "m_f_strict"))):
            mc1, mc2, mn = A.alloc(256, BF16), A.alloc(256, BF16), A.alloc(128, BF16)
            tri = A.alloc(128)
            P.dma("pool", I_dma(mc2[:, 0:128], self.Cd[nS]), w=[tmk])
            P.dma("pool", I_dma(mc2[:, 128:256], self.Cd[nI]), w=[tmk])
            P.dma("pool", I_dma(mn, self.Cd[nST]), w=[tmk])
            P.dma("sp", I_dma(tri, self.Cd[nI]), w=[tmk])
            P.dve(I_ts(mc1[:, 0:128], mc2[:, 0:128], -1.0, ALU.mult), r=[tmk], w=[tmk])
            P.dve(I_copy(mc1[:, 128:256], mc2[:, 128:256]), r=[tmk], w=[tmk])
            P.dve(I_ts(mn, mn, -1.0, ALU.mult), r=[tmk], w=[tmk])
            P.dve(I_ts(tri, tri, -DS, ALU.mult), r=[tmk], w=[tmk])
            msk[d] = (mc1, mc2, mn, tri)
        allneg = A.alloc(128)
        P.dve(I_ts(allneg, self.ones, -DS, ALU.mult), r=[self.t_c], w=[tmk])
        Mst = [[A.alloc(512) for _ in range(2)] for _ in range(2)]
        tM = [toks(2) for _ in range(2)]
        for d in range(2):
            P.pool(I_memset(Mst[d][0], 0.0), w=[tM[d][0]])
        mpar = [0, 0]
        r_s, k_s, v_s, sig, a_s, kx, kk, b_s, kE, tA, tB, cum_s, cumC_s = [a512() for _ in range(13)]
        tk_ = {n: Tok() for n in "r k v sig a kx kk b kE tA tB cum cumC l1 la e1 e2 e3 e4 kt bt k2 rt kh bh vb ktb rtb fT pc dg".split()}
        l1 = AL(128, BF16)
        la = AL(128, BF16)
        e1, e2, e3, e4 = [a512(BF16) for _ in range(4)]
        kt_, bt_, k2_, rt_ = [a512(BF16) for _ in range(4)]
        kh_, bh_, vb_ = [a512(BF16) for _ in range(3)]
        red8 = A.alloc(16)
        tred = Tok()
        pcb = a512()
        dg = a512()
        fT = [AL(4 * 128, BF16).rearrange("p (q t) -> p q t", q=4) for _ in range(4)]
        tfT = toks(4)
        hb = []
        for h in range(8):
            d_ = {}
            for nm, sz in (("X0", 256), ("X1", 256), ("P0", 128), ("P1", 128), ("A1", 256), ("A2", 256), ("KAV", 128), ("WU", 128)):
                d_[nm] = AL(sz, BF16)
            d_["t"] = {nm: Tok() for nm in ("X0", "X1", "P0", "P1", "A1", "A2", "KAV", "WU")}
            hb.append(d_)
        PhiT = AL(512)
        Gall = AL(512)
        QT = AL(8 * 128).rearrange("p (h t) -> p h t", h=8)
        Y0s = a512()
        yout = AL(1024)
        tPhi, tG, tQT, tY0, tyo = Tok(), Tok(), Tok(), Tok(), Tok()
        tys = [[Tok() for _ in range(NT)] for _ in range(2)]
        order = [[16, 17] + list(range(16)), [17, 16] + list(range(15, -1, -1))]
        nsteps = NT if self.stop != f"rw1_{li}" else 3
        for step in range(nsteps):
            for d in range(2):
                i = order[d][step]
                mc1, mc2, mn, tri = msk[d]
                tgi = min(i // 4, 4)
                th = self.t_h[tgi]
                cols = slice(i * 128, (i + 1) * 128)
                for q, (dst, tn) in enumerate(((r_s, "r"), (k_s, "k"), (v_s, "v"))):
                    ps, pt = self.PS.next()
                    for kc in range(8):
                        P.pe(I_mm(ps, self.hT3[:, kc, cols], wrkv[q][:, kc, :], start=(kc == 0), stop=(kc == 7)), r=[tw[q], th], w=[pt])
                    P.act(I_acopy(dst, ps), r=[pt], w=[tk_[tn]])
                P.pool(I_copy(vb_, v_s), r=[tk_["v"]], w=[tk_["vb"]])
                for (c0, dst, tn, fn) in ((d * 64, l1, "l1", AF.Tanh), (128 + d * 64, la, "la", AF.Copy)):
                    ps, pt = self.PS.next()
                    for kc in range(8):
                        P.pe(I_mm(ps[0:64, 0:128], lo1[:, kc, c0:c0 + 64], self.hT3[:, kc, cols], start=(kc == 0), stop=(kc == 7)), r=[tlo, th], w=[pt])
                    P.act(I_act(dst[0:64, :], ps[0:64, 0:128], fn), r=[pt], w=[tk_[tn]])
                for (src, tn_s, wi, bi, dst, tn) in ((l1, "l1", d, d, sig, "sig"), (la, "la", 2 + d, 2 + d, a_s, "a")):
                    ps, pt = self.PS.next()
                    P.pe(I_mm(ps, src[0:64, :], lo2[0:64, wi, :], start=True, stop=False), r=[tk_[tn_s], tlo], w=[pt])
                    P.pe(I_mm(ps, self.ones_b[0:1, 0:128], brow[0:1, bi * 512:(bi + 1) * 512], start=False, stop=True), r=[tlo, self.t_c], w=[pt])
                    P.act(I_act(dst, ps, AF.Sigmoid), r=[pt], w=[tk_[tn]])
                P.pool(I_tt(kx, k_s, kk_bc, ALU.mult), r=[tk_["k"], tbc], w=[tk_["kx"]])
                P.pool(I_tt(tA, kx, kx, ALU.mult), r=[tk_["kx"]], w=[tk_["tA"]])
                P.dve(I_red(red8[:, 0:8], tA.rearrange("p (h j) -> p h j", h=8)), r=[tk_["tA"]], w=[tred])
                P.dve(I_ts(red8[:, 0:8], red8[:, 0:8], 1e-24, ALU.max), r=[tred], w=[tred])
                self.rsqrt(red8[:, 0:8], red8[:, 0:8], 1.0, 1e-24, [tred], tred)
                P.dve(I_tt(kk.rearrange("p (h j) -> p h j", h=8), kx.rearrange("p (h j) -> p h j", h=8),
                           red8[:, 0:8].unsqueeze(2).to_broadcast([128, 8, 64]), ALU.mult), r=[tk_["kx"], tred], w=[tk_["kk"]])
                P.pool(I_tt(b_s, kk, a_s, ALU.mult), r=[tk_["kk"], tk_["a"]], w=[tk_["b"]])
                P.dve(I_stt(tB, a_s, -1.0, ka_bc, ALU.add, ALU.mult), r=[tk_["a"], tbc], w=[tk_["tB"]])
                P.dve(I_stt(kE, tB, 1.0, k_s, ALU.add, ALU.mult), r=[tk_["tB"], tk_["k"]], w=[tk_["kE"]])
                P.pool(I_tt(tA, r_s, kE, ALU.mult), r=[tk_["r"], tk_["kE"], tred], w=[tk_["tA"]])
                P.pool(I_tt(tA, tA, rk_bc, ALU.mult), r=[tk_["tA"], tbc], w=[tk_["tA"]])
                P.dve(I_red(red8[:, 8:16], tA.rearrange("p (h j) -> p h j", h=8)), r=[tk_["tA"]], w=[tred])
                P.dve(I_tt(yout[:, 512:1024].rearrange("p (h j) -> p h j", h=8), v_s.rearrange("p (h j) -> p h j", h=8),
                           red8[:, 8:16].unsqueeze(2).to_broadcast([128, 8, 64]), ALU.mult), r=[tk_["v"], tred], w=[tyo])
                psc, ptc = self.PS.next()
                psC, ptC = self.PS.next()
                P.pe(I_mm(psc, tri, sig), r=[tmk, tk_["sig"]], w=[ptc])
                P.pe(I_mm(psC, allneg, sig), r=[tmk, tk_["sig"]], w=[ptC])
                P.act(I_acopy(cum_s, psc), r=[ptc], w=[tk_["cum"]])
                P.act(I_acopy(cumC_s, psC), r=[ptC], w=[tk_["cumC"]])
                P.dve(I_stt(tB, sig, DS, cum_s, ALU.mult, ALU.add), r=[tk_["sig"], tk_["cum"], tk_["kE"]], w=[tk_["tB"]])
                P.act(I_act(e1, tB, AF.Exp), r=[tk_["tB"]], w=[tk_["e1"]])
                P.act(I_act(e2, cum_s, AF.Exp, scale=-1.0), r=[tk_["cum"]], w=[tk_["e2"]])
                P.act(I_act(e3, cum_s, AF.Exp), r=[tk_["cum"]], w=[tk_["e3"]])
                P.dve(I_tt(tA, cumC_s, cum_s, ALU.subtract), r=[tk_["cumC"], tk_["cum"], tred], w=[tk_["tA"]])
                P.act(I_act(e4, tA, AF.Exp), r=[tk_["tA"]], w=[tk_["e4"]])
                P.act(I_act(pcb, cumC_s, AF.Exp), r=[tk_["cumC"]], w=[tk_["pc"]])
                P.pool(I_tt(dg[0:64, :].rearrange("p (h j) -> p h j", h=8), pcb[0:64, :].rearrange("p (h j) -> p h j", h=8),
                            self.ident[0:64, 0:64].unsqueeze(1).to_broadcast([64, 8, 64]), ALU.mult), r=[tk_["pc"], self.t_c], w=[tk_["dg"]])
                P.dve(I_tt(kt_, kk, e1, ALU.mult), r=[tk_["kk"], tk_["e1"]], w=[tk_["kt"]])
                P.pool(I_tt(bt_, b_s, e2, ALU.mult), r=[tk_["b"], tk_["e2"]], w=[tk_["bt"]])
                P.dve(I_tt(k2_, kE, e2, ALU.mult), r=[tk_["kE"], tk_["e2"]], w=[tk_["k2"]])
                P.pool(I_tt(rt_, r_s, e3, ALU.mult), r=[tk_["r"], tk_["e3"]], w=[tk_["rt"]])
                P.dve(I_tt(kh_, kE, e4, ALU.mult), r=[tk_["kE"], tk_["e4"]], w=[tk_["kh"]])
                P.pool(I_tt(bh_, b_s, e4, ALU.mult), r=[tk_["b"], tk_["e4"]], w=[tk_["bh"]])
                for g in range(4):
                    ps, pt = self.PS.next()
                    psb = ps.bitcast(BF16)
                    for q, (src, tn) in enumerate(((kt_, "kt"), (rt_, "rt"), (bt_, "bt"), (k2_, "k2"))):
                        P.pe(I_tr(psb[:, q * 128:(q + 1) * 128], src[:, g * 128:(g + 1) * 128], self.ident_b), r=[tk_[tn], self.t_c], w=[pt])
                    P.op("act" if g % 2 == 0 else "dve", (I_acopy if g % 2 == 0 else I_copy)(fT[g].rearrange("p q t -> p (q t)"), psb[:, 0:512]), r=[pt], w=[tfT[g]])
                for h in range(8):
                    g, R_ = h // 2, slice((h % 2) * 64, (h % 2) * 64 + 64)
                    H = hb[h]
                    f = fT[g]
                    ps1, pt1 = self.PS.next()
                    P.pe(I_mm(ps1[:, 0:256], f[R_, 2, :], f[R_, 0:2, :].rearrange("p q t -> p (q t)")), r=[tfT[g]], w=[pt1])
                    P.dve(I_tt(H["A1"], ps1[:, 0:256], mc1, ALU.mult), r=[pt1, tmk], w=[H["t"]["A1"]])
                    ps2, pt2 = self.PS.next()
                    P.pe(I_mm(ps2[:, 0:256], f[R_, 3, :], f[R_, 0:2, :].rearrange("p q t -> p (q t)")), r=[tfT[g]], w=[pt2])
                    P.dve(I_tt(H["A2"], ps2[:, 0:256], mc2, ALU.mult), r=[pt2, tmk], w=[H["t"]["A2"]])
                    ps3, pt3 = self.PS.next()
                    P.pe(I_mm(ps3[:, 0:128], f[R_, 0, :], f[R_, 2, :]), r=[tfT[g]], w=[pt3])
                    P.dve(I_tt(H["X0"][:, 0:128], ps3[:, 0:128], mn, ALU.mult), r=[pt3, tmk], w=[H["t"]["X0"]])
                    P.pool(I_copy(H["X0"][:, 128:256], H["A1"][:, 0:128]), r=[H["t"]["A1"]], w=[H["t"]["X0"]])
                    P.pool(I_tt(H["P0"], H["A1"][:, 0:128], self.ident_b, ALU.add), r=[H["t"]["A1"], self.t_c], w=[H["t"]["P0"]])
                cur = 0
                for lvl in range(1, 7):
                    nxt = 1 - cur
                    for h in range(8):
                        H = hb[h]
                        XNc, XNn = H[f"X{cur}"], H[f"X{nxt}"]
                        tXc, tXn = H["t"][f"X{cur}"], H["t"][f"X{nxt}"]
                        Nc, Xc = XNc[:, 0:128], XNc[:, 128:256]
                        psA, ptA = self.PS.next()
                        P.pe(I_mm(psA[:, 0:128], Xc, Nc), r=[tXc], w=[ptA])
                        w_ = 128
                        if lvl < 6:
                            P.pe(I_mm(psA[:, 128:256], Nc, Xc), r=[tXc], w=[ptA])
                            w_ = 256
                        if h % 2 == 0:
                            P.act(I_acopy(XNn[:, 0:w_], psA[:, 0:w_]), r=[ptA], w=[tXn])
                        else:
                            P.dve(I_copy(XNn[:, 0:w_], psA[:, 0:w_]), r=[ptA], w=[tXn])
                    for h in range(8):
                        H = hb[h]
                        XNn, Pc, Pn = H[f"X{nxt}"], H[f"P{cur}"], H[f"P{nxt}"]
                        tXn, tPc, tPn = H["t"][f"X{nxt}"], H["t"][f"P{cur}"], H["t"][f"P{nxt}"]
                        psP, ptP = self.PS.next()
                        P.pe(I_mm(psP[:, 0:128], XNn[:, 0:128], Pc), r=[tXn, tPc], w=[ptP])
                        P.dve(I_tt(Pn, psP[:, 0:128], Pc, ALU.add), r=[ptP, tPc], w=[tPn])
                    cur = nxt
                for h in range(8):
                    hc = slice(h * 64, (h + 1) * 64)
                    H = hb[h]
                    P.pool(I_copy(H["KAV"][:, 0:64], kt_[:, hc]), r=[tk_["kt"]], w=[H["t"]["KAV"]])
                    ps, pt = self.PS.next()
                    P.pe(I_mm(ps[:, 0:64], H["A2"][:, 0:128], vb_[:, hc]), r=[H["t"]["A2"], tk_["vb"]], w=[pt])
                    P.act(I_acopy(H["KAV"][:, 64:128], ps[:, 0:64]), r=[pt], w=[H["t"]["KAV"]])
                for h in range(8):
                    H = hb[h]
                    TT, tTT = H[f"P{cur}"], H["t"][f"P{cur}"]
                    ps, pt = self.PS.next()
                    P.pe(I_mm(ps[:, 0:128], TT, H["KAV"]), r=[tTT, H["t"]["KAV"]], w=[pt])
                    P.dve(I_ts(H["WU"], ps[:, 0:128], -1.0, ALU.mult), r=[pt], w=[H["t"]["WU"]])
                psY0, ptY0 = self.PSacc.next()
                psPhi, ptPhi = self.PSacc.next()
                psG, ptG = self.PSacc.next()
                for h in range(8):
                    hc = slice(h * 64, (h + 1) * 64)
                    H = hb[h]
                    tWU = H["t"]["WU"]
                    ps, pt = self.PS.next()
                    P.pe(I_mm(ps[0:64, 0:128], H["WU"][:, 0:64], H["A1"][:, 128:256], start=True, stop=False), r=[tWU, H["t"]["A1"]], w=[pt])
                    P.pe(I_mm(ps[0:64, 0:128], rt_[:, hc], self.ident_b, start=False, stop=True), r=[tk_["rt"], self.t_c], w=[pt])
                    P.act(I_acopy(QT[0:64, h, :], ps[0:64, 0:128]), r=[pt], w=[tQT])
                    P.pe(I_mm(psY0[:, hc], H["A1"][:, 128:256], H["WU"][:, 64:128], start=True, stop=False), r=[tWU, H["t"]["A1"]], w=[ptY0])
                    P.pe(I_mm(psY0[:, hc], H["A2"][:, 128:256], vb_[:, hc], start=False, stop=True), r=[H["t"]["A2"], tk_["vb"]], w=[ptY0])
                    P.pe(I_mm(psPhi[0:64, hc], H["WU"][:, 0:64], bh_[:, hc]), r=[tWU, tk_["bh"]], w=[ptPhi])
                    P.pe(I_mm(psG[0:64, hc], bh_[:, hc], H["WU"][:, 64:128], start=True, stop=False), r=[tWU, tk_["bh"]], w=[ptG])
                    P.pe(I_mm(psG[0:64, hc], kh_[:, hc], vb_[:, hc], start=False, stop=True), r=[tk_["kh"], tk_["vb"]], w=[ptG])
                P.dve(I_copy(Y0s, psY0), r=[ptY0], w=[tY0])
                P.dve(I_tt(PhiT[0:64, :], psPhi[0:64, :], dg[0:64, :], ALU.add), r=[ptPhi, tk_["dg"]], w=[tPhi])
                P.act(I_acopy(Gall[0:64, :], psG[0:64, :]), r=[ptG], w=[tG])
                pc_, pn_ = mpar[d], 1 - mpar[d]
                Mc, Mn = Mst[d][pc_], Mst[d][pn_]
                tMc, tMn = tM[d][pc_], tM[d][pn_]
                psY, ptY = self.PSacc.next()
                for h in range(8):
                    hc = slice(h * 64, (h + 1) * 64)
                    P.pe(I_mm(psY[:, hc], QT[0:64, h, :], Mc[0:64, hc]), r=[tQT, tMc], w=[ptY])
                P.dve(I_tt(yout[:, 0:512], psY, Y0s, ALU.add), r=[ptY, tY0], w=[tyo])
                psM, ptM = self.PS.next()
                for h in range(8):
                    hc = slice(h * 64, (h + 1) * 64)
                    P.pe(I_mm(psM[0:64, hc], PhiT[0:64, hc], Mc[0:64, hc]), r=[tPhi, tMc], w=[ptM])
                P.dve(I_tt(Mn[0:64, :], psM[0:64, :], Gall[0:64, :], ALU.add), r=[ptM, tG], w=[tMn])
                mpar[d] = pn_
                P.dma("sp", I_dma(self.yscr_d[d, i], yout), r=[tyo], w=[tys[d][i]])
        P.barrier()
        B.off = 0
        ya_r = Ring([AL(1024) for _ in range(2)])
        yb_r = Ring([AL(1024) for _ in range(2)])
        gs1 = AL(128, BF16)
        gs2 = AL(128, BF16)
        tgs = Tok()
        stg_r = Ring([AL(512, BF16) for _ in range(2)])
        yc = a512()
        tyc = Tok()
        for i in range(NT):
            if nsteps < NT and i not in (16, 17):
                continue
            cols = slice(i * 128, (i + 1) * 128)
            th = self.t_h[min(i // 4, 4)]
            ya, tya = ya_r.next()
            yb, tyb = yb_r.next()
            P.dma("sp", I_dma(ya, self.yscr_d[0, i]), r=[tys[0][i]], w=[tya])
            P.dma("act", I_dma(yb, self.yscr_d[1, i]), r=[tys[1][i]], w=[tyb])
            P.pool(I_tt(ya, ya, yb, ALU.add), r=[tya, tyb], w=[tya])
            y3 = ya[:, 0:512].rearrange("p (h j) -> p h j", h=8)
            P.dve(I_red(red8[:, 0:8], y3), r=[tya], w=[tred])
            P.dve(I_ts(red8[:, 0:8], red8[:, 0:8], 1.0 / 64, ALU.mult), r=[tred], w=[tred])
            P.dve(I_tt(yc.rearrange("p (h j) -> p h j", h=8), y3, red8[:, 0:8].unsqueeze(2).to_broadcast([128, 8, 64]), ALU.subtract),
                  r=[tya, tred], w=[tyc])
            P.pool(I_tt(yb[:, 0:512], yc, yc, ALU.mult), r=[tyc], w=[tyb])
            P.dve(I_red(red8[:, 8:16], yb[:, 0:512].rearrange("p (h j) -> p h j", h=8)), r=[tyb], w=[tred])
            self.rsqrt(red8[:, 8:16], red8[:, 8:16], 1.0 / 64, 64e-5, [tred], tred)
            P.dve(I_tt(yc.rearrange("p (h j) -> p h j", h=8), yc.rearrange("p (h j) -> p h j", h=8),
                       red8[:, 8:16].unsqueeze(2).to_broadcast([128, 8, 64]), ALU.mult), r=[tyc, tred], w=[tyc])
            P.pool(I_tt(yc, yc, lg_bc, ALU.mult), r=[tyc, tbc], w=[tyc])
            P.pool(I_tt(yc, yc, lb_bc, ALU.add), r=[tyc, tbc], w=[tyc])
            P.pool(I_tt(yc, yc, ya[:, 512:1024], ALU.add), r=[tyc, tya], w=[tyc])
            ps, pt = self.PS.next()
            for kc in range(8):
                P.pe(I_mm(ps[:, 0:128], lo1[:, kc, 256:384], self.hT3[:, kc, cols], start=(kc == 0), stop=(kc == 7)), r=[tlo, th], w=[pt])
            P.act(I_act(gs1, ps[:, 0:128], AF.Sigmoid), r=[pt], w=[tgs])
            ps, pt = self.PS.next()
            for kc in range(8):
                P.pe(I_mm(ps[0:32, 0:128], lo1[:, kc, 384:416], self.hT3[:, kc, cols], start=(kc == 0), stop=(kc == 7)), r=[tlo, th], w=[pt])
            P.act(I_act(gs2[0:32, :], ps[0:32, 0:128], AF.Sigmoid), r=[pt], w=[tgs])
            ps, pt = self.PS.next()
            P.pe(I_mm(ps, gs1, lo2[:, 4, :], start=True, stop=False), r=[tgs, tlo], w=[pt])
            P.pe(I_mm(ps, gs2[0:32, :], lo2[0:32, 5, :], start=False, stop=True), r=[tgs, tlo], w=[pt])
            P.dve(I_tt(yc, yc, ps, ALU.mult), r=[tyc, pt], w=[tyc])
            ps, pt = self.PS.next()
            for g in range(4):
                P.pe(I_tr(ps[:, g * 128:(g + 1) * 128], yc[:, g * 128:(g + 1) * 128], self.ident), r=[tyc, self.t_c], w=[pt])
            stg, tst = stg_r.next()
            P.act(I_acopy(stg, ps), r=[pt], w=[tst])
            P.dma("sp", I_dma(self.brT_d[2, :, :, cols].rearrange("k p t -> p k t"), stg.rearrange("p (k t) -> p k t", k=4)),
                  r=[tst], w=[self.t_br[2]])
        P.barrier()
        A.release(m)

    def moe_router_setup(self, li):
        P, A = self.P, self.A
        self.wr = A.alloc(8 * 32).rearrange("p (k c) -> p k c", k=8)
        self.t_wr = Tok()
        P.dma("sp", I_dma(self.wr, self.W["router_w"][li].rearrange("(k p) c -> p k c", p=128)), w=[self.t_wr])
        self.rbrow = A.alloc(32)
        P.dma("sp", I_dma(self.rbrow[0:1, :], self.W["router_b"][li:li + 1, :]), w=[self.t_wr])
        self.GT = A.alloc(T)
        self.t_GT = Tok()
        self.rt_r = Ring([A.alloc(128) for _ in range(2)])

    def router_cb(self, tg, s, n, h2f3, th2f):
        P = self.P
        for j in range(n // 128):
            c0 = s + j * 128
            ps, pt = self.PS.next()
            for kc in range(8):
                P.pe(I_mm(ps[:, 0:32], h2f3[:, kc, j * 128:(j + 1) * 128], self.wr[:, kc, :], start=(kc == 0), stop=False),
                     r=[th2f, self.t_wr], w=[pt])
            P.pe(I_mm(ps[:, 0:32], self.ones[0:1, 0:128], self.rbrow[0:1, 0:32], start=False, stop=True), r=[self.t_wr, self.t_c], w=[pt])
            wk, twk = self.rt_r.next()
            lg, t8, msk, e_, sm = wk[:, 0:32], wk[:, 32:40], wk[:, 40:72], wk[:, 72:104], wk[:, 104:108]
            P.dve(I_copy(lg, ps[:, 0:32]), r=[pt], w=[twk])
            P.dve(lambda e, o=t8, i=lg: e.max(out=o, in_=i), r=[twk], w=[twk])
            P.dve(I_ts(msk, lg, t8[:, 3:4], ALU.is_ge), r=[twk], w=[twk])
            P.dve(I_ts(sm[:, 0:1], t8[:, 0:1], -1.0, ALU.mult), r=[twk], w=[twk])
            P.act(I_act(e_, lg, AF.Exp, bias=sm[:, 0:1]), r=[twk], w=[twk])
            P.dve(I_tt(e_, e_, msk, ALU.mult), r=[twk], w=[twk])
            P.dve(I_red(sm[:, 1:2], e_), r=[twk], w=[twk])
            P.dve(lambda e, o=sm[:, 2:3], i=sm[:, 1:2]: e.reciprocal(out=o, in_=i), r=[twk], w=[twk])
            P.dve(I_ts(e_, e_, sm[:, 2:3], ALU.mult), r=[twk], w=[twk])
            ps2, pt2 = self.PS.next()
            P.pe(I_tr(ps2[0:32, 0:128], e_, self.ident), r=[twk, self.t_c], w=[pt2])
            P.dve(I_copy(self.GT[0:32, c0:c0 + 128], ps2[0:32, 0:128]), r=[pt2], w=[self.t_GT])

    def moe_phase(self, li):
        P, A = self.P, self.A
        m = A.mark()
        b1T = A.alloc(16 * 32).rearrange("p (c e) -> p c e", c=16)
        tb1T = Tok()
        m2 = A.mark()
        self.moe_router_setup(li)
        self.norm_phase(1, li, router=self.router_cb)
        self.dbg(f"GT{li}", self.GT[0:32, :], self.t_GT, [32, T])
        tgt = Tok()
        P.dma("sp", I_dma(self.gt_d, self.GT[0:32, :]), r=[self.t_GT], w=[tgt])
        b1s = A.alloc(2048)
        tb1 = Tok()
        P.dma("act", I_dma(b1s[0:32, :], self.W["exp_b1"][li]), w=[tb1])
        for c in range(16):
            ps, pt = self.PS.next()
            P.pe(I_tr(ps[:, 0:32], b1s[0:32, c * 128:(c + 1) * 128], self.ident[0:32, 0:32]), r=[tb1, self.t_c], w=[pt])
            P.dve(I_copy(b1T[:, c, :], ps[:, 0:32]), r=[pt], w=[tb1T])
        b2s = A.alloc(1024)
        tb2 = Tok()
        P.dma("act", I_dma(b2s[0:32, :], self.W["exp_b2"][li]), w=[tb2])
        for tg, (s, n) in enumerate(TGS):
            col = 0 if tg < 4 else 1
            for cc in range(8):
                ps, pt = self.PS.next()
                P.pe(I_mm(ps[:, 0:n], b2s[0:32, cc * 128:(cc + 1) * 128], self.GT[0:32, s:s + n]), r=[tb2, self.t_GT], w=[pt])
                P.dve(I_stt(self.xT3[:, cc, s:s + n], ps[:, 0:n], self.eff4[:, 5, cc, col:col + 1], self.xT3[:, cc, s:s + n], ALU.mult, ALU.add),
                      r=[pt, self.t_eff, self.t_x[tg]], w=[self.t_x[tg]])
        P.barrier()
        A.release(m2)
        w1_r = Ring([A.alloc(8 * 256, BF16) for _ in range(4)])
        w2_r = Ring([A.alloc(8 * 128, BF16) for _ in range(4)])
        gate_r = Ring([A.alloc(T, BF16) for _ in range(2)])
        aT3 = A.alloc(8 * T, BF16).rearrange("p (c t) -> p c t", c=8)
        taT = toks(5)
        tmp_r = Ring([A.alloc(512) for _ in range(2)])
        g_r = Ring([A.alloc(512) for _ in range(3)])
        s_r = Ring([A.alloc(512, BF16) for _ in range(2)])
        l_r = Ring([A.alloc(512, BF16) for _ in range(2)])
        a_r = Ring([A.alloc(512, BF16) for _ in range(2)])
        W1, W2 = self.W["exp_w1"][li], self.W["exp_w2"][li]
        n_exp = 32 if self.stop != f"moe1_{li}" else self.n_exp_dbg
        loads, comps = [], []
        bufmap = {}

        def mk_gate(e):
            def f():
                gate, tgate = gate_r.next()
                P.dma("pool", I_dma(gate, self.gt_d[e:e + 1, :].to_broadcast([128, T])), r=[tgt], w=[tgate])
                bufmap[("g", e)] = (gate, tgate)
            return f

        def mk_w1(e, c):
            def f():
                wp, twp = w1_r.next()
                wp3 = wp.rearrange("p (k c) -> p k c", k=8)
                P.dma("pool", I_dma(wp3[:, :, 0:128], W1[e][:, c * 128:(c + 1) * 128].rearrange("(k p) c -> p k c", p=128)), w=[twp])
                P.dma("pool", I_dma(wp3[:, :, 128:256], W1[e][:, 1024 + c * 128:1024 + (c + 1) * 128].rearrange("(k p) c -> p k c", p=128)), w=[twp])
                bufmap[("w1", e, c)] = (wp3, twp)
            return f

        def mk_w2(e, cc):
            def f():
                w2p, tw2 = w2_r.next()
                w2p3 = w2p.rearrange("p (k c) -> p k c", k=8)
                P.dma("pool", I_dma(w2p3, W2[e][:, cc * 128:(cc + 1) * 128].rearrange("(k p) c -> p k c", p=128)), w=[tw2])
                bufmap[("w2", e, cc)] = (w2p3, tw2)
            return f

        def mk_hid(e, c):
            def f():
                wp3, twp = bufmap[("w1", e, c)]
                for tg, (s, n) in enumerate(TGS):
                    psg, ptg = self.PS.next()
                    psl, ptl = self.PS.next()
                    for kc in range(8):
                        P.pe(I_mm(psg[:, 0:n], wp3[:, kc, 0:128], self.hT3[:, kc, s:s + n], start=(kc == 0), stop=(kc == 7)),
                             r=[twp, self.t_h[tg]], w=[ptg])
                    for kc in range(8):
                        P.pe(I_mm(psl[:, 0:n], wp3[:, kc, 128:256], self.hT3[:, kc, s:s + n], start=(kc == 0), stop=(kc == 7)),
                             r=[twp, self.t_h[tg]], w=[ptl])
                    g_, tg_ = g_r.next()
                    s_, ts_ = s_r.next()
                    l_, tl_ = l_r.next()
                    a_, ta_ = a_r.next()
                    P.dve(I_ts(g_[:, 0:n], psg[:, 0:n], b1T[:, c, e:e + 1], ALU.add, 7.0, ALU.min), r=[ptg, tb1T], w=[tg_])
                    P.act(I_act(s_[:, 0:n], g_[:, 0:n], AF.Sigmoid, scale=1.702), r=[tg_], w=[ts_])
                    P.act(I_act(l_[:, 0:n], psl[:, 0:n], AF.Identity, bias=b1T[:, 8 + c, e:e + 1]), r=[ptl, tb1T], w=[tl_])
                    P.dve(I_ts(l_[:, 0:n], l_[:, 0:n], 7.0, ALU.min, -7.0, ALU.max), r=[tl_], w=[tl_])
                    P.dve(I_tt(a_[:, 0:n], g_[:, 0:n], s_[:, 0:n], ALU.mult), r=[tg_, ts_], w=[ta_])
                    P.dve(I_stt(aT3[:, c, s:s + n], l_[:, 0:n], 1.0, a_[:, 0:n], ALU.add, ALU.mult), r=[ta_, tl_], w=[taT[tg]])
            return f

        def mk_y(e, cc):
            def f():
                gate, tgate = bufmap[("g", e)]
                w2p3, tw2 = bufmap[("w2", e, cc)]
                for tg, (s, n) in enumerate(TGS):
                    col = 0 if tg < 4 else 1
                    ps, pt = self.PS.next()
                    for c in range(8):
                        P.pe(I_mm(ps[:, 0:n], w2p3[:, c, :], aT3[:, c, s:s + n], start=(c == 0), stop=(c == 7)), r=[tw2, taT[tg]], w=[pt])
                    tmp, ttmp = tmp_r.next()
                    P.dve(I_tt(tmp[:, 0:n], ps[:, 0:n], gate[:, s:s + n], ALU.mult), r=[pt, tgate], w=[ttmp])
                    P.dve(I_stt(self.xT3[:, cc, s:s + n], tmp[:, 0:n], self.eff4[:, 5, cc, col:col + 1], self.xT3[:, cc, s:s + n], ALU.mult, ALU.add),
                          r=[ttmp, self.t_eff, self.t_x[tg]], w=[self.t_x[tg]])
            return f

        for e in range(n_exp):
            loads.append(mk_gate(e))
            for c in range(8):
                loads.append(mk_w1(e, c))
                comps.append((len(loads) - 1, mk_hid(e, c)))
            for cc in range(8):
                loads.append(mk_w2(e, cc))
                comps.append((len(loads) - 1, mk_y(e, cc)))
        LOOK = 3
        ptr = 0
        for need, f in comps:
            tgt_ptr = min(len(loads), need + 1 + LOOK)
            while ptr < tgt_ptr:
                loads[ptr]()
                ptr += 1
            f()
        P.barrier()
        A.release(m)

    def build(self):
        P = self.P
        self.setup()
        self.load_x()
        for li in range(self.n_layers):
            self.mod_phase(li)
            self.norm_phase(0, li)
            self.dbg(f"hT{li}", self.hT, self.t_h, [128, 8 * T], BF16)
            self.dbg(f"eff{li}", self.eff, self.t_eff, [128, 96])
            if self.stop == f"norm1_{li}":
                break
            self.spill_x()
            if "attn" not in self.skip:
                self.attn_phase(li)
            if self.stop in (f"attn_{li}", f"attnproj_{li}"):
                break
            if "conv" not in self.skip:
                self.conv_phase(li)
            if self.stop == f"conv_{li}":
                break
            if "rw" not in self.skip:
                self.rwkv_phase(li)
            if self.stop in (f"rw_{li}", f"rw1_{li}"):
                break
            self.restore_x()
            self.merge_phase(li)
            self.dbg(f"xmid{li}", self.xT, self.t_x, [128, 8 * T])
            if self.stop == f"merge_{li}":
                break
            self.moe_phase(li)
            self.dbg(f"xout{li}", self.xT, self.t_x, [128, 8 * T])
            if self.stop in (f"moe_{li}", f"moe1_{li}"):
                break
        if self.stop is None:
            self.final_phase()
        if self.dbg_br:
            t = Tok()
            d = self.nc.dram_tensor("dbg_brT", [3, 4, 128, T], BF16, kind="ExternalOutput").ap()
            P.dma("sp", I_dma(d, self.brT_d), r=self.t_br, w=[t])
            self.out_toks.append(t)
            self.dbg_out["brT"] = "dbg_brT"
        P.op("sp", None, r=self.out_toks)
        P.emit(self.nc, self.st)
        self.st.close()
        return self.nc


_CONSTS = None


def make_in_maps(inputs):
    global _CONSTS
    if _CONSTS is None:
        _CONSTS = make_consts()
    shared = {n: np.ascontiguousarray(np.asarray(inputs[n], dtype=np.float32)) for n, _ in WEIGHT_SPECS}
    for n in CONST_NAMES:
        shared["k_" + n] = _CONSTS[n]
    x = np.asarray(inputs["x"], dtype=np.float32)
    ctx = np.asarray(inputs["ctx"], dtype=np.float32)
    c = np.asarray(inputs["c"], dtype=np.float32)
    c_ctx = np.asarray(inputs["c_ctx"], dtype=np.float32)
    maps = []
    for b in range(8):
        m = dict(shared)
        m["x"] = np.ascontiguousarray(x[b])
        m["ctx"] = np.ascontiguousarray(ctx[b])
        m["c2"] = np.ascontiguousarray(np.stack([c[b], c_ctx], 0))
        maps.append(m)
    return maps


def kernel(**inputs):
    mk = MK()
    nc = mk.build()
    maps = make_in_maps(inputs)
    res = run_bass_kernel_spmd(nc, maps, core_ids=list(range(8)))
    return np.stack([np.asarray(res.results[b]["out"], dtype=np.float32) for b in range(8)], 0)
```

```python
import math
import numpy as np
from contextlib import ExitStack
import concourse.bass as bass
import concourse.mybir as mybir
from concourse.bass_utils import run_bass_kernel_spmd

F32 = mybir.dt.float32
BF16 = mybir.dt.bfloat16
AF = mybir.ActivationFunctionType
ALU = mybir.AluOpType
AX = mybir.AxisListType

D = 1024
TL = 2048
TC = 256
T = TL + TC
NT = T // 128
DEPTH = 2
IN_COLS = 7680
TGS = [(0, 512), (512, 512), (1024, 512), (1536, 512), (2048, 256)]

ENGS = ("pe", "act", "dve", "pool", "sp")
NDMASEM = 24


class Tok:
    __slots__ = ("w", "rs", "name")

    def __init__(self, name=""):
        self.w = None
        self.rs = []
        self.name = name


def toks(n):
    return [Tok() for _ in range(n)]


class Ev:
    __slots__ = ("kind", "eng", "idx", "sem", "target", "op")

    def __init__(self, kind, eng, idx, sem=None, target=None, op=None):
        self.kind, self.eng, self.idx, self.sem, self.target, self.op = kind, eng, idx, sem, target, op


class Op:
    __slots__ = ("eng", "fn", "dma", "idx", "waits", "flag", "tick", "dsem", "dtarget", "prewait")


class Prog:
    def __init__(self):
        self.ops = {e: [] for e in ENGS}
        self.ndma = {e: 0 for e in ENGS}
        self.seen = {e: {} for e in ENGS}
        self.last = {e: None for e in ENGS}
        self.pend_dma = {}

    def op(self, eng, fn, r=(), w=(), dma=False, extra=()):
        o = Op()
        o.eng, o.fn, o.dma = eng, fn, dma
        o.idx = len(self.ops[eng])
        o.flag = False
        o.tick = None
        o.prewait = None
        waits = {}

        def need(ev, force=False):
            if ev is None:
                return
            if ev.kind == "dma":
                k = ("dma", ev.eng, ev.sem)
                if waits.get(k, (0, None))[0] < ev.target:
                    waits[k] = (ev.target, ev)
            else:
                if ev.eng == eng and not dma and not force:
                    if eng == "pe":
                        return
                    if o.idx - ev.idx > 2:
                        return
                k = ("eng", ev.eng)
                if waits.get(k, (-1, None))[0] < ev.idx:
                    waits[k] = (ev.idx, ev)

        for t in r:
            need(t.w)
        for t in w:
            need(t.w)
            for ev in t.rs:
                need(ev)
        for ev in extra:
            if not (ev.kind == "eng" and ev.eng == eng):
                need(ev, force=True)
        seen = self.seen[eng]
        fw = {}
        for k, (v, ev) in waits.items():
            if seen.get(k, -1) >= v:
                continue
            seen[k] = v
            fw[k] = (v, ev)
            if ev.kind == "eng":
                ev.op.flag = True
        o.waits = fw
        if dma:
            n = self.ndma[eng]
            self.ndma[eng] = n + 1
            o.dsem = n % NDMASEM
            o.dtarget = 16 * (n // NDMASEM + 1)
            if n >= NDMASEM:
                o.prewait = (o.dsem, o.dtarget - 16)
            ev = Ev("dma", eng, o.idx, o.dsem, o.dtarget, o)
            self.pend_dma[(eng, o.dsem)] = ev
        else:
            ev = Ev("eng", eng, o.idx, op=o)
            if fn is not None:
                self.last[eng] = ev
        for t in r:
            t.rs.append(ev)
        for t in w:
            t.w = ev
            t.rs = []
        self.ops[eng].append(o)
        return o

    def pe(self, fn, r=(), w=()):
        return self.op("pe", fn, r, w)

    def act(self, fn, r=(), w=()):
        return self.op("act", fn, r, w)

    def dve(self, fn, r=(), w=()):
        return self.op("dve", fn, r, w)

    def pool(self, fn, r=(), w=()):
        return self.op("pool", fn, r, w)

    def dma(self, q, fn, r=(), w=()):
        return self.op(q, fn, r, w, dma=True)

    def barrier(self):
        evs = [ev for ev in self.last.values() if ev is not None] + list(self.pend_dma.values())
        self.pend_dma = {}
        for e in ENGS:
            self.op(e, None, extra=evs)

    def emit(self, nc, stack):
        for e in ENGS:
            t = 0
            for o in self.ops[e]:
                if o.flag and not o.dma:
                    t += 1
                    o.tick = t
        esem = {e: stack.enter_context(nc.semaphore(f"s_{e}")) for e in ENGS}
        dsem = {e: [stack.enter_context(nc.semaphore(f"d_{e}{i}")) for i in range(NDMASEM)]
                for e in ENGS if self.ndma[e] > 0}
        block = stack.enter_context(nc.Block())
        prog = self

        def runner(ename):
            def f(eng):
                for o in prog.ops[ename]:
                    for k, (v, ev) in o.waits.items():
                        if k[0] == "dma":
                            eng.wait_ge(dsem[k[1]][k[2]], v)
                        else:
                            eng.wait_ge(esem[k[1]], ev.op.tick)
                    if o.dma and o.prewait is not None:
                        eng.wait_ge(dsem[ename][o.prewait[0]], o.prewait[1])
                    if o.fn is None:
                        continue
                    ins = o.fn(eng)
                    if o.dma:
                        ins.then_inc(dsem[ename][o.dsem], 16)
                    elif o.flag:
                        ins.then_inc(esem[ename], 1)
            return f

        block.sync(runner("sp"))
        block.tensor(runner("pe"))
        block.scalar(runner("act"))
        block.vector(runner("dve"))
        block.gpsimd(runner("pool"))


class Arena:
    def __init__(self, ap, n):
        self.ap, self.n, self.off = ap, n, 0

    def alloc(self, n, dt=F32):
        nf = n if dt == F32 else (n + 1) // 2
        nf = (nf + 7) // 8 * 8
        assert self.off + nf <= self.n, ("arena overflow", self.off, nf, self.n)
        a = self.ap[:, self.off:self.off + nf]
        self.off += nf
        if dt != F32:
            a = a.bitcast(dt)[:, 0:n]
        else:
            a = a[:, 0:n]
        return a

    def mark(self):
        return self.off

    def release(self, m):
        self.off = m


class Ring:
    def __init__(self, bufs):
        self.bufs = bufs
        self.toks = toks(len(bufs))
        self.i = 0

    def next(self):
        j = self.i % len(self.bufs)
        self.i += 1
        return self.bufs[j], self.toks[j]


def make_consts():
    c = {}
    c["ident"] = np.eye(128, dtype=np.float32)
    c["ones"] = np.ones((128, 128), dtype=np.float32)
    pos_r = (np.arange(TL) // 64).astype(np.float32)
    pos_c = (np.arange(TL) % 64).astype(np.float32)
    inv = np.power(10000.0, -np.arange(16, dtype=np.float32) / 16).astype(np.float32)
    cosT = np.zeros((128, TL), np.float32)
    sinT = np.zeros((128, TL), np.float32)
    pm = np.zeros((128, 128), np.float32)
    for p in range(128):
        d = p % 64
        pos = pos_r if d < 32 else pos_c
        ang = pos * inv[d % 16]
        cosT[p] = np.cos(ang)
        sinT[p] = np.sin(ang)
        if (d % 32) < 16:
            pm[p + 16, p] = -1.0
        else:
            pm[p - 16, p] = 1.0
    c["cosT"], c["sinT"], c["pm"] = cosT, sinT, pm
    s = np.arange(128)[:, None]
    t = np.arange(128)[None, :]
    c["m_f_strict"] = (s < t).astype(np.float32)
    c["m_f_incl"] = (s <= t).astype(np.float32)
    c["m_r_strict"] = (s > t).astype(np.float32)
    c["m_r_incl"] = (s >= t).astype(np.float32)
    bd = np.zeros((128, 128), np.float32)
    bd[:64, :64] = 1
    bd[64:, 64:] = 1
    c["blockdiag"] = bd
    return c


CONST_NAMES = ["ident", "ones", "cosT", "sinT", "pm", "m_f_strict", "m_f_incl", "m_r_strict", "m_r_incl", "blockdiag"]

WEIGHT_SPECS = [
    ("w_mod", [DEPTH, D, 6 * D]), ("b_mod", [DEPTH, 6 * D]), ("norm1_g", [DEPTH, D]), ("norm2_g", [DEPTH, D]),
    ("w_in", [DEPTH, D, IN_COLS]), ("da_lambda", [DEPTH, 4, 64]), ("da_subln_g", [DEPTH, 128]),
    ("conv_w", [DEPTH, 3, 512]), ("rw_w0", [DEPTH, 2, 512]), ("rw_w1", [DEPTH, 2, D, 64]),
    ("rw_w2", [DEPTH, 2, 64, 512]), ("rw_a0", [DEPTH, 2, 512]), ("rw_a1", [DEPTH, 2, D, 64]),
    ("rw_a2", [DEPTH, 2, 64, 512]), ("rw_g1", [DEPTH, D, 160]), ("rw_g2", [DEPTH, 160, 512]),
    ("rw_k_k", [DEPTH, 512]), ("rw_k_a", [DEPTH, 512]), ("rw_r_k", [DEPTH, 8, 64]),
    ("rw_lnx_g", [DEPTH, 512]), ("rw_lnx_b", [DEPTH, 512]), ("w_branch", [DEPTH, 3, 512, D]),
    ("w_out", [DEPTH, D, D]), ("router_w", [DEPTH, D, 32]), ("router_b", [DEPTH, 32]),
    ("exp_w1", [DEPTH, 32, D, 2 * D]), ("exp_b1", [DEPTH, 32, 2 * D]), ("exp_w2", [DEPTH, 32, D, D]),
    ("exp_b2", [DEPTH, 32, D]), ("final_g", [D]),
]


def I_copy(o, i):
    return lambda e: e.tensor_copy(out=o, in_=i)


def I_acopy(o, i):
    return lambda e: e.activation(out=o, in_=i, func=AF.Copy)


def I_act(o, i, func, bias=None, scale=None):
    kw = {}
    if bias is not None:
        kw["bias"] = bias
    if scale is not None:
        kw["scale"] = scale
    return lambda e: e.activation(out=o, in_=i, func=func, **kw)


def I_mm(o, l, r, start=True, stop=True):
    return lambda e: e.matmul(o, lhsT=l, rhs=r, start=start, stop=stop)


def I_tr(o, i, ident):
    return lambda e: e.transpose(out=o, in_=i, identity=ident)


def I_tt(o, a, b, op):
    return lambda e: e.tensor_tensor(out=o, in0=a, in1=b, op=op)


def I_ts(o, a, s1, op0, s2=None, op1=None):
    if op1 is None:
        return lambda e: e.tensor_scalar(out=o, in0=a, scalar1=s1, scalar2=None, op0=op0)
    return lambda e: e.tensor_scalar(out=o, in0=a, scalar1=s1, scalar2=s2, op0=op0, op1=op1)


def I_stt(o, in0, scalar, in1, op0, op1):
    return lambda e: e.scalar_tensor_tensor(out=o, in0=in0, scalar=scalar, in1=in1, op0=op0, op1=op1)


def I_red(o, i, op=ALU.add):
    return lambda e: e.tensor_reduce(out=o, in_=i, axis=AX.X, op=op)


def I_dma(o, i):
    return lambda e: e.dma_start(out=o, in_=i)


def I_memset(o, v):
    return lambda e: e.memset(o, v)


ARENA_F32 = 52224


class MK:
    def __init__(self, n_layers=DEPTH, dbg=(), stop=None, skip=(), dbg_br=False):
        self.skip = set(skip)
        self.n_exp_dbg = 1
        self.dbg_br = dbg_br
        self.n_layers = n_layers
        self.dbg_names = set(dbg)
        self.stop = stop
        nc = bass.Bass("TRN2", target_bir_lowering=False)
        self.nc = nc
        self.st = ExitStack()
        self.P = Prog()
        self.x_d = nc.dram_tensor("x", [TL, D], F32, kind="ExternalInput").ap()
        self.ctx_d = nc.dram_tensor("ctx", [TC, D], F32, kind="ExternalInput").ap()
        self.c2_d = nc.dram_tensor("c2", [2, D], F32, kind="ExternalInput").ap()
        self.W = {n: nc.dram_tensor(n, s, F32, kind="ExternalInput").ap() for n, s in WEIGHT_SPECS}
        cs = make_consts()
        self.Cd = {n: nc.dram_tensor("k_" + n, list(cs[n].shape), F32, kind="ExternalInput").ap() for n in CONST_NAMES}
        self.out_d = nc.dram_tensor("out", [TL, D], F32, kind="ExternalOutput").ap()
        self.brT_d = nc.dram_tensor("brT", [3, 4, 128, T], BF16).ap()
        self.yscr_d = nc.dram_tensor("yscr", [2, NT, 128, 1024], F32).ap()
        self.gt_d = nc.dram_tensor("gtscr", [32, T], F32).ap()
        self.dbg_out = {}
        self.out_toks = []
        arena_t = self.st.enter_context(nc.sbuf_tensor("arena", [128, ARENA_F32], F32))
        self.A = Arena(arena_t[:], ARENA_F32)
        banks = [self.st.enter_context(nc.psum_tensor(f"psb{i}", [128, 512], F32))[:] for i in range(8)]
        self.PS = Ring(banks[0:4])
        self.PSacc = Ring(banks[4:8])
        self.PS8 = Ring(banks)
        self.PS8.toks = self.PS.toks + self.PSacc.toks
        self.t_br = toks(3)
        self.xscr_d = nc.dram_tensor("xscr", [128, 8 * T], F32).ap()

    def dbg(self, name, ap, tok, shape, dt=F32):
        if name not in self.dbg_names:
            return
        d = self.nc.dram_tensor("dbg_" + name, list(shape), dt, kind="ExternalOutput").ap()
        t = Tok()
        self.P.dma("sp", I_dma(d, ap), r=tok if isinstance(tok, (list, tuple)) else [tok], w=[t])
        self.out_toks.append(t)
        self.dbg_out[name] = "dbg_" + name

    def load_w_bf16(self, dst3, src2d, tok, kc=8):
        self.P.dma("pool", I_dma(dst3, src2d.rearrange("(k p) c -> p k c", p=128)), w=[tok])

    def rows_to_cols(self, dst, rows, n, tok_rows, tok_dst, scale=None):
        P = self.P
        ps, pt = self.PS.next()
        for j in range(n):
            P.pe(I_mm(ps[:, j:j + 1], rows[0:1, j * 128:(j + 1) * 128], self.ones[0:1, 0:1]), r=[tok_rows, self.t_c], w=[pt])
        P.dve(I_copy(dst, ps[:, 0:n]), r=[pt], w=[tok_dst])

    def bcast_rows(self, dst, row, n, tok_row, tok_dst, scale=None, ones=None):
        P = self.P
        ps, pt = self.PS.next()
        P.pe(I_mm(ps[:, 0:n], (self.ones if ones is None else ones)[0:1, 0:128], row[0:1, 0:n]), r=[tok_row, self.t_c], w=[pt])
        if scale is None:
            P.dve(I_copy(dst, ps[:, 0:n]), r=[pt], w=[tok_dst])
        else:
            P.dve(I_ts(dst, ps[:, 0:n], float(scale), ALU.mult), r=[pt], w=[tok_dst])

    def rsqrt(self, out, in_, mult, eps, r, tok):
        self.P.act(I_act(out, in_, AF.Ln, bias=self.epsc[:, self.eps_idx[eps]:self.eps_idx[eps] + 1][0:out.shape[0]], scale=float(mult)), r=list(r) + [self.t_c], w=[tok])
        self.P.act(I_act(out, out, AF.Exp, scale=-0.5), r=[tok], w=[tok])

    def tgs(self, li):
        return TGS if li < self.n_layers - 1 else TGS[:4]

    def AL(self, n, dt=F32):
        try:
            return self.B.alloc(n, dt)
        except AssertionError:
            return self.A.alloc(n, dt)

    def spill_x(self):
        P = self.P
        self.t_xs = Tok()
        for k in range(8):
            P.dma("sp" if k % 2 == 0 else "act", I_dma(self.xscr_d[:, k * T:(k + 1) * T], self.xT[:, k * T:(k + 1) * T]), r=self.t_x, w=[self.t_xs])
        P.barrier()
        self.B = Arena(self.xT, 8 * T)

    def restore_x(self):
        P = self.P
        P.barrier()
        for k in range(8):
            P.dma("sp" if k % 2 == 0 else "act", I_dma(self.xT[:, k * T:(k + 1) * T], self.xscr_d[:, k * T:(k + 1) * T]), r=[self.t_xs], w=self.t_x)
        P.barrier()

    def setup(self):
        P, A = self.P, self.A
        self.t_c = Tok()
        self.ident = A.alloc(128)
        self.ones = A.alloc(128)
        self.ident_b = A.alloc(128, BF16)
        self.ones_b = A.alloc(128, BF16)
        P.dma("sp", I_dma(self.ident, self.Cd["ident"]), w=[self.t_c])
        P.dma("sp", I_dma(self.ones, self.Cd["ones"]), w=[self.t_c])
        P.dma("pool", I_dma(self.ident_b, self.Cd["ident"]), w=[self.t_c])
        P.dma("pool", I_dma(self.ones_b, self.Cd["ones"]), w=[self.t_c])
        self.epsc = A.alloc(8)
        self.eps_idx = {1e-6: 0, 1e-5: 1, 64e-5: 2, 1e-24: 3}
        for v, i in self.eps_idx.items():
            P.dve(I_memset(self.epsc[:, i:i + 1], v), w=[self.t_c])
        self.xT = A.alloc(8 * T)
        self.xT3 = self.xT.rearrange("p (k t) -> p k t", k=8)
        self.t_x = toks(5)
        self.hT = A.alloc(8 * T, BF16)
        self.hT3 = self.hT.rearrange("p (k t) -> p k t", k=8)
        self.t_h = toks(5)
        self.csT = A.alloc(16)
        self.t_cs = Tok()
        self.eff = A.alloc(96)
        self.eff4 = self.eff.rearrange("p (n k c) -> p n k c", n=6, k=8)
        self.t_eff = Tok()
        self.gvec = A.alloc(24)
        self.t_gvec = Tok()

    def load_x(self):
        P, A = self.P, self.A
        m = A.mark()
        stage = Ring([A.alloc(1024) for _ in range(2)])
        for i in range(NT):
            sb, stt = stage.next()
            src = self.x_d[i * 128:(i + 1) * 128, :] if i < 16 else self.ctx_d[(i - 16) * 128:(i - 15) * 128, :]
            P.dma("sp", I_dma(sb, src), w=[stt])
            for half in range(2):
                ps, pt = self.PS.next()
                for j in range(4):
                    k = half * 4 + j
                    P.pe(I_tr(ps[:, j * 128:(j + 1) * 128], sb[:, k * 128:(k + 1) * 128], self.ident), r=[stt, self.t_c], w=[pt])
                fn = I_copy(self.xT3[:, half * 4:half * 4 + 4, i * 128:(i + 1) * 128], ps.rearrange("p (a b) -> p a b", a=4))
                P.dve(fn, r=[pt], w=[self.t_x[min(i // 4, 4)]])
        c2s = A.alloc(1024)
        tcs = Tok()
        P.dma("sp", I_dma(c2s[0:2, :], self.c2_d), w=[tcs])
        P.act(I_act(c2s[0:2, :], c2s[0:2, :], AF.Silu), r=[tcs], w=[tcs])
        ps, pt = self.PS.next()
        for k in range(8):
            P.pe(I_tr(ps[:, k * 2:k * 2 + 2], c2s[0:2, k * 128:(k + 1) * 128], self.ident[0:2, 0:2]), r=[tcs, self.t_c], w=[pt])
        P.dve(I_copy(self.csT, ps[:, 0:16]), r=[pt], w=[self.t_cs])
        P.barrier()
        A.release(m)

    def mod_phase(self, li):
        P, A = self.P, self.A
        m = A.mark()
        brow = A.alloc(6 * D)
        grow = A.alloc(3 * D)
        tb, tg = Tok(), Tok()
        P.dma("sp", I_dma(brow[0:1, :], self.W["b_mod"][li:li + 1, :]), w=[tb])
        P.dma("sp", I_dma(grow[0:1, 0:D], self.W["norm1_g"][li:li + 1, :]), w=[tg])
        P.dma("sp", I_dma(grow[0:1, D:2 * D], self.W["norm2_g"][li:li + 1, :]), w=[tg])
        P.dma("sp", I_dma(grow[0:1, 2 * D:3 * D], self.W["final_g"].rearrange("(o d) -> o d", o=1)), w=[tg])
        self.rows_to_cols(self.gvec, grow, 24, tg, self.t_gvec)
        wring = Ring([A.alloc(8 * 512) for _ in range(2)])
        psm, ptm = self.PS.next()
        for piece in range(12):
            wp, wt = wring.next()
            wp3 = wp.rearrange("p (k c) -> p k c", k=8)
            src = self.W["w_mod"][li][:, piece * 512:(piece + 1) * 512].rearrange("(k p) c -> p k c", p=128)
            P.dma("sp" if piece % 2 == 0 else "act", I_dma(wp3, src), w=[wt])
            for jj in range(4):
                j = piece * 4 + jj
                for kc in range(8):
                    P.pe(I_mm(psm[:, j * 2:j * 2 + 2], wp3[:, kc, jj * 128:(jj + 1) * 128], self.csT[:, kc * 2:kc * 2 + 2],
                              start=(kc == 0), stop=False), r=[wt, self.t_cs], w=[ptm])
                P.pe(I_mm(psm[:, j * 2:j * 2 + 2], brow[0:1, j * 128:(j + 1) * 128], self.ones[0:1, 0:2], start=False, stop=True),
                     r=[tb, self.t_c], w=[ptm])
        P.dve(I_copy(self.eff, psm[:, 0:96]), r=[ptm], w=[self.t_eff])
        for n, gi in ((1, 0), (4, 1)):
            P.dve(I_stt(self.eff4[:, n], self.eff4[:, n], 1.0,
                        self.gvec[:, gi * 8:(gi + 1) * 8].unsqueeze(2).to_broadcast([128, 8, 2]), ALU.add, ALU.mult),
                  r=[self.t_eff, self.t_gvec], w=[self.t_eff])
        P.barrier()
        A.release(m)

    def norm_phase(self, which, li, router=None):
        P, A = self.P, self.A
        m = A.mark()
        n_sh, n_sc = (0, 1) if which == 0 else (3, 4)
        sq = A.alloc(8 * 512)
        sq3 = sq.rearrange("p (k t) -> p k t", k=8)
        tsq = Tok()
        rstd_r = Ring([A.alloc(512) for _ in range(2)])
        tmp_r = Ring([A.alloc(512) for _ in range(3)])
        if router is not None:
            h2f = A.alloc(8 * 512)
            h2f3 = h2f.rearrange("p (k t) -> p k t", k=8)
            th2f = Tok()
        for tg, (s, n) in enumerate(TGS if which == 0 else self.tgs(li)):
            col = 0 if tg < 4 else 1
            P.act(I_act(sq3[:, :, 0:n], self.xT3[:, :, s:s + n], AF.Square), r=[self.t_x[tg]], w=[tsq])
            ps, pt = self.PS.next()
            for k in range(8):
                P.pe(I_mm(ps[:, 0:n], self.ones, sq3[:, k, 0:n], start=(k == 0), stop=(k == 7)), r=[tsq, self.t_c], w=[pt])
            rstd, trs = rstd_r.next()
            self.rsqrt(rstd[:, 0:n], ps[:, 0:n], 1.0 / D, 1e-6, [pt], trs)
            for k in range(8):
                tmp, tt = tmp_r.next()
                P.op("dve",
                     I_stt(tmp[:, 0:n], self.xT3[:, k, s:s + n], self.eff4[:, n_sc, k, col:col + 1], rstd[:, 0:n], ALU.mult, ALU.mult),
                     r=[self.t_x[tg], self.t_eff, trs], w=[tt])
                if router is None:
                    P.act(I_act(self.hT3[:, k, s:s + n], tmp[:, 0:n], AF.Identity, bias=self.eff4[:, n_sh, k, col:col + 1]),
                          r=[tt, self.t_eff], w=[self.t_h[tg]])
                else:
                    P.act(I_act(h2f3[:, k, 0:n], tmp[:, 0:n], AF.Identity, bias=self.eff4[:, n_sh, k, col:col + 1]),
                          r=[tt, self.t_eff], w=[th2f])
                    P.pool(I_copy(self.hT3[:, k, s:s + n], h2f3[:, k, 0:n]), r=[th2f], w=[self.t_h[tg]])
            if router is not None:
                router(tg, s, n, h2f3, th2f)
        P.barrier()
        A.release(m)


    def attn_phase(self, li):
        P, A = self.P, self.A
        m = A.mark()
        self.B.off = 0
        lam_init = 0.8 - 0.6 * math.exp(-0.3 * li)
        W = self.W["w_in"][li]
        wq = self.AL(8 * 512, BF16).rearrange("p (k c) -> p k c", k=8)
        wk = self.AL(8 * 512, BF16).rearrange("p (k c) -> p k c", k=8)
        wv = self.AL(8 * 512, BF16).rearrange("p (k c) -> p k c", k=8)
        tw = toks(3)
        self.load_w_bf16(wq, W[:, 0:512], tw[0])
        self.load_w_bf16(wk, W[:, 512:1024], tw[1])
        self.load_w_bf16(wv, W[:, 1024:1536], tw[2])
        cosT = self.AL(TL)
        sinT = self.AL(TL)
        pm = self.AL(128, BF16)
        trope = Tok()
        P.dma("sp", I_dma(cosT, self.Cd["cosT"]), w=[trope])
        P.dma("act", I_dma(sinT, self.Cd["sinT"]), w=[trope])
        P.dma("pool", I_dma(pm, self.Cd["pm"]), w=[trope])
        qT = self.AL(4 * T, BF16).rearrange("p (h t) -> p h t", h=4)
        kT = self.AL(4 * T, BF16).rearrange("p (h t) -> p h t", h=4)
        vaug = self.AL(NT * 4 * 130, BF16).rearrange("p (i h e) -> p i h e", i=NT, h=4)
        t_q, t_k, t_v = toks(5), toks(5), toks(NT)
        tv1 = Tok()
        P.pool(I_memset(vaug[:, :, :, 128:129], 1.0), w=t_v)
        rows = self.AL(512)
        trow = Tok()
        P.dma("sp", I_dma(rows[0:1, 0:256], self.W["da_lambda"][li:li + 1].rearrange("o a b -> o (a b)")), w=[trow])
        P.dma("sp", I_dma(rows[0:1, 256:384], self.W["da_subln_g"][li:li + 1, :]), w=[trow])
        lsm = self.AL(16)
        tl = Tok()
        P.dve(I_tt(rows[0:1, 384:448], rows[0:1, 0:64], rows[0:1, 64:128], ALU.mult), r=[trow], w=[trow])
        P.dve(I_tt(rows[0:1, 448:512], rows[0:1, 128:192], rows[0:1, 192:256], ALU.mult), r=[trow], w=[trow])
        P.dve(I_red(lsm[0:1, 0:2], rows[0:1, 384:512].rearrange("o (a b) -> o a b", a=2)), r=[trow], w=[tl])
        P.act(I_act(lsm[0:1, 2:4], lsm[0:1, 0:2], AF.Exp), r=[tl], w=[tl])
        P.dve(I_tt(lsm[0:1, 4:5], lsm[0:1, 3:4], lsm[0:1, 2:3], ALU.subtract), r=[tl], w=[tl])
        P.dve(I_ts(lsm[0:1, 4:5], lsm[0:1, 4:5], -lam_init, ALU.add), r=[tl], w=[tl])
        neglam = self.AL(8)
        gsub = self.AL(128)
        tng = Tok()
        self.bcast_rows(neglam[:, 0:1], lsm[:, 4:5], 1, tl, tng)
        self.bcast_rows(gsub, rows[:, 256:384], 128, trow, tng, scale=(1.0 - lam_init))
        raw_r = Ring([self.AL(512, BF16) for _ in range(2)])
        t1_r = Ring([self.AL(512) for _ in range(2)])
        t2_r = Ring([self.AL(512) for _ in range(2)])
        for (w3, twk, dstT, tdst) in ((wq, tw[0], qT, t_q), (wk, tw[1], kT, t_k)):
            for h in range(4):
                for tg, (s, n) in enumerate(TGS):
                    ps, pt = self.PS.next()
                    for kc in range(8):
                        P.pe(I_mm(ps[:, 0:n], w3[:, kc, h * 128:(h + 1) * 128], self.hT3[:, kc, s:s + n], start=(kc == 0), stop=(kc == 7)),
                             r=[twk, self.t_h[tg]], w=[pt])
                    if tg == 4:
                        P.act(I_acopy(dstT[:, h, s:s + n], ps[:, 0:n]), r=[pt], w=[tdst[tg]])
                        continue
                    raw, tr = raw_r.next()
                    P.act(I_acopy(raw[:, 0:n], ps[:, 0:n]), r=[pt], w=[tr])
                    ps2, pt2 = self.PS.next()
                    P.pe(I_mm(ps2[:, 0:n], pm, raw[:, 0:n]), r=[trope, tr], w=[pt2])
                    a1, ta1 = t1_r.next()
                    a2, ta2 = t2_r.next()
                    P.pool(I_tt(a1[:, 0:n], raw[:, 0:n], cosT[:, s:s + n], ALU.mult), r=[tr, trope], w=[ta1])
                    P.dve(I_tt(a2[:, 0:n], ps2[:, 0:n], sinT[:, s:s + n], ALU.mult), r=[pt2, trope], w=[ta2])
                    P.pool(I_tt(dstT[:, h, s:s + n], a1[:, 0:n], a2[:, 0:n], ALU.add), r=[ta1, ta2], w=[tdst[tg]])
        for i in range(NT):
            ps, pt = self.PS.next()
            for kc in range(8):
                P.pe(I_mm(ps, self.hT3[:, kc, i * 128:(i + 1) * 128], wv[:, kc, :], start=(kc == 0), stop=(kc == 7)),
                     r=[tw[2], self.t_h[min(i // 4, 4)]], w=[pt])
            P.act(I_acopy(vaug[:, i, :, 0:128], ps.rearrange("p (h e) -> p h e", h=4)), r=[pt], w=[t_v[i]])
        self.dbg("qT", qT.rearrange("p h t -> p (h t)"), t_q, [128, 4 * T], BF16)
        self.dbg("kT", kT.rearrange("p h t -> p (h t)"), t_k, [128, 4 * T], BF16)
        if self.stop == f"attnproj_{li}":
            P.barrier()
            A.release(m)
            return
        Ea_r = Ring([self.AL(NT * 256, BF16) for _ in range(2)])
        osb = [self.AL(4 * 130) for _ in range(2)]
        tosb = toks(2)
        oda = self.AL(4 * 512)
        oda3 = oda.rearrange("p (j c) -> p j c", j=4)
        toda = Tok()
        sm_r = Ring([self.AL(8) for _ in range(2)])
        o1_r = Ring([self.AL(128) for _ in range(2)])
        o2_r = Ring([self.AL(128) for _ in range(2)])
        stg_r = Ring([self.AL(512, BF16) for _ in range(2)])
        groups = [(s, 256, list(range(NT)), s // 512) for s in range(0, TL, 256)] + [(2048, 256, [16, 17], 4)]
        for (qs0, qn, ktiles, tg) in groups:
            nq = qn // 128
            for h in range(4):
                for sm in range(2):
                    R_ = slice(sm * 64, (sm + 1) * 64)
                    Ea, tea = Ea_r.next()
                    Ea3 = Ea.rearrange("p (k q) -> p k q", k=NT)
                    for idx, kt in enumerate(ktiles):
                        ps, pt = self.PS.next()
                        P.pe(I_mm(ps[:, 0:qn], kT[R_, h, kt * 128:(kt + 1) * 128], qT[R_, h, qs0:qs0 + qn]),
                             r=[t_k[min(kt // 4, 4)], t_q[tg]], w=[pt])
                        P.act(I_act(Ea3[:, idx, 0:qn], ps[:, 0:qn], AF.Exp, scale=0.125), r=[pt], w=[tea])
                    for j in range(nq):
                        ob, tob = self.PSacc.next()
                        for idx, kt in enumerate(ktiles):
                            P.pe(I_mm(ob[:, 0:129], Ea3[:, idx, j * 128:(j + 1) * 128], vaug[:, kt, h, 0:129],
                                      start=(idx == 0), stop=(idx == len(ktiles) - 1)), r=[tea, t_v[kt]], w=[tob])
                        P.dve(I_copy(osb[sm][:, j * 130:j * 130 + 129], ob[:, 0:129]), r=[tob], w=[tosb[sm]])
                for j in range(nq):
                    s8, ts8 = sm_r.next()
                    o1, to1 = o1_r.next()
                    o2, to2 = o2_r.next()
                    P.dve(lambda e, o=s8[:, 0:1], i=osb[0][:, j * 130 + 128:j * 130 + 129]: e.reciprocal(out=o, in_=i), r=[tosb[0]], w=[ts8])
                    P.dve(lambda e, o=s8[:, 1:2], i=osb[1][:, j * 130 + 128:j * 130 + 129]: e.reciprocal(out=o, in_=i), r=[tosb[1]], w=[ts8])
                    P.dve(I_tt(s8[:, 2:3], s8[:, 1:2], neglam[:, 0:1], ALU.mult), r=[ts8, tng], w=[ts8])
                    P.dve(I_ts(o1, osb[0][:, j * 130:j * 130 + 128], s8[:, 0:1], ALU.mult), r=[tosb[0], ts8], w=[to1])
                    P.dve(I_stt(o1, osb[1][:, j * 130:j * 130 + 128], s8[:, 2:3], o1, ALU.mult, ALU.add), r=[tosb[1], ts8, to1], w=[to1])
                    P.pool(I_tt(o2, o1, o1, ALU.mult), r=[to1], w=[to2])
                    P.dve(I_red(s8[:, 3:4], o2), r=[to2], w=[ts8])
                    self.rsqrt(s8[:, 4:5], s8[:, 3:4], 1.0 / 128, 1e-5, [ts8], ts8)
                    P.dve(I_stt(oda3[:, j, h * 128:(h + 1) * 128], o1, s8[:, 4:5], gsub, ALU.mult, ALU.mult), r=[to1, ts8, tng], w=[toda])
            for j in range(nq):
                ti = qs0 // 128 + j
                ps, pt = self.PS.next()
                for h in range(4):
                    P.pe(I_tr(ps[:, h * 128:(h + 1) * 128], oda3[:, j, h * 128:(h + 1) * 128], self.ident), r=[toda, self.t_c], w=[pt])
                stg, tst = stg_r.next()
                P.act(I_acopy(stg, ps), r=[pt], w=[tst])
                P.dma("sp", I_dma(self.brT_d[0, :, :, ti * 128:(ti + 1) * 128].rearrange("k p t -> p k t"),
                                  stg.rearrange("p (k t) -> p k t", k=4)), r=[tst], w=[self.t_br[0]])
        P.barrier()
        A.release(m)

    def conv_phase(self, li):
        P, A = self.P, self.A
        m = A.mark()
        self.B.off = 0
        W = self.W["w_in"][li]
        ws = [self.AL(8 * 512, BF16).rearrange("p (k c) -> p k c", k=8) for _ in range(3)]
        tw = toks(3)
        for i in range(3):
            self.load_w_bf16(ws[i], W[:, 1536 + i * 512:2048 + i * 512], tw[i])
        rows = self.AL(1536)
        trow = Tok()
        P.dma("sp", I_dma(rows[0:1, :], self.W["conv_w"][li:li + 1].rearrange("o a b -> o (a b)")), w=[trow])
        cw = self.AL(16)
        tcw = Tok()
        self.rows_to_cols(cw[:, 0:12], rows, 12, trow, tcw)
        ubuf = self.AL(2312)
        tub = Tok()
        P.pool(I_memset(ubuf, 0.0), w=[tub])
        sbT = self.AL(T, BF16)
        tsb = Tok()
        sg_r = Ring([self.AL(512) for _ in range(2)])
        c1 = self.AL(TL)
        tc1 = Tok()
        osc = self.AL(T, BF16)
        tosc = Tok()
        for ch in range(4):
            for tg, (s, n) in enumerate(TGS):
                pss = [self.PS.next() for _ in range(3)]
                for i in range(3):
                    for kc in range(8):
                        P.pe(I_mm(pss[i][0][:, 0:n], ws[i][:, kc, ch * 128:(ch + 1) * 128], self.hT3[:, kc, s:s + n],
                                  start=(kc == 0), stop=(kc == 7)), r=[tw[i], self.t_h[tg]], w=[pss[i][1]])
                off = 1 + s if tg < 4 else 2051
                sg, tsg = sg_r.next()
                P.act(I_acopy(sbT[:, s:s + n], pss[0][0][:, 0:n]), r=[pss[0][1]], w=[tsb])
                P.act(I_acopy(sg[:, 0:n], pss[1][0][:, 0:n]), r=[pss[1][1]], w=[tsg])
                P.dve(I_tt(ubuf[:, off:off + n], pss[2][0][:, 0:n], sg[:, 0:n], ALU.mult), r=[pss[2][1], tsg], w=[tub])
            for (o0, u0, n) in ((0, 0, TL), (TL, 2050, TC)):
                P.dve(I_ts(c1[:, 0:n], ubuf[:, u0:u0 + n], cw[:, ch:ch + 1], ALU.mult), r=[tub, tcw], w=[tc1])
                P.dve(I_stt(c1[:, 0:n], ubuf[:, u0 + 1:u0 + 1 + n], cw[:, 4 + ch:5 + ch], c1[:, 0:n], ALU.mult, ALU.add), r=[tub, tcw], w=[tc1])
                P.dve(I_stt(c1[:, 0:n], ubuf[:, u0 + 2:u0 + 2 + n], cw[:, 8 + ch:9 + ch], c1[:, 0:n], ALU.mult, ALU.add), r=[tub, tcw], w=[tc1])
                P.pool(I_tt(osc[:, o0:o0 + n], c1[:, 0:n], sbT[:, o0:o0 + n], ALU.mult), r=[tc1, tsb], w=[tosc])
            P.dma("sp", I_dma(self.brT_d[1, ch], osc), r=[tosc], w=[self.t_br[1]])
        P.barrier()
        A.release(m)

    def merge_phase(self, li):
        P, A = self.P, self.A
        m = A.mark()
        W = self.W["w_in"][li]
        wg = A.alloc(8 * 1024, BF16).rearrange("p (k c) -> p k c", k=8)
        wb = A.alloc(4 * 1024, BF16).rearrange("p (k c) -> p k c", k=4)
        twg, twb = Tok(), Tok()
        mg = A.alloc(8 * T, BF16).rearrange("p (k t) -> p k t", k=8)
        tmg = toks(5)
        br_r = Ring([A.alloc(4 * 512, BF16) for _ in range(2)])
        gs_r = Ring([A.alloc(512) for _ in range(2)])
        for n_ in range(3):
            self.load_w_bf16(wg, W[:, 4608 + n_ * 1024:4608 + (n_ + 1) * 1024], twg)
            self.load_w_bf16(wb, self.W["w_branch"][li, n_], twb, kc=4)
            for tg, (s, n) in enumerate(self.tgs(li)):
                br, tbr = br_r.next()
                br3 = br.rearrange("p (k t) -> p k t", k=4)
                P.dma("sp", I_dma(br3[:, :, 0:n], self.brT_d[n_, :, :, s:s + n].rearrange("k p t -> p k t")), r=[self.t_br[n_]], w=[tbr])
                for cc in range(8):
                    psg, ptg = self.PS.next()
                    for kc in range(8):
                        P.pe(I_mm(psg[:, 0:n], wg[:, kc, cc * 128:(cc + 1) * 128], self.hT3[:, kc, s:s + n], start=(kc == 0), stop=(kc == 7)),
                             r=[twg, self.t_h[tg]], w=[ptg])
                    gs, tgs = gs_r.next()
                    P.act(I_act(gs[:, 0:n], psg[:, 0:n], AF.Sigmoid), r=[ptg], w=[tgs])
                    psp, ptp = self.PS.next()
                    for kc in range(4):
                        P.pe(I_mm(psp[:, 0:n], wb[:, kc, cc * 128:(cc + 1) * 128], br3[:, kc, 0:n], start=(kc == 0), stop=(kc == 3)),
                             r=[twb, tbr], w=[ptp])
                    if n_ == 0:
                        P.dve(I_tt(mg[:, cc, s:s + n], psp[:, 0:n], gs[:, 0:n], ALU.mult), r=[ptp, tgs], w=[tmg[tg]])
                    else:
                        P.dve(I_tt(gs[:, 0:n], psp[:, 0:n], gs[:, 0:n], ALU.mult), r=[ptp, tgs], w=[tgs])
                        P.pool(I_tt(mg[:, cc, s:s + n], mg[:, cc, s:s + n], gs[:, 0:n], ALU.add), r=[tmg[tg], tgs], w=[tmg[tg]])
        wo = wg
        self.load_w_bf16(wo, self.W["w_out"][li], twg)
        for tg, (s, n) in enumerate(self.tgs(li)):
            col = 0 if tg < 4 else 1
            for cc in range(8):
                ps, pt = self.PS.next()
                for kc in range(8):
                    P.pe(I_mm(ps[:, 0:n], wo[:, kc, cc * 128:(cc + 1) * 128], mg[:, kc, s:s + n], start=(kc == 0), stop=(kc == 7)),
                         r=[twg, tmg[tg]], w=[pt])
                P.dve(I_stt(self.xT3[:, cc, s:s + n], ps[:, 0:n], self.eff4[:, 2, cc, col:col + 1], self.xT3[:, cc, s:s + n], ALU.mult, ALU.add),
                      r=[pt, self.t_eff, self.t_x[tg]], w=[self.t_x[tg]])
        P.barrier()
        A.release(m)

    def final_phase(self):
        P, A = self.P, self.A
        m = A.mark()
        sq = A.alloc(8 * 512)
        sq3 = sq.rearrange("p (k t) -> p k t", k=8)
        tsq = Tok()
        rstd_r = Ring([A.alloc(512) for _ in range(2)])
        y_r = Ring([A.alloc(8 * 512) for _ in range(2)])
        o_r = Ring([A.alloc(1024) for _ in range(3)])
        for tg, (s, n) in enumerate(TGS[:4]):
            P.act(I_act(sq3[:, :, 0:n], self.xT3[:, :, s:s + n], AF.Square), r=[self.t_x[tg]], w=[tsq])
            ps, pt = self.PS.next()
            for k in range(8):
                P.pe(I_mm(ps[:, 0:n], self.ones, sq3[:, k, 0:n], start=(k == 0), stop=(k == 7)), r=[tsq, self.t_c], w=[pt])
            rstd, trs = rstd_r.next()
            self.rsqrt(rstd[:, 0:n], ps[:, 0:n], 1.0 / D, 1e-6, [pt], trs)
            y, ty = y_r.next()
            y3 = y.rearrange("p (k t) -> p k t", k=8)
            for k in range(8):
                P.dve(I_stt(y3[:, k, 0:n], self.xT3[:, k, s:s + n], self.gvec[:, 16 + k:17 + k], rstd[:, 0:n], ALU.mult, ALU.mult),
                      r=[self.t_x[tg], self.t_gvec, trs], w=[ty])
            for j in range(n // 128):
                ti = s // 128 + j
                ob, tob = o_r.next()
                for half in range(2):
                    ps, pt = self.PS.next()
                    for q in range(4):
                        k = half * 4 + q
                        P.pe(I_tr(ps[:, q * 128:(q + 1) * 128], y3[:, k, j * 128:(j + 1) * 128], self.ident), r=[ty, self.t_c], w=[pt])
                    P.act(I_acopy(ob[:, half * 512:(half + 1) * 512], ps), r=[pt], w=[tob])
                to = Tok()
                P.dma("sp", I_dma(self.out_d[ti * 128:(ti + 1) * 128, :], ob), r=[tob], w=[to])
                self.out_toks.append(to)
        P.barrier()
        A.release(m)

    def rwkv_phase(self, li):
        P, A = self.P, self.A
        m = A.mark()
        W = self.W["w_in"][li]
        DS = 0.606531
        self.B.off = 0
        B = self.B
        AL = self.AL
        a512 = lambda dt=F32: AL(512, dt)
        wrkv = [A.alloc(8 * 512, BF16).rearrange("p (k c) -> p k c", k=8) for _ in range(3)]
        tw = toks(3)
        for i in range(3):
            self.load_w_bf16(wrkv[i], W[:, 3072 + i * 512:3584 + i * 512], tw[i])
        lo1 = A.alloc(8 * 416, BF16).rearrange("p (k c) -> p k c", k=8)
        tlo = Tok()
        for d in range(2):
            self.load_w_bf16(lo1[:, :, d * 64:(d + 1) * 64], self.W["rw_w1"][li, d], tlo)
            self.load_w_bf16(lo1[:, :, 128 + d * 64:128 + (d + 1) * 64], self.W["rw_a1"][li, d], tlo)
        self.load_w_bf16(lo1[:, :, 256:416], self.W["rw_g1"][li], tlo)
        lo2 = A.alloc(6 * 512, BF16).rearrange("p (k c) -> p k c", k=6)
        for d in range(2):
            P.dma("pool", I_dma(lo2[0:64, d, :], self.W["rw_w2"][li, d]), w=[tlo])
            P.dma("pool", I_dma(lo2[0:64, 2 + d, :], self.W["rw_a2"][li, d]), w=[tlo])
        P.dma("pool", I_dma(lo2[:, 4, :], self.W["rw_g2"][li, 0:128, :]), w=[tlo])
        P.dma("pool", I_dma(lo2[0:32, 5, :], self.W["rw_g2"][li, 128:160, :]), w=[tlo])
        brow = A.alloc(4 * 512, BF16)
        P.dma("pool", I_dma(brow[0:1, 0:1024], self.W["rw_w0"][li:li + 1].rearrange("o a b -> o (a b)")), w=[tlo])
        P.dma("pool", I_dma(brow[0:1, 1024:2048], self.W["rw_a0"][li:li + 1].rearrange("o a b -> o (a b)")), w=[tlo])
        rows = A.alloc(5 * 512, BF16)
        trow = Tok()
        for i, nm in enumerate(["rw_k_k", "rw_k_a", "rw_lnx_g", "rw_lnx_b"]):
            P.dma("pool", I_dma(rows[0:1, i * 512:(i + 1) * 512], self.W[nm][li:li + 1, :]), w=[trow])
        P.dma("pool", I_dma(rows[0:1, 2048:2560], self.W["rw_r_k"][li:li + 1].rearrange("o a b -> o (a b)")), w=[trow])
        bc = [A.alloc(512) for _ in range(5)]
        tbc = Tok()
        for i in range(5):
            self.bcast_rows(bc[i], rows[:, i * 512:(i + 1) * 512], 512, trow, tbc, ones=self.ones_b)
        kk_bc, ka_bc, lg_bc, lb_bc, rk_bc = bc
        A.release(A.mark())
        msk = {}
        tmk = Tok()
        for d, (nS, nI, nST) in enumerate((("m_f_strict", "m_f_incl", "m_r_strict"), ("m_r_strict", "m_r_incl", "m_f_strict"))):
            mc1, mc2, mn = A.alloc(256, BF16), A.alloc(256, BF16), A.alloc(128, BF16)
            tri = A.alloc(128)
            P.dma("pool", I_dma(mc2[:, 0:128], self.Cd[nS]), w=[tmk])
            P.dma("pool", I_dma(mc2[:, 128:256], self.Cd[nI]), w=[tmk])
            P.dma("pool", I_dma(mn, self.Cd[nST]), w=[tmk])
            P.dma("sp", I_dma(tri, self.Cd[nI]), w=[tmk])
            P.dve(I_ts(mc1[:, 0:128], mc2[:, 0:128], -1.0, ALU.mult), r=[tmk], w=[tmk])
            P.dve(I_copy(mc1[:, 128:256], mc2[:, 128:256]), r=[tmk], w=[tmk])
            P.dve(I_ts(mn, mn, -1.0, ALU.mult), r=[tmk], w=[tmk])
            P.dve(I_ts(tri, tri, -DS, ALU.mult), r=[tmk], w=[tmk])
            msk[d] = (mc1, mc2, mn, tri)
        allneg = A.alloc(128)
        P.dve(I_ts(allneg, self.ones, -DS, ALU.mult), r=[self.t_c], w=[tmk])
        Mst = [[A.alloc(512) for _ in range(2)] for _ in range(2)]
        tM = [toks(2) for _ in range(2)]
        for d in range(2):
            P.pool(I_memset(Mst[d][0], 0.0), w=[tM[d][0]])
        mpar = [0, 0]
        r_s, k_s, v_s, sig, a_s, kx, kk, b_s, kE, tA, tB, cum_s, cumC_s = [a512() for _ in range(13)]
        tk_ = {n: Tok() for n in "r k v sig a kx kk b kE tA tB cum cumC l1 la e1 e2 e3 e4 kt bt k2 rt kh bh vb ktb rtb fT pc dg".split()}
        l1 = AL(128, BF16)
        la = AL(128, BF16)
        e1, e2, e3, e4 = [a512(BF16) for _ in range(4)]
        kt_, bt_, k2_, rt_ = [a512(BF16) for _ in range(4)]
        kh_, bh_, vb_ = [a512(BF16) for _ in range(3)]
        red8 = A.alloc(16)
        tred = Tok()
        pcb = a512()
        dg = a512()
        fT = [AL(4 * 128, BF16).rearrange("p (q t) -> p q t", q=4) for _ in range(4)]
        tfT = toks(4)
        hb = []
        for h in range(8):
            d_ = {}
            for nm, sz in (("X0", 256), ("X1", 256), ("P0", 128), ("P1", 128), ("A1", 256), ("A2", 256), ("KAV", 128), ("WU", 128)):
                d_[nm] = AL(sz, BF16)
            d_["t"] = {nm: Tok() for nm in ("X0", "X1", "P0", "P1", "A1", "A2", "KAV", "WU")}
            hb.append(d_)
        PhiT = AL(512)
        Gall = AL(512)
        QT = AL(8 * 128).rearrange("p (h t) -> p h t", h=8)
        Y0s = a512()
        yout = AL(1024)
        tPhi, tG, tQT, tY0, tyo = Tok(), Tok(), Tok(), Tok(), Tok()
        tys = [[Tok() for _ in range(NT)] for _ in range(2)]
        order = [[16, 17] + list(range(16)), [17, 16] + list(range(15, -1, -1))]
        nsteps = NT if self.stop != f"rw1_{li}" else 3
        for step in range(nsteps):
            for d in range(2):
                i = order[d][step]
                mc1, mc2, mn, tri = msk[d]
                tgi = min(i // 4, 4)
                th = self.t_h[tgi]
                cols = slice(i * 128, (i + 1) * 128)
                for q, (dst, tn) in enumerate(((r_s, "r"), (k_s, "k"), (v_s, "v"))):
                    ps, pt = self.PS.next()
                    for kc in range(8):
                        P.pe(I_mm(ps, self.hT3[:, kc, cols], wrkv[q][:, kc, :], start=(kc == 0), stop=(kc == 7)), r=[tw[q], th], w=[pt])
                    P.act(I_acopy(dst, ps), r=[pt], w=[tk_[tn]])
                P.pool(I_copy(vb_, v_s), r=[tk_["v"]], w=[tk_["vb"]])
                for (c0, dst, tn, fn) in ((d * 64, l1, "l1", AF.Tanh), (128 + d * 64, la, "la", AF.Copy)):
                    ps, pt = self.PS.next()
                    for kc in range(8):
                        P.pe(I_mm(ps[0:64, 0:128], lo1[:, kc, c0:c0 + 64], self.hT3[:, kc, cols], start=(kc == 0), stop=(kc == 7)), r=[tlo, th], w=[pt])
                    P.act(I_act(dst[0:64, :], ps[0:64, 0:128], fn), r=[pt], w=[tk_[tn]])
                for (src, tn_s, wi, bi, dst, tn) in ((l1, "l1", d, d, sig, "sig"), (la, "la", 2 + d, 2 + d, a_s, "a")):
                    ps, pt = self.PS.next()
                    P.pe(I_mm(ps, src[0:64, :], lo2[0:64, wi, :], start=True, stop=False), r=[tk_[tn_s], tlo], w=[pt])
                    P.pe(I_mm(ps, self.ones_b[0:1, 0:128], brow[0:1, bi * 512:(bi + 1) * 512], start=False, stop=True), r=[tlo, self.t_c], w=[pt])
                    P.act(I_act(dst, ps, AF.Sigmoid), r=[pt], w=[tk_[tn]])
                P.pool(I_tt(kx, k_s, kk_bc, ALU.mult), r=[tk_["k"], tbc], w=[tk_["kx"]])
                P.pool(I_tt(tA, kx, kx, ALU.mult), r=[tk_["kx"]], w=[tk_["tA"]])
                P.dve(I_red(red8[:, 0:8], tA.rearrange("p (h j) -> p h j", h=8)), r=[tk_["tA"]], w=[tred])
                P.dve(I_ts(red8[:, 0:8], red8[:, 0:8], 1e-24, ALU.max), r=[tred], w=[tred])
                self.rsqrt(red8[:, 0:8], red8[:, 0:8], 1.0, 1e-24, [tred], tred)
                P.dve(I_tt(kk.rearrange("p (h j) -> p h j", h=8), kx.rearrange("p (h j) -> p h j", h=8),
                           red8[:, 0:8].unsqueeze(2).to_broadcast([128, 8, 64]), ALU.mult), r=[tk_["kx"], tred], w=[tk_["kk"]])
                P.pool(I_tt(b_s, kk, a_s, ALU.mult), r=[tk_["kk"], tk_["a"]], w=[tk_["b"]])
                P.dve(I_stt(tB, a_s, -1.0, ka_bc, ALU.add, ALU.mult), r=[tk_["a"], tbc], w=[tk_["tB"]])
                P.dve(I_stt(kE, tB, 1.0, k_s, ALU.add, ALU.mult), r=[tk_["tB"], tk_["k"]], w=[tk_["kE"]])
                P.pool(I_tt(tA, r_s, kE, ALU.mult), r=[tk_["r"], tk_["kE"], tred], w=[tk_["tA"]])
                P.pool(I_tt(tA, tA, rk_bc, ALU.mult), r=[tk_["tA"], tbc], w=[tk_["tA"]])
                P.dve(I_red(red8[:, 8:16], tA.rearrange("p (h j) -> p h j", h=8)), r=[tk_["tA"]], w=[tred])
                P.dve(I_tt(yout[:, 512:1024].rearrange("p (h j) -> p h j", h=8), v_s.rearrange("p (h j) -> p h j", h=8),
                           red8[:, 8:16].unsqueeze(2).to_broadcast([128, 8, 64]), ALU.mult), r=[tk_["v"], tred], w=[tyo])
                psc, ptc = self.PS.next()
                psC, ptC = self.PS.next()
                P.pe(I_mm(psc, tri, sig), r=[tmk, tk_["sig"]], w=[ptc])
                P.pe(I_mm(psC, allneg, sig), r=[tmk, tk_["sig"]], w=[ptC])
                P.act(I_acopy(cum_s, psc), r=[ptc], w=[tk_["cum"]])
                P.act(I_acopy(cumC_s, psC), r=[ptC], w=[tk_["cumC"]])
                P.dve(I_stt(tB, sig, DS, cum_s, ALU.mult, ALU.add), r=[tk_["sig"], tk_["cum"], tk_["kE"]], w=[tk_["tB"]])
                P.act(I_act(e1, tB, AF.Exp), r=[tk_["tB"]], w=[tk_["e1"]])
                P.act(I_act(e2, cum_s, AF.Exp, scale=-1.0), r=[tk_["cum"]], w=[tk_["e2"]])
                P.act(I_act(e3, cum_s, AF.Exp), r=[tk_["cum"]], w=[tk_["e3"]])
                P.dve(I_tt(tA, cumC_s, cum_s, ALU.subtract), r=[tk_["cumC"], tk_["cum"], tred], w=[tk_["tA"]])
                P.act(I_act(e4, tA, AF.Exp), r=[tk_["tA"]], w=[tk_["e4"]])
                P.act(I_act(pcb, cumC_s, AF.Exp), r=[tk_["cumC"]], w=[tk_["pc"]])
                P.pool(I_tt(dg[0:64, :].rearrange("p (h j) -> p h j", h=8), pcb[0:64, :].rearrange("p (h j) -> p h j", h=8),
                            self.ident[0:64, 0:64].unsqueeze(1).to_broadcast([64, 8, 64]), ALU.mult), r=[tk_["pc"], self.t_c], w=[tk_["dg"]])
                P.dve(I_tt(kt_, kk, e1, ALU.mult), r=[tk_["kk"], tk_["e1"]], w=[tk_["kt"]])
                P.pool(I_tt(bt_, b_s, e2, ALU.mult), r=[tk_["b"], tk_["e2"]], w=[tk_["bt"]])
                P.dve(I_tt(k2_, kE, e2, ALU.mult), r=[tk_["kE"], tk_["e2"]], w=[tk_["k2"]])
                P.pool(I_tt(rt_, r_s, e3, ALU.mult), r=[tk_["r"], tk_["e3"]], w=[tk_["rt"]])
                P.dve(I_tt(kh_, kE, e4, ALU.mult), r=[tk_["kE"], tk_["e4"]], w=[tk_["kh"]])
                P.pool(I_tt(bh_, b_s, e4, ALU.mult), r=[tk_["b"], tk_["e4"]], w=[tk_["bh"]])
                for g in range(4):
                    ps, pt = self.PS.next()
                    psb = ps.bitcast(BF16)
                    for q, (src, tn) in enumerate(((kt_, "kt"), (rt_, "rt"), (bt_, "bt"), (k2_, "k2"))):
                        P.pe(I_tr(psb[:, q * 128:(q + 1) * 128], src[:, g * 128:(g + 1) * 128], self.ident_b), r=[tk_[tn], self.t_c], w=[pt])
                    P.op("act" if g % 2 == 0 else "dve", (I_acopy if g % 2 == 0 else I_copy)(fT[g].rearrange("p q t -> p (q t)"), psb[:, 0:512]), r=[pt], w=[tfT[g]])
                for h in range(8):
                    g, R_ = h // 2, slice((h % 2) * 64, (h % 2) * 64 + 64)
                    H = hb[h]
                    f = fT[g]
                    ps1, pt1 = self.PS.next()
                    P.pe(I_mm(ps1[:, 0:256], f[R_, 2, :], f[R_, 0:2, :].rearrange("p q t -> p (q t)")), r=[tfT[g]], w=[pt1])
                    P.dve(I_tt(H["A1"], ps1[:, 0:256], mc1, ALU.mult), r=[pt1, tmk], w=[H["t"]["A1"]])
                    ps2, pt2 = self.PS.next()
                    P.pe(I_mm(ps2[:, 0:256], f[R_, 3, :], f[R_, 0:2, :].rearrange("p q t -> p (q t)")), r=[tfT[g]], w=[pt2])
                    P.dve(I_tt(H["A2"], ps2[:, 0:256], mc2, ALU.mult), r=[pt2, tmk], w=[H["t"]["A2"]])
                    ps3, pt3 = self.PS.next()
                    P.pe(I_mm(ps3[:, 0:128], f[R_, 0, :], f[R_, 2, :]), r=[tfT[g]], w=[pt3])
                    P.dve(I_tt(H["X0"][:, 0:128], ps3[:, 0:128], mn, ALU.mult), r=[pt3, tmk], w=[H["t"]["X0"]])
                    P.pool(I_copy(H["X0"][:, 128:256], H["A1"][:, 0:128]), r=[H["t"]["A1"]], w=[H["t"]["X0"]])
                    P.pool(I_tt(H["P0"], H["A1"][:, 0:128], self.ident_b, ALU.add), r=[H["t"]["A1"], self.t_c], w=[H["t"]["P0"]])
                cur = 0
                for lvl in range(1, 7):
                    nxt = 1 - cur
                    for h in range(8):
                        H = hb[h]
                        XNc, XNn = H[f"X{cur}"], H[f"X{nxt}"]
                        tXc, tXn = H["t"][f"X{cur}"], H["t"][f"X{nxt}"]
                        Nc, Xc = XNc[:, 0:128], XNc[:, 128:256]
                        psA, ptA = self.PS.next()
                        P.pe(I_mm(psA[:, 0:128], Xc, Nc), r=[tXc], w=[ptA])
                        w_ = 128
                        if lvl < 6:
                            P.pe(I_mm(psA[:, 128:256], Nc, Xc), r=[tXc], w=[ptA])
                            w_ = 256
                        if h % 2 == 0:
                            P.act(I_acopy(XNn[:, 0:w_], psA[:, 0:w_]), r=[ptA], w=[tXn])
                        else:
                            P.dve(I_copy(XNn[:, 0:w_], psA[:, 0:w_]), r=[ptA], w=[tXn])
                    for h in range(8):
                        H = hb[h]
                        XNn, Pc, Pn = H[f"X{nxt}"], H[f"P{cur}"], H[f"P{nxt}"]
                        tXn, tPc, tPn = H["t"][f"X{nxt}"], H["t"][f"P{cur}"], H["t"][f"P{nxt}"]
                        psP, ptP = self.PS.next()
                        P.pe(I_mm(psP[:, 0:128], XNn[:, 0:128], Pc), r=[tXn, tPc], w=[ptP])
                        P.dve(I_tt(Pn, psP[:, 0:128], Pc, ALU.add), r=[ptP, tPc], w=[tPn])
                    cur = nxt
                for h in range(8):
                    hc = slice(h * 64, (h + 1) * 64)
                    H = hb[h]
                    P.pool(I_copy(H["KAV"][:, 0:64], kt_[:, hc]), r=[tk_["kt"]], w=[H["t"]["KAV"]])
                    ps, pt = self.PS.next()
                    P.pe(I_mm(ps[:, 0:64], H["A2"][:, 0:128], vb_[:, hc]), r=[H["t"]["A2"], tk_["vb"]], w=[pt])
                    P.act(I_acopy(H["KAV"][:, 64:128], ps[:, 0:64]), r=[pt], w=[H["t"]["KAV"]])
                for h in range(8):
                    H = hb[h]
                    TT, tTT = H[f"P{cur}"], H["t"][f"P{cur}"]
                    ps, pt = self.PS.next()
                    P.pe(I_mm(ps[:, 0:128], TT, H["KAV"]), r=[tTT, H["t"]["KAV"]], w=[pt])
                    P.dve(I_ts(H["WU"], ps[:, 0:128], -1.0, ALU.mult), r=[pt], w=[H["t"]["WU"]])
                psY0, ptY0 = self.PSacc.next()
                psPhi, ptPhi = self.PSacc.next()
                psG, ptG = self.PSacc.next()
                for h in range(8):
                    hc = slice(h * 64, (h + 1) * 64)
                    H = hb[h]
                    tWU = H["t"]["WU"]
                    ps, pt = self.PS.next()
                    P.pe(I_mm(ps[0:64, 0:128], H["WU"][:, 0:64], H["A1"][:, 128:256], start=True, stop=False), r=[tWU, H["t"]["A1"]], w=[pt])
                    P.pe(I_mm(ps[0:64, 0:128], rt_[:, hc], self.ident_b, start=False, stop=True), r=[tk_["rt"], self.t_c], w=[pt])
                    P.act(I_acopy(QT[0:64, h, :], ps[0:64, 0:128]), r=[pt], w=[tQT])
                    P.pe(I_mm(psY0[:, hc], H["A1"][:, 128:256], H["WU"][:, 64:128], start=True, stop=False), r=[tWU, H["t"]["A1"]], w=[ptY0])
                    P.pe(I_mm(psY0[:, hc], H["A2"][:, 128:256], vb_[:, hc], start=False, stop=True), r=[H["t"]["A2"], tk_["vb"]], w=[ptY0])
                    P.pe(I_mm(psPhi[0:64, hc], H["WU"][:, 0:64], bh_[:, hc]), r=[tWU, tk_["bh"]], w=[ptPhi])
                    P.pe(I_mm(psG[0:64, hc], bh_[:, hc], H["WU"][:, 64:128], start=True, stop=False), r=[tWU, tk_["bh"]], w=[ptG])
                    P.pe(I_mm(psG[0:64, hc], kh_[:, hc], vb_[:, hc], start=False, stop=True), r=[tk_["kh"], tk_["vb"]], w=[ptG])
                P.dve(I_copy(Y0s, psY0), r=[ptY0], w=[tY0])
                P.dve(I_tt(PhiT[0:64, :], psPhi[0:64, :], dg[0:64, :], ALU.add), r=[ptPhi, tk_["dg"]], w=[tPhi])
                P.act(I_acopy(Gall[0:64, :], psG[0:64, :]), r=[ptG], w=[tG])
                pc_, pn_ = mpar[d], 1 - mpar[d]
                Mc, Mn = Mst[d][pc_], Mst[d][pn_]
                tMc, tMn = tM[d][pc_], tM[d][pn_]
                psY, ptY = self.PSacc.next()
                for h in range(8):
                    hc = slice(h * 64, (h + 1) * 64)
                    P.pe(I_mm(psY[:, hc], QT[0:64, h, :], Mc[0:64, hc]), r=[tQT, tMc], w=[ptY])
                P.dve(I_tt(yout[:, 0:512], psY, Y0s, ALU.add), r=[ptY, tY0], w=[tyo])
                psM, ptM = self.PS.next()
                for h in range(8):
                    hc = slice(h * 64, (h + 1) * 64)
                    P.pe(I_mm(psM[0:64, hc], PhiT[0:64, hc], Mc[0:64, hc]), r=[tPhi, tMc], w=[ptM])
                P.dve(I_tt(Mn[0:64, :], psM[0:64, :], Gall[0:64, :], ALU.add), r=[ptM, tG], w=[tMn])
                mpar[d] = pn_
                P.dma("sp", I_dma(self.yscr_d[d, i], yout), r=[tyo], w=[tys[d][i]])
        P.barrier()
        B.off = 0
        ya_r = Ring([AL(1024) for _ in range(2)])
        yb_r = Ring([AL(1024) for _ in range(2)])
        gs1 = AL(128, BF16)
        gs2 = AL(128, BF16)
        tgs = Tok()
        stg_r = Ring([AL(512, BF16) for _ in range(2)])
        yc = a512()
        tyc = Tok()
        for i in range(NT):
            if nsteps < NT and i not in (16, 17):
                continue
            cols = slice(i * 128, (i + 1) * 128)
            th = self.t_h[min(i // 4, 4)]
            ya, tya = ya_r.next()
            yb, tyb = yb_r.next()
            P.dma("sp", I_dma(ya, self.yscr_d[0, i]), r=[tys[0][i]], w=[tya])
            P.dma("act", I_dma(yb, self.yscr_d[1, i]), r=[tys[1][i]], w=[tyb])
            P.pool(I_tt(ya, ya, yb, ALU.add), r=[tya, tyb], w=[tya])
            y3 = ya[:, 0:512].rearrange("p (h j) -> p h j", h=8)
            P.dve(I_red(red8[:, 0:8], y3), r=[tya], w=[tred])
            P.dve(I_ts(red8[:, 0:8], red8[:, 0:8], 1.0 / 64, ALU.mult), r=[tred], w=[tred])
            P.dve(I_tt(yc.rearrange("p (h j) -> p h j", h=8), y3, red8[:, 0:8].unsqueeze(2).to_broadcast([128, 8, 64]), ALU.subtract),
                  r=[tya, tred], w=[tyc])
            P.pool(I_tt(yb[:, 0:512], yc, yc, ALU.mult), r=[tyc], w=[tyb])
            P.dve(I_red(red8[:, 8:16], yb[:, 0:512].rearrange("p (h j) -> p h j", h=8)), r=[tyb], w=[tred])
            self.rsqrt(red8[:, 8:16], red8[:, 8:16], 1.0 / 64, 64e-5, [tred], tred)
            P.dve(I_tt(yc.rearrange("p (h j) -> p h j", h=8), yc.rearrange("p (h j) -> p h j", h=8),
                       red8[:, 8:16].unsqueeze(2).to_broadcast([128, 8, 64]), ALU.mult), r=[tyc, tred], w=[tyc])
            P.pool(I_tt(yc, yc, lg_bc, ALU.mult), r=[tyc, tbc], w=[tyc])
            P.pool(I_tt(yc, yc, lb_bc, ALU.add), r=[tyc, tbc], w=[tyc])
            P.pool(I_tt(yc, yc, ya[:, 512:1024], ALU.add), r=[tyc, tya], w=[tyc])
            ps, pt = self.PS.next()
            for kc in range(8):
                P.pe(I_mm(ps[:, 0:128], lo1[:, kc, 256:384], self.hT3[:, kc, cols], start=(kc == 0), stop=(kc == 7)), r=[tlo, th], w=[pt])
            P.act(I_act(gs1, ps[:, 0:128], AF.Sigmoid), r=[pt], w=[tgs])
            ps, pt = self.PS.next()
            for kc in range(8):
                P.pe(I_mm(ps[0:32, 0:128], lo1[:, kc, 384:416], self.hT3[:, kc, cols], start=(kc == 0), stop=(kc == 7)), r=[tlo, th], w=[pt])
            P.act(I_act(gs2[0:32, :], ps[0:32, 0:128], AF.Sigmoid), r=[pt], w=[tgs])
            ps, pt = self.PS.next()
            P.pe(I_mm(ps, gs1, lo2[:, 4, :], start=True, stop=False), r=[tgs, tlo], w=[pt])
            P.pe(I_mm(ps, gs2[0:32, :], lo2[0:32, 5, :], start=False, stop=True), r=[tgs, tlo], w=[pt])
            P.dve(I_tt(yc, yc, ps, ALU.mult), r=[tyc, pt], w=[tyc])
            ps, pt = self.PS.next()
            for g in range(4):
                P.pe(I_tr(ps[:, g * 128:(g + 1) * 128], yc[:, g * 128:(g + 1) * 128], self.ident), r=[tyc, self.t_c], w=[pt])
            stg, tst = stg_r.next()
            P.act(I_acopy(stg, ps), r=[pt], w=[tst])
            P.dma("sp", I_dma(self.brT_d[2, :, :, cols].rearrange("k p t -> p k t"), stg.rearrange("p (k t) -> p k t", k=4)),
                  r=[tst], w=[self.t_br[2]])
        P.barrier()
        A.release(m)

    def moe_router_setup(self, li):
        P, A = self.P, self.A
        self.wr = A.alloc(8 * 32).rearrange("p (k c) -> p k c", k=8)
        self.t_wr = Tok()
        P.dma("sp", I_dma(self.wr, self.W["router_w"][li].rearrange("(k p) c -> p k c", p=128)), w=[self.t_wr])
        self.rbrow = A.alloc(32)
        P.dma("sp", I_dma(self.rbrow[0:1, :], self.W["router_b"][li:li + 1, :]), w=[self.t_wr])
        self.GT = A.alloc(T)
        self.t_GT = Tok()
        self.rt_r = Ring([A.alloc(128) for _ in range(2)])

    def router_cb(self, tg, s, n, h2f3, th2f):
        P = self.P
        for j in range(n // 128):
            c0 = s + j * 128
            ps, pt = self.PS.next()
            for kc in range(8):
                P.pe(I_mm(ps[:, 0:32], h2f3[:, kc, j * 128:(j + 1) * 128], self.wr[:, kc, :], start=(kc == 0), stop=False),
                     r=[th2f, self.t_wr], w=[pt])
            P.pe(I_mm(ps[:, 0:32], self.ones[0:1, 0:128], self.rbrow[0:1, 0:32], start=False, stop=True), r=[self.t_wr, self.t_c], w=[pt])
            wk, twk = self.rt_r.next()
            lg, t8, msk, e_, sm = wk[:, 0:32], wk[:, 32:40], wk[:, 40:72], wk[:, 72:104], wk[:, 104:108]
            P.dve(I_copy(lg, ps[:, 0:32]), r=[pt], w=[twk])
            P.dve(lambda e, o=t8, i=lg: e.max(out=o, in_=i), r=[twk], w=[twk])
            P.dve(I_ts(msk, lg, t8[:, 3:4], ALU.is_ge), r=[twk], w=[twk])
            P.dve(I_ts(sm[:, 0:1], t8[:, 0:1], -1.0, ALU.mult), r=[twk], w=[twk])
            P.act(I_act(e_, lg, AF.Exp, bias=sm[:, 0:1]), r=[twk], w=[twk])
            P.dve(I_tt(e_, e_, msk, ALU.mult), r=[twk], w=[twk])
            P.dve(I_red(sm[:, 1:2], e_), r=[twk], w=[twk])
            P.dve(lambda e, o=sm[:, 2:3], i=sm[:, 1:2]: e.reciprocal(out=o, in_=i), r=[twk], w=[twk])
            P.dve(I_ts(e_, e_, sm[:, 2:3], ALU.mult), r=[twk], w=[twk])
            ps2, pt2 = self.PS.next()
            P.pe(I_tr(ps2[0:32, 0:128], e_, self.ident), r=[twk, self.t_c], w=[pt2])
            P.dve(I_copy(self.GT[0:32, c0:c0 + 128], ps2[0:32, 0:128]), r=[pt2], w=[self.t_GT])

    def moe_phase(self, li):
        P, A = self.P, self.A
        m = A.mark()
        b1T = A.alloc(16 * 32).rearrange("p (c e) -> p c e", c=16)
        tb1T = Tok()
        m2 = A.mark()
        self.moe_router_setup(li)
        self.norm_phase(1, li, router=self.router_cb)
        self.dbg(f"GT{li}", self.GT[0:32, :], self.t_GT, [32, T])
        tgt = Tok()
        P.dma("sp", I_dma(self.gt_d, self.GT[0:32, :]), r=[self.t_GT], w=[tgt])
        b1s = A.alloc(2048)
        tb1 = Tok()
        P.dma("act", I_dma(b1s[0:32, :], self.W["exp_b1"][li]), w=[tb1])
        for c in range(16):
            ps, pt = self.PS.next()
            P.pe(I_tr(ps[:, 0:32], b1s[0:32, c * 128:(c + 1) * 128], self.ident[0:32, 0:32]), r=[tb1, self.t_c], w=[pt])
            P.dve(I_copy(b1T[:, c, :], ps[:, 0:32]), r=[pt], w=[tb1T])
        b2s = A.alloc(1024)
        tb2 = Tok()
        P.dma("act", I_dma(b2s[0:32, :], self.W["exp_b2"][li]), w=[tb2])
        for tg, (s, n) in enumerate(self.tgs(li)):
            col = 0 if tg < 4 else 1
            for cc in range(8):
                ps, pt = self.PS.next()
                P.pe(I_mm(ps[:, 0:n], b2s[0:32, cc * 128:(cc + 1) * 128], self.GT[0:32, s:s + n]), r=[tb2, self.t_GT], w=[pt])
                P.dve(I_stt(self.xT3[:, cc, s:s + n], ps[:, 0:n], self.eff4[:, 5, cc, col:col + 1], self.xT3[:, cc, s:s + n], ALU.mult, ALU.add),
                      r=[pt, self.t_eff, self.t_x[tg]], w=[self.t_x[tg]])
        P.barrier()
        A.release(m2)
        w1_r = Ring([A.alloc(8 * 256, BF16) for _ in range(4)])
        w2_r = Ring([A.alloc(8 * 128, BF16) for _ in range(4)])
        gate_r = Ring([A.alloc(T, BF16) for _ in range(2)])
        aT3 = A.alloc(8 * T, BF16).rearrange("p (c t) -> p c t", c=8)
        taT = toks(5)
        tmp_r = Ring([A.alloc(512) for _ in range(2)])
        g_r = Ring([A.alloc(512) for _ in range(3)])
        s_r = Ring([A.alloc(512, BF16) for _ in range(2)])
        l_r = Ring([A.alloc(512, BF16) for _ in range(2)])
        a_r = Ring([A.alloc(512, BF16) for _ in range(2)])
        W1, W2 = self.W["exp_w1"][li], self.W["exp_w2"][li]
        n_exp = 32 if self.stop != f"moe1_{li}" else self.n_exp_dbg
        loads, comps = [], []
        bufmap = {}

        def mk_gate(e):
            def f():
                gate, tgate = gate_r.next()
                P.dma("pool", I_dma(gate, self.gt_d[e:e + 1, :].to_broadcast([128, T])), r=[tgt], w=[tgate])
                bufmap[("g", e)] = (gate, tgate)
            return f

        def mk_w1(e, c):
            def f():
                wp, twp = w1_r.next()
                wp3 = wp.rearrange("p (k c) -> p k c", k=8)
                P.dma("pool", I_dma(wp3[:, :, 0:128], W1[e][:, c * 128:(c + 1) * 128].rearrange("(k p) c -> p k c", p=128)), w=[twp])
                P.dma("pool", I_dma(wp3[:, :, 128:256], W1[e][:, 1024 + c * 128:1024 + (c + 1) * 128].rearrange("(k p) c -> p k c", p=128)), w=[twp])
                bufmap[("w1", e, c)] = (wp3, twp)
            return f

        def mk_w2(e, cc):
            def f():
                w2p, tw2 = w2_r.next()
                w2p3 = w2p.rearrange("p (k c) -> p k c", k=8)
                P.dma("pool", I_dma(w2p3, W2[e][:, cc * 128:(cc + 1) * 128].rearrange("(k p) c -> p k c", p=128)), w=[tw2])
                bufmap[("w2", e, cc)] = (w2p3, tw2)
            return f

        def mk_hid(e, c):
            def f():
                wp3, twp = bufmap[("w1", e, c)]
                for tg, (s, n) in enumerate(self.tgs(li)):
                    psg, ptg = self.PS.next()
                    psl, ptl = self.PS.next()
                    for kc in range(8):
                        P.pe(I_mm(psg[:, 0:n], wp3[:, kc, 0:128], self.hT3[:, kc, s:s + n], start=(kc == 0), stop=(kc == 7)),
                             r=[twp, self.t_h[tg]], w=[ptg])
                    for kc in range(8):
                        P.pe(I_mm(psl[:, 0:n], wp3[:, kc, 128:256], self.hT3[:, kc, s:s + n], start=(kc == 0), stop=(kc == 7)),
                             r=[twp, self.t_h[tg]], w=[ptl])
                    g_, tg_ = g_r.next()
                    s_, ts_ = s_r.next()
                    l_, tl_ = l_r.next()
                    a_, ta_ = a_r.next()
                    P.dve(I_ts(g_[:, 0:n], psg[:, 0:n], b1T[:, c, e:e + 1], ALU.add, 7.0, ALU.min), r=[ptg, tb1T], w=[tg_])
                    P.act(I_act(s_[:, 0:n], g_[:, 0:n], AF.Sigmoid, scale=1.702), r=[tg_], w=[ts_])
                    P.act(I_act(l_[:, 0:n], psl[:, 0:n], AF.Identity, bias=b1T[:, 8 + c, e:e + 1]), r=[ptl, tb1T], w=[tl_])
                    P.dve(I_ts(l_[:, 0:n], l_[:, 0:n], 7.0, ALU.min, -7.0, ALU.max), r=[tl_], w=[tl_])
                    P.dve(I_tt(a_[:, 0:n], g_[:, 0:n], s_[:, 0:n], ALU.mult), r=[tg_, ts_], w=[ta_])
                    P.dve(I_stt(aT3[:, c, s:s + n], l_[:, 0:n], 1.0, a_[:, 0:n], ALU.add, ALU.mult), r=[ta_, tl_], w=[taT[tg]])
            return f

        def mk_y(e, cc):
            def f():
                gate, tgate = bufmap[("g", e)]
                w2p3, tw2 = bufmap[("w2", e, cc)]
                for tg, (s, n) in enumerate(self.tgs(li)):
                    col = 0 if tg < 4 else 1
                    ps, pt = self.PS.next()
                    for c in range(8):
                        P.pe(I_mm(ps[:, 0:n], w2p3[:, c, :], aT3[:, c, s:s + n], start=(c == 0), stop=(c == 7)), r=[tw2, taT[tg]], w=[pt])
                    tmp, ttmp = tmp_r.next()
                    P.dve(I_tt(tmp[:, 0:n], ps[:, 0:n], gate[:, s:s + n], ALU.mult), r=[pt, tgate], w=[ttmp])
                    P.dve(I_stt(self.xT3[:, cc, s:s + n], tmp[:, 0:n], self.eff4[:, 5, cc, col:col + 1], self.xT3[:, cc, s:s + n], ALU.mult, ALU.add),
                          r=[ttmp, self.t_eff, self.t_x[tg]], w=[self.t_x[tg]])
            return f

        for e in range(n_exp):
            loads.append(mk_gate(e))
            for c in range(8):
                loads.append(mk_w1(e, c))
                comps.append((len(loads) - 1, mk_hid(e, c)))
            for cc in range(8):
                loads.append(mk_w2(e, cc))
                comps.append((len(loads) - 1, mk_y(e, cc)))
        LOOK = 3
        ptr = 0
        for need, f in comps:
            tgt_ptr = min(len(loads), need + 1 + LOOK)
            while ptr < tgt_ptr:
                loads[ptr]()
                ptr += 1
            f()
        P.barrier()
        A.release(m)

    def build(self):
        P = self.P
        self.setup()
        self.load_x()
        for li in range(self.n_layers):
            self.mod_phase(li)
            self.norm_phase(0, li)
            self.dbg(f"hT{li}", self.hT, self.t_h, [128, 8 * T], BF16)
            self.dbg(f"eff{li}", self.eff, self.t_eff, [128, 96])
            if self.stop == f"norm1_{li}":
                break
            self.spill_x()
            if "attn" not in self.skip:
                self.attn_phase(li)
            if self.stop in (f"attn_{li}", f"attnproj_{li}"):
                break
            if "conv" not in self.skip:
                self.conv_phase(li)
            if self.stop == f"conv_{li}":
                break
            if "rw" not in self.skip:
                self.rwkv_phase(li)
            if self.stop in (f"rw_{li}", f"rw1_{li}"):
                break
            self.restore_x()
            self.merge_phase(li)
            self.dbg(f"xmid{li}", self.xT, self.t_x, [128, 8 * T])
            if self.stop == f"merge_{li}":
                break
            self.moe_phase(li)
            self.dbg(f"xout{li}", self.xT, self.t_x, [128, 8 * T])
            if self.stop in (f"moe_{li}", f"moe1_{li}"):
                break
        if self.stop is None:
            self.final_phase()
        if self.dbg_br:
            t = Tok()
            d = self.nc.dram_tensor("dbg_brT", [3, 4, 128, T], BF16, kind="ExternalOutput").ap()
            P.dma("sp", I_dma(d, self.brT_d), r=self.t_br, w=[t])
            self.out_toks.append(t)
            self.dbg_out["brT"] = "dbg_brT"
        P.op("sp", None, r=self.out_toks)
        P.emit(self.nc, self.st)
        self.st.close()
        return self.nc


_CONSTS = None


def make_in_maps(inputs):
    global _CONSTS
    if _CONSTS is None:
        _CONSTS = make_consts()
    shared = {n: np.ascontiguousarray(np.asarray(inputs[n], dtype=np.float32)) for n, _ in WEIGHT_SPECS}
    for n in CONST_NAMES:
        shared["k_" + n] = _CONSTS[n]
    x = np.asarray(inputs["x"], dtype=np.float32)
    ctx = np.asarray(inputs["ctx"], dtype=np.float32)
    c = np.asarray(inputs["c"], dtype=np.float32)
    c_ctx = np.asarray(inputs["c_ctx"], dtype=np.float32)
    maps = []
    for b in range(8):
        m = dict(shared)
        m["x"] = np.ascontiguousarray(x[b])
        m["ctx"] = np.ascontiguousarray(ctx[b])
        m["c2"] = np.ascontiguousarray(np.stack([c[b], c_ctx], 0))
        maps.append(m)
    return maps


def kernel(**inputs):
    mk = MK()
    nc = mk.build()
    maps = make_in_maps(inputs)
    res = run_bass_kernel_spmd(nc, maps, core_ids=list(range(8)))
    return np.stack([np.asarray(res.results[b]["out"], dtype=np.float32) for b in range(8)], 0)
```

```python
import math
import numpy as np
from contextlib import ExitStack
import concourse.bass as bass
import concourse.mybir as mybir
from concourse.bass_utils import run_bass_kernel_spmd

F32 = mybir.dt.float32
BF16 = mybir.dt.bfloat16
AF = mybir.ActivationFunctionType
ALU = mybir.AluOpType
AX = mybir.AxisListType

D = 1024
TL = 2048
TC = 256
T = TL + TC
NT = T // 128
DEPTH = 2
IN_COLS = 7680
TGS = [(0, 512), (512, 512), (1024, 512), (1536, 512), (2048, 256)]

ENGS = ("pe", "act", "dve", "pool", "sp")
NDMASEM = 24


class Tok:
    __slots__ = ("w", "rs", "name")

    def __init__(self, name=""):
        self.w = None
        self.rs = []
        self.name = name


def toks(n):
    return [Tok() for _ in range(n)]


class Ev:
    __slots__ = ("kind", "eng", "idx", "sem", "target", "op")

    def __init__(self, kind, eng, idx, sem=None, target=None, op=None):
        self.kind, self.eng, self.idx, self.sem, self.target, self.op = kind, eng, idx, sem, target, op


class Op:
    __slots__ = ("eng", "fn", "dma", "idx", "waits", "flag", "tick", "dsem", "dtarget", "prewait")


class Prog:
    def __init__(self):
        self.ops = {e: [] for e in ENGS}
        self.ndma = {e: 0 for e in ENGS}
        self.seen = {e: {} for e in ENGS}
        self.last = {e: None for e in ENGS}
        self.pend_dma = {}

    def op(self, eng, fn, r=(), w=(), dma=False, extra=()):
        o = Op()
        o.eng, o.fn, o.dma = eng, fn, dma
        o.idx = len(self.ops[eng])
        o.flag = False
        o.tick = None
        o.prewait = None
        waits = {}

        def need(ev, force=False):
            if ev is None:
                return
            if ev.kind == "dma":
                k = ("dma", ev.eng, ev.sem)
                if waits.get(k, (0, None))[0] < ev.target:
                    waits[k] = (ev.target, ev)
            else:
                if ev.eng == eng and not dma and not force:
                    if eng == "pe":
                        return
                    if o.idx - ev.idx > 2:
                        return
                k = ("eng", ev.eng)
                if waits.get(k, (-1, None))[0] < ev.idx:
                    waits[k] = (ev.idx, ev)

        for t in r:
            need(t.w)
        for t in w:
            need(t.w)
            for ev in t.rs:
                need(ev)
        for ev in extra:
            if not (ev.kind == "eng" and ev.eng == eng):
                need(ev, force=True)
        seen = self.seen[eng]
        fw = {}
        for k, (v, ev) in waits.items():
            if seen.get(k, -1) >= v:
                continue
            seen[k] = v
            fw[k] = (v, ev)
            if ev.kind == "eng":
                ev.op.flag = True
        o.waits = fw
        if dma:
            n = self.ndma[eng]
            self.ndma[eng] = n + 1
            o.dsem = n % NDMASEM
            o.dtarget = 16 * (n // NDMASEM + 1)
            if n >= NDMASEM:
                o.prewait = (o.dsem, o.dtarget - 16)
            ev = Ev("dma", eng, o.idx, o.dsem, o.dtarget, o)
            self.pend_dma[(eng, o.dsem)] = ev
        else:
            ev = Ev("eng", eng, o.idx, op=o)
            if fn is not None:
                self.last[eng] = ev
        for t in r:
            t.rs.append(ev)
        for t in w:
            t.w = ev
            t.rs = []
        self.ops[eng].append(o)
        return o

    def pe(self, fn, r=(), w=()):
        return self.op("pe", fn, r, w)

    def act(self, fn, r=(), w=()):
        return self.op("act", fn, r, w)

    def dve(self, fn, r=(), w=()):
        return self.op("dve", fn, r, w)

    def pool(self, fn, r=(), w=()):
        return self.op("pool", fn, r, w)

    def dma(self, q, fn, r=(), w=()):
        return self.op(q, fn, r, w, dma=True)

    def barrier(self):
        evs = [ev for ev in self.last.values() if ev is not None] + list(self.pend_dma.values())
        self.pend_dma = {}
        for e in ENGS:
            self.op(e, None, extra=evs)

    def emit(self, nc, stack):
        for e in ENGS:
            t = 0
            for o in self.ops[e]:
                if o.flag and not o.dma:
                    t += 1
                    o.tick = t
        esem = {e: stack.enter_context(nc.semaphore(f"s_{e}")) for e in ENGS}
        dsem = {e: [stack.enter_context(nc.semaphore(f"d_{e}{i}")) for i in range(NDMASEM)]
                for e in ENGS if self.ndma[e] > 0}
        block = stack.enter_context(nc.Block())
        prog = self

        def runner(ename):
            def f(eng):
                for o in prog.ops[ename]:
                    for k, (v, ev) in o.waits.items():
                        if k[0] == "dma":
                            eng.wait_ge(dsem[k[1]][k[2]], v)
                        else:
                            eng.wait_ge(esem[k[1]], ev.op.tick)
                    if o.dma and o.prewait is not None:
                        eng.wait_ge(dsem[ename][o.prewait[0]], o.prewait[1])
                    if o.fn is None:
                        continue
                    ins = o.fn(eng)
                    if o.dma:
                        ins.then_inc(dsem[ename][o.dsem], 16)
                    elif o.flag:
                        ins.then_inc(esem[ename], 1)
            return f

        block.sync(runner("sp"))
        block.tensor(runner("pe"))
        block.scalar(runner("act"))
        block.vector(runner("dve"))
        block.gpsimd(runner("pool"))


class Arena:
    def __init__(self, ap, n):
        self.ap, self.n, self.off = ap, n, 0

    def alloc(self, n, dt=F32):
        nf = n if dt == F32 else (n + 1) // 2
        nf = (nf + 7) // 8 * 8
        assert self.off + nf <= self.n, ("arena overflow", self.off, nf, self.n)
        a = self.ap[:, self.off:self.off + nf]
        self.off += nf
        if dt != F32:
            a = a.bitcast(dt)[:, 0:n]
        else:
            a = a[:, 0:n]
        return a

    def mark(self):
        return self.off

    def release(self, m):
        self.off = m


class Ring:
    def __init__(self, bufs):
        self.bufs = bufs
        self.toks = toks(len(bufs))
        self.i = 0

    def next(self):
        j = self.i % len(self.bufs)
        self.i += 1
        return self.bufs[j], self.toks[j]


def make_consts():
    c = {}
    c["ident"] = np.eye(128, dtype=np.float32)
    c["ones"] = np.ones((128, 128), dtype=np.float32)
    pos_r = (np.arange(TL) // 64).astype(np.float32)
    pos_c = (np.arange(TL) % 64).astype(np.float32)
    inv = np.power(10000.0, -np.arange(16, dtype=np.float32) / 16).astype(np.float32)
    cosT = np.zeros((128, TL), np.float32)
    sinT = np.zeros((128, TL), np.float32)
    pm = np.zeros((128, 128), np.float32)
    for p in range(128):
        d = p % 64
        pos = pos_r if d < 32 else pos_c
        ang = pos * inv[d % 16]
        cosT[p] = np.cos(ang)
        sinT[p] = np.sin(ang)
        if (d % 32) < 16:
            pm[p + 16, p] = -1.0
        else:
            pm[p - 16, p] = 1.0
    c["cosT"], c["sinT"], c["pm"] = cosT, sinT, pm
    s = np.arange(128)[:, None]
    t = np.arange(128)[None, :]
    c["m_f_strict"] = (s < t).astype(np.float32)
    c["m_f_incl"] = (s <= t).astype(np.float32)
    c["m_r_strict"] = (s > t).astype(np.float32)
    c["m_r_incl"] = (s >= t).astype(np.float32)
    bd = np.zeros((128, 128), np.float32)
    bd[:64, :64] = 1
    bd[64:, 64:] = 1
    c["blockdiag"] = bd
    return c


CONST_NAMES = ["ident", "ones", "cosT", "sinT", "pm", "m_f_strict", "m_f_incl", "m_r_strict", "m_r_incl", "blockdiag"]

WEIGHT_SPECS = [
    ("w_mod", [DEPTH, D, 6 * D]), ("b_mod", [DEPTH, 6 * D]), ("norm1_g", [DEPTH, D]), ("norm2_g", [DEPTH, D]),
    ("w_in", [DEPTH, D, IN_COLS]), ("da_lambda", [DEPTH, 4, 64]), ("da_subln_g", [DEPTH, 128]),
    ("conv_w", [DEPTH, 3, 512]), ("rw_w0", [DEPTH, 2, 512]), ("rw_w1", [DEPTH, 2, D, 64]),
    ("rw_w2", [DEPTH, 2, 64, 512]), ("rw_a0", [DEPTH, 2, 512]), ("rw_a1", [DEPTH, 2, D, 64]),
    ("rw_a2", [DEPTH, 2, 64, 512]), ("rw_g1", [DEPTH, D, 160]), ("rw_g2", [DEPTH, 160, 512]),
    ("rw_k_k", [DEPTH, 512]), ("rw_k_a", [DEPTH, 512]), ("rw_r_k", [DEPTH, 8, 64]),
    ("rw_lnx_g", [DEPTH, 512]), ("rw_lnx_b", [DEPTH, 512]), ("w_branch", [DEPTH, 3, 512, D]),
    ("w_out", [DEPTH, D, D]), ("router_w", [DEPTH, D, 32]), ("router_b", [DEPTH, 32]),
    ("exp_w1", [DEPTH, 32, D, 2 * D]), ("exp_b1", [DEPTH, 32, 2 * D]), ("exp_w2", [DEPTH, 32, D, D]),
    ("exp_b2", [DEPTH, 32, D]), ("final_g", [D]),
]


def I_copy(o, i):
    return lambda e: e.tensor_copy(out=o, in_=i)


def I_acopy(o, i):
    return lambda e: e.activation(out=o, in_=i, func=AF.Copy)


def I_act(o, i, func, bias=None, scale=None):
    kw = {}
    if bias is not None:
        kw["bias"] = bias
    if scale is not None:
        kw["scale"] = scale
    return lambda e: e.activation(out=o, in_=i, func=func, **kw)


def I_mm(o, l, r, start=True, stop=True):
    return lambda e: e.matmul(o, lhsT=l, rhs=r, start=start, stop=stop)


def I_tr(o, i, ident):
    return lambda e: e.transpose(out=o, in_=i, identity=ident)


def I_tt(o, a, b, op):
    return lambda e: e.tensor_tensor(out=o, in0=a, in1=b, op=op)


def I_ts(o, a, s1, op0, s2=None, op1=None):
    if op1 is None:
        return lambda e: e.tensor_scalar(out=o, in0=a, scalar1=s1, scalar2=None, op0=op0)
    return lambda e: e.tensor_scalar(out=o, in0=a, scalar1=s1, scalar2=s2, op0=op0, op1=op1)


def I_stt(o, in0, scalar, in1, op0, op1):
    return lambda e: e.scalar_tensor_tensor(out=o, in0=in0, scalar=scalar, in1=in1, op0=op0, op1=op1)


def I_red(o, i, op=ALU.add):
    return lambda e: e.tensor_reduce(out=o, in_=i, axis=AX.X, op=op)


def I_dma(o, i):
    return lambda e: e.dma_start(out=o, in_=i)


def I_memset(o, v):
    return lambda e: e.memset(o, v)


ARENA_F32 = 52224


class MK:
    def __init__(self, n_layers=DEPTH, dbg=(), stop=None, skip=(), dbg_br=False):
        self.skip = set(skip)
        self.n_exp_dbg = 1
        self.dbg_br = dbg_br
        self.n_layers = n_layers
        self.dbg_names = set(dbg)
        self.stop = stop
        nc = bass.Bass("TRN2", target_bir_lowering=False)
        self.nc = nc
        self.st = ExitStack()
        self.P = Prog()
        self.x_d = nc.dram_tensor("x", [TL, D], F32, kind="ExternalInput").ap()
        self.ctx_d = nc.dram_tensor("ctx", [TC, D], F32, kind="ExternalInput").ap()
        self.c2_d = nc.dram_tensor("c2", [2, D], F32, kind="ExternalInput").ap()
        self.W = {n: nc.dram_tensor(n, s, F32, kind="ExternalInput").ap() for n, s in WEIGHT_SPECS}
        cs = make_consts()
        self.Cd = {n: nc.dram_tensor("k_" + n, list(cs[n].shape), F32, kind="ExternalInput").ap() for n in CONST_NAMES}
        self.out_d = nc.dram_tensor("out", [TL, D], F32, kind="ExternalOutput").ap()
        self.brT_d = nc.dram_tensor("brT", [3, 4, 128, T], BF16).ap()
        self.yscr_d = nc.dram_tensor("yscr", [2, NT, 128, 1024], F32).ap()
        self.gt_d = nc.dram_tensor("gtscr", [32, T], F32).ap()
        self.dbg_out = {}
        self.out_toks = []
        arena_t = self.st.enter_context(nc.sbuf_tensor("arena", [128, ARENA_F32], F32))
        self.A = Arena(arena_t[:], ARENA_F32)
        banks = [self.st.enter_context(nc.psum_tensor(f"psb{i}", [128, 512], F32))[:] for i in range(8)]
        self.PS = Ring(banks[0:4])
        self.PSacc = Ring(banks[4:8])
        self.PS8 = Ring(banks)
        self.PS8.toks = self.PS.toks + self.PSacc.toks
        self.t_br = toks(3)
        self.xscr_d = nc.dram_tensor("xscr", [128, 8 * T], F32).ap()

    def dbg(self, name, ap, tok, shape, dt=F32):
        if name not in self.dbg_names:
            return
        d = self.nc.dram_tensor("dbg_" + name, list(shape), dt, kind="ExternalOutput").ap()
        t = Tok()
        self.P.dma("sp", I_dma(d, ap), r=tok if isinstance(tok, (list, tuple)) else [tok], w=[t])
        self.out_toks.append(t)
        self.dbg_out[name] = "dbg_" + name

    def load_w_bf16(self, dst3, src2d, tok, kc=8):
        self.P.dma("pool", I_dma(dst3, src2d.rearrange("(k p) c -> p k c", p=128)), w=[tok])

    def rows_to_cols(self, dst, rows, n, tok_rows, tok_dst, scale=None):
        P = self.P
        ps, pt = self.PS.next()
        for j in range(n):
            P.pe(I_mm(ps[:, j:j + 1], rows[0:1, j * 128:(j + 1) * 128], self.ones[0:1, 0:1]), r=[tok_rows, self.t_c], w=[pt])
        P.dve(I_copy(dst, ps[:, 0:n]), r=[pt], w=[tok_dst])

    def bcast_rows(self, dst, row, n, tok_row, tok_dst, scale=None, ones=None):
        P = self.P
        ps, pt = self.PS.next()
        P.pe(I_mm(ps[:, 0:n], (self.ones if ones is None else ones)[0:1, 0:128], row[0:1, 0:n]), r=[tok_row, self.t_c], w=[pt])
        if scale is None:
            P.dve(I_copy(dst, ps[:, 0:n]), r=[pt], w=[tok_dst])
        else:
            P.dve(I_ts(dst, ps[:, 0:n], float(scale), ALU.mult), r=[pt], w=[tok_dst])

    def rsqrt(self, out, in_, mult, eps, r, tok):
        self.P.act(I_act(out, in_, AF.Ln, bias=self.epsc[:, self.eps_idx[eps]:self.eps_idx[eps] + 1][0:out.shape[0]], scale=float(mult)), r=list(r) + [self.t_c], w=[tok])
        self.P.act(I_act(out, out, AF.Exp, scale=-0.5), r=[tok], w=[tok])

    def tgs(self, li):
        return TGS if li < self.n_layers - 1 else TGS[:4]

    def AL(self, n, dt=F32):
        try:
            return self.B.alloc(n, dt)
        except AssertionError:
            return self.A.alloc(n, dt)

    def spill_x(self):
        P = self.P
        self.t_xs = Tok()
        for k in range(8):
            P.dma("sp" if k % 2 == 0 else "act", I_dma(self.xscr_d[:, k * T:(k + 1) * T], self.xT[:, k * T:(k + 1) * T]), r=self.t_x, w=[self.t_xs])
        P.barrier()
        self.B = Arena(self.xT, 8 * T)

    def restore_x(self):
        P = self.P
        P.barrier()
        for k in range(8):
            P.dma("sp" if k % 2 == 0 else "act", I_dma(self.xT[:, k * T:(k + 1) * T], self.xscr_d[:, k * T:(k + 1) * T]), r=[self.t_xs], w=self.t_x)
        P.barrier()

    def setup(self):
        P, A = self.P, self.A
        self.t_c = Tok()
        self.ident = A.alloc(128)
        self.ones = A.alloc(128)
        self.ident_b = A.alloc(128, BF16)
        self.ones_b = A.alloc(128, BF16)
        P.dma("sp", I_dma(self.ident, self.Cd["ident"]), w=[self.t_c])
        P.dma("sp", I_dma(self.ones, self.Cd["ones"]), w=[self.t_c])
        P.dma("pool", I_dma(self.ident_b, self.Cd["ident"]), w=[self.t_c])
        P.dma("pool", I_dma(self.ones_b, self.Cd["ones"]), w=[self.t_c])
        self.epsc = A.alloc(8)
        self.eps_idx = {1e-6: 0, 1e-5: 1, 64e-5: 2, 1e-24: 3}
        for v, i in self.eps_idx.items():
            P.dve(I_memset(self.epsc[:, i:i + 1], v), w=[self.t_c])
        self.xT = A.alloc(8 * T)
        self.xT3 = self.xT.rearrange("p (k t) -> p k t", k=8)
        self.t_x = toks(5)
        self.hT = A.alloc(8 * T, BF16)
        self.hT3 = self.hT.rearrange("p (k t) -> p k t", k=8)
        self.t_h = toks(5)
        self.csT = A.alloc(16)
        self.t_cs = Tok()
        self.eff = A.alloc(96)
        self.eff4 = self.eff.rearrange("p (n k c) -> p n k c", n=6, k=8)
        self.t_eff = Tok()
        self.gvec = A.alloc(24)
        self.t_gvec = Tok()

    def load_x(self):
        P, A = self.P, self.A
        m = A.mark()
        stage = Ring([A.alloc(1024) for _ in range(2)])
        for i in range(NT):
            sb, stt = stage.next()
            src = self.x_d[i * 128:(i + 1) * 128, :] if i < 16 else self.ctx_d[(i - 16) * 128:(i - 15) * 128, :]
            P.dma("sp", I_dma(sb, src), w=[stt])
            for half in range(2):
                ps, pt = self.PS.next()
                for j in range(4):
                    k = half * 4 + j
                    P.pe(I_tr(ps[:, j * 128:(j + 1) * 128], sb[:, k * 128:(k + 1) * 128], self.ident), r=[stt, self.t_c], w=[pt])
                fn = I_copy(self.xT3[:, half * 4:half * 4 + 4, i * 128:(i + 1) * 128], ps.rearrange("p (a b) -> p a b", a=4))
                P.dve(fn, r=[pt], w=[self.t_x[min(i // 4, 4)]])
        c2s = A.alloc(1024)
        tcs = Tok()
        P.dma("sp", I_dma(c2s[0:2, :], self.c2_d), w=[tcs])
        P.act(I_act(c2s[0:2, :], c2s[0:2, :], AF.Silu), r=[tcs], w=[tcs])
        ps, pt = self.PS.next()
        for k in range(8):
            P.pe(I_tr(ps[:, k * 2:k * 2 + 2], c2s[0:2, k * 128:(k + 1) * 128], self.ident[0:2, 0:2]), r=[tcs, self.t_c], w=[pt])
        P.dve(I_copy(self.csT, ps[:, 0:16]), r=[pt], w=[self.t_cs])
        P.barrier()
        A.release(m)

    def mod_phase(self, li):
        P, A = self.P, self.A
        m = A.mark()
        brow = A.alloc(6 * D)
        grow = A.alloc(3 * D)
        tb, tg = Tok(), Tok()
        P.dma("sp", I_dma(brow[0:1, :], self.W["b_mod"][li:li + 1, :]), w=[tb])
        P.dma("sp", I_dma(grow[0:1, 0:D], self.W["norm1_g"][li:li + 1, :]), w=[tg])
        P.dma("sp", I_dma(grow[0:1, D:2 * D], self.W["norm2_g"][li:li + 1, :]), w=[tg])
        P.dma("sp", I_dma(grow[0:1, 2 * D:3 * D], self.W["final_g"].rearrange("(o d) -> o d", o=1)), w=[tg])
        self.rows_to_cols(self.gvec, grow, 24, tg, self.t_gvec)
        wring = Ring([A.alloc(8 * 512) for _ in range(3)])
        psm, ptm = self.PS.next()
        for piece in range(12):
            wp, wt = wring.next()
            wp3 = wp.rearrange("p (k c) -> p k c", k=8)
            src = self.W["w_mod"][li][:, piece * 512:(piece + 1) * 512].rearrange("(k p) c -> p k c", p=128)
            P.dma("sp" if piece % 2 == 0 else "act", I_dma(wp3, src), w=[wt])
            for jj in range(4):
                j = piece * 4 + jj
                for kc in range(8):
                    P.pe(I_mm(psm[:, j * 2:j * 2 + 2], wp3[:, kc, jj * 128:(jj + 1) * 128], self.csT[:, kc * 2:kc * 2 + 2],
                              start=(kc == 0), stop=False), r=[wt, self.t_cs], w=[ptm])
                P.pe(I_mm(psm[:, j * 2:j * 2 + 2], brow[0:1, j * 128:(j + 1) * 128], self.ones[0:1, 0:2], start=False, stop=True),
                     r=[tb, self.t_c], w=[ptm])
        P.dve(I_copy(self.eff, psm[:, 0:96]), r=[ptm], w=[self.t_eff])
        for n, gi in ((1, 0), (4, 1)):
            P.dve(I_stt(self.eff4[:, n], self.eff4[:, n], 1.0,
                        self.gvec[:, gi * 8:(gi + 1) * 8].unsqueeze(2).to_broadcast([128, 8, 2]), ALU.add, ALU.mult),
                  r=[self.t_eff, self.t_gvec], w=[self.t_eff])
        P.barrier()
        A.release(m)

    def norm_phase(self, which, li, router=None):
        P, A = self.P, self.A
        m = A.mark()
        n_sh, n_sc = (0, 1) if which == 0 else (3, 4)
        sq = A.alloc(8 * 512)
        sq3 = sq.rearrange("p (k t) -> p k t", k=8)
        tsq = Tok()
        rstd_r = Ring([A.alloc(512) for _ in range(2)])
        tmp_r = Ring([A.alloc(512) for _ in range(3)])
        if router is not None:
            h2f = A.alloc(8 * 512)
            h2f3 = h2f.rearrange("p (k t) -> p k t", k=8)
            th2f = Tok()
        for tg, (s, n) in enumerate(TGS if which == 0 else self.tgs(li)):
            col = 0 if tg < 4 else 1
            P.act(I_act(sq3[:, :, 0:n], self.xT3[:, :, s:s + n], AF.Square), r=[self.t_x[tg]], w=[tsq])
            ps, pt = self.PS.next()
            for k in range(8):
                P.pe(I_mm(ps[:, 0:n], self.ones, sq3[:, k, 0:n], start=(k == 0), stop=(k == 7)), r=[tsq, self.t_c], w=[pt])
            rstd, trs = rstd_r.next()
            self.rsqrt(rstd[:, 0:n], ps[:, 0:n], 1.0 / D, 1e-6, [pt], trs)
            for k in range(8):
                tmp, tt = tmp_r.next()
                P.op("dve",
                     I_stt(tmp[:, 0:n], self.xT3[:, k, s:s + n], self.eff4[:, n_sc, k, col:col + 1], rstd[:, 0:n], ALU.mult, ALU.mult),
                     r=[self.t_x[tg], self.t_eff, trs], w=[tt])
                if router is None:
                    P.act(I_act(self.hT3[:, k, s:s + n], tmp[:, 0:n], AF.Identity, bias=self.eff4[:, n_sh, k, col:col + 1]),
                          r=[tt, self.t_eff], w=[self.t_h[tg]])
                else:
                    P.act(I_act(h2f3[:, k, 0:n], tmp[:, 0:n], AF.Identity, bias=self.eff4[:, n_sh, k, col:col + 1]),
                          r=[tt, self.t_eff], w=[th2f])
                    P.pool(I_copy(self.hT3[:, k, s:s + n], h2f3[:, k, 0:n]), r=[th2f], w=[self.t_h[tg]])
            if router is not None:
                router(tg, s, n, h2f3, th2f)
        P.barrier()
        A.release(m)


    def attn_phase(self, li):
        P, A = self.P, self.A
        m = A.mark()
        self.B.off = 0
        lam_init = 0.8 - 0.6 * math.exp(-0.3 * li)
        W = self.W["w_in"][li]
        wq = self.AL(8 * 512, BF16).rearrange("p (k c) -> p k c", k=8)
        wk = self.AL(8 * 512, BF16).rearrange("p (k c) -> p k c", k=8)
        wv = self.AL(8 * 512, BF16).rearrange("p (k c) -> p k c", k=8)
        tw = toks(3)
        self.load_w_bf16(wq, W[:, 0:512], tw[0])
        self.load_w_bf16(wk, W[:, 512:1024], tw[1])
        self.load_w_bf16(wv, W[:, 1024:1536], tw[2])
        cosT = self.AL(TL)
        sinT = self.AL(TL)
        pm = self.AL(128, BF16)
        trope = Tok()
        P.dma("sp", I_dma(cosT, self.Cd["cosT"]), w=[trope])
        P.dma("act", I_dma(sinT, self.Cd["sinT"]), w=[trope])
        P.dma("pool", I_dma(pm, self.Cd["pm"]), w=[trope])
        qT = self.AL(4 * T, BF16).rearrange("p (h t) -> p h t", h=4)
        kT = self.AL(4 * T, BF16).rearrange("p (h t) -> p h t", h=4)
        vaug = self.AL(NT * 4 * 130, BF16).rearrange("p (i h e) -> p i h e", i=NT, h=4)
        t_q, t_k, t_v = toks(5), toks(5), toks(NT)
        tv1 = Tok()
        P.pool(I_memset(vaug[:, :, :, 128:129], 1.0), w=t_v)
        rows = self.AL(512)
        trow = Tok()
        P.dma("sp", I_dma(rows[0:1, 0:256], self.W["da_lambda"][li:li + 1].rearrange("o a b -> o (a b)")), w=[trow])
        P.dma("sp", I_dma(rows[0:1, 256:384], self.W["da_subln_g"][li:li + 1, :]), w=[trow])
        lsm = self.AL(16)
        tl = Tok()
        P.dve(I_tt(rows[0:1, 384:448], rows[0:1, 0:64], rows[0:1, 64:128], ALU.mult), r=[trow], w=[trow])
        P.dve(I_tt(rows[0:1, 448:512], rows[0:1, 128:192], rows[0:1, 192:256], ALU.mult), r=[trow], w=[trow])
        P.dve(I_red(lsm[0:1, 0:2], rows[0:1, 384:512].rearrange("o (a b) -> o a b", a=2)), r=[trow], w=[tl])
        P.act(I_act(lsm[0:1, 2:4], lsm[0:1, 0:2], AF.Exp), r=[tl], w=[tl])
        P.dve(I_tt(lsm[0:1, 4:5], lsm[0:1, 3:4], lsm[0:1, 2:3], ALU.subtract), r=[tl], w=[tl])
        P.dve(I_ts(lsm[0:1, 4:5], lsm[0:1, 4:5], -lam_init, ALU.add), r=[tl], w=[tl])
        neglam = self.AL(8)
        gsub = self.AL(128)
        tng = Tok()
        self.bcast_rows(neglam[:, 0:1], lsm[:, 4:5], 1, tl, tng)
        self.bcast_rows(gsub, rows[:, 256:384], 128, trow, tng, scale=(1.0 - lam_init))
        raw_r = Ring([self.AL(512, BF16) for _ in range(2)])
        t1_r = Ring([self.AL(512) for _ in range(2)])
        t2_r = Ring([self.AL(512) for _ in range(2)])
        for (w3, twk, dstT, tdst) in ((wq, tw[0], qT, t_q), (wk, tw[1], kT, t_k)):
            for h in range(4):
                for tg, (s, n) in enumerate(TGS):
                    ps, pt = self.PS.next()
                    for kc in range(8):
                        P.pe(I_mm(ps[:, 0:n], w3[:, kc, h * 128:(h + 1) * 128], self.hT3[:, kc, s:s + n], start=(kc == 0), stop=(kc == 7)),
                             r=[twk, self.t_h[tg]], w=[pt])
                    if tg == 4:
                        P.act(I_acopy(dstT[:, h, s:s + n], ps[:, 0:n]), r=[pt], w=[tdst[tg]])
                        continue
                    raw, tr = raw_r.next()
                    P.act(I_acopy(raw[:, 0:n], ps[:, 0:n]), r=[pt], w=[tr])
                    ps2, pt2 = self.PS.next()
                    P.pe(I_mm(ps2[:, 0:n], pm, raw[:, 0:n]), r=[trope, tr], w=[pt2])
                    a1, ta1 = t1_r.next()
                    a2, ta2 = t2_r.next()
                    P.pool(I_tt(a1[:, 0:n], raw[:, 0:n], cosT[:, s:s + n], ALU.mult), r=[tr, trope], w=[ta1])
                    P.dve(I_tt(a2[:, 0:n], ps2[:, 0:n], sinT[:, s:s + n], ALU.mult), r=[pt2, trope], w=[ta2])
                    P.pool(I_tt(dstT[:, h, s:s + n], a1[:, 0:n], a2[:, 0:n], ALU.add), r=[ta1, ta2], w=[tdst[tg]])
        for i in range(NT):
            ps, pt = self.PS.next()
            for kc in range(8):
                P.pe(I_mm(ps, self.hT3[:, kc, i * 128:(i + 1) * 128], wv[:, kc, :], start=(kc == 0), stop=(kc == 7)),
                     r=[tw[2], self.t_h[min(i // 4, 4)]], w=[pt])
            P.act(I_acopy(vaug[:, i, :, 0:128], ps.rearrange("p (h e) -> p h e", h=4)), r=[pt], w=[t_v[i]])
        self.dbg("qT", qT.rearrange("p h t -> p (h t)"), t_q, [128, 4 * T], BF16)
        self.dbg("kT", kT.rearrange("p h t -> p (h t)"), t_k, [128, 4 * T], BF16)
        if self.stop == f"attnproj_{li}":
            P.barrier()
            A.release(m)
            return
        Ea_r = Ring([self.AL(NT * 256, BF16) for _ in range(2)])
        osb = [self.AL(4 * 130) for _ in range(2)]
        tosb = toks(2)
        oda = self.AL(4 * 512)
        oda3 = oda.rearrange("p (j c) -> p j c", j=4)
        toda = Tok()
        sm_r = Ring([self.AL(8) for _ in range(2)])
        o1_r = Ring([self.AL(128) for _ in range(2)])
        o2_r = Ring([self.AL(128) for _ in range(2)])
        stg_r = Ring([self.AL(512, BF16) for _ in range(2)])
        groups = [(s, 256, list(range(NT)), s // 512) for s in range(0, TL, 256)] + [(2048, 256, [16, 17], 4)]
        iters = [(qs0, qn, ktiles, tg, h, sm) for (qs0, qn, ktiles, tg) in groups for h in range(4) for sm in range(2)]
        Ea_bufs = [Ea_r.next() for _ in range(2)]

        def S_list(it):
            qs0, qn, ktiles, tg, h, sm = iters[it]
            R_ = slice(sm * 64, (sm + 1) * 64)
            Ea, tea = Ea_bufs[it % 2]
            Ea3 = Ea.rearrange("p (k q) -> p k q", k=NT)
            out = []
            for idx, kt in enumerate(ktiles):
                def f(idx=idx, kt=kt):
                    ps, pt = self.PS.next()
                    P.pe(I_mm(ps[:, 0:qn], kT[R_, h, kt * 128:(kt + 1) * 128], qT[R_, h, qs0:qs0 + qn]),
                         r=[t_k[min(kt // 4, 4)], t_q[tg]], w=[pt])
                    P.act(I_act(Ea3[:, idx, 0:qn], ps[:, 0:qn], AF.Exp, scale=0.125), r=[pt], w=[tea])
                out.append(f)
            return out

        def PV_list(it):
            qs0, qn, ktiles, tg, h, sm = iters[it]
            Ea, tea = Ea_bufs[it % 2]
            Ea3 = Ea.rearrange("p (k q) -> p k q", k=NT)
            out = []
            st_ = {}
            for j in range(qn // 128):
                for idx, kt in enumerate(ktiles):
                    def f(j=j, idx=idx, kt=kt):
                        if idx == 0:
                            st_[j] = self.PSacc.next()
                        ob, tob = st_[j]
                        P.pe(I_mm(ob[:, 0:129], Ea3[:, idx, j * 128:(j + 1) * 128], vaug[:, kt, h, 0:129],
                                  start=(idx == 0), stop=(idx == len(ktiles) - 1)), r=[tea, t_v[kt]], w=[tob])
                        if idx == len(ktiles) - 1:
                            P.dve(I_copy(osb[sm][:, j * 130:j * 130 + 129], ob[:, 0:129]), r=[tob], w=[tosb[sm]])
                    out.append(f)
            return out

        def post(it):
            qs0, qn, ktiles, tg, h, sm = iters[it]
            nq = qn // 128
            if sm == 0:
                return
            for j in range(nq):
                s8, ts8 = sm_r.next()
                o1, to1 = o1_r.next()
                o2, to2 = o2_r.next()
                P.dve(lambda e, o=s8[:, 0:1], i=osb[0][:, j * 130 + 128:j * 130 + 129]: e.reciprocal(out=o, in_=i), r=[tosb[0]], w=[ts8])
                P.dve(lambda e, o=s8[:, 1:2], i=osb[1][:, j * 130 + 128:j * 130 + 129]: e.reciprocal(out=o, in_=i), r=[tosb[1]], w=[ts8])
                P.dve(I_tt(s8[:, 2:3], s8[:, 1:2], neglam[:, 0:1], ALU.mult), r=[ts8, tng], w=[ts8])
                P.dve(I_ts(o1, osb[0][:, j * 130:j * 130 + 128], s8[:, 0:1], ALU.mult), r=[tosb[0], ts8], w=[to1])
                P.dve(I_stt(o1, osb[1][:, j * 130:j * 130 + 128], s8[:, 2:3], o1, ALU.mult, ALU.add), r=[tosb[1], ts8, to1], w=[to1])
                P.pool(I_tt(o2, o1, o1, ALU.mult), r=[to1], w=[to2])
                P.dve(I_red(s8[:, 3:4], o2), r=[to2], w=[ts8])
                self.rsqrt(s8[:, 4:5], s8[:, 3:4], 1.0 / 128, 1e-5, [ts8], ts8)
                P.dve(I_stt(oda3[:, j, h * 128:(h + 1) * 128], o1, s8[:, 4:5], gsub, ALU.mult, ALU.mult), r=[to1, ts8, tng], w=[toda])
            if h < 3:
                return
            for j in range(nq):
                ti = qs0 // 128 + j
                ps, pt = self.PS.next()
                for hh in range(4):
                    P.pe(I_tr(ps[:, hh * 128:(hh + 1) * 128], oda3[:, j, hh * 128:(hh + 1) * 128], self.ident), r=[toda, self.t_c], w=[pt])
                stg, tst = stg_r.next()
                P.act(I_acopy(stg, ps), r=[pt], w=[tst])
                P.dma("sp", I_dma(self.brT_d[0, :, :, ti * 128:(ti + 1) * 128].rearrange("k p t -> p k t"),
                                  stg.rearrange("p (k t) -> p k t", k=4)), r=[tst], w=[self.t_br[0]])

        for f in S_list(0):
            f()
        for it in range(len(iters)):
            Sn = S_list(it + 1) if it + 1 < len(iters) else []
            Pv = PV_list(it)
            ia = 0
            for ib, f in enumerate(Pv):
                f()
                want = (ib + 1) * len(Sn) // len(Pv)
                while ia < want:
                    Sn[ia]()
                    ia += 1
            while ia < len(Sn):
                Sn[ia]()
                ia += 1
            post(it)
        P.barrier()
        A.release(m)

    def conv_phase(self, li):
        P, A = self.P, self.A
        m = A.mark()
        self.B.off = 0
        W = self.W["w_in"][li]
        ws = [self.AL(8 * 512, BF16).rearrange("p (k c) -> p k c", k=8) for _ in range(3)]
        tw = toks(3)
        for i in range(3):
            self.load_w_bf16(ws[i], W[:, 1536 + i * 512:2048 + i * 512], tw[i])
        rows = self.AL(1536)
        trow = Tok()
        P.dma("sp", I_dma(rows[0:1, :], self.W["conv_w"][li:li + 1].rearrange("o a b -> o (a b)")), w=[trow])
        cw = self.AL(16)
        tcw = Tok()
        self.rows_to_cols(cw[:, 0:12], rows, 12, trow, tcw)
        ubuf = self.AL(2312)
        tub = Tok()
        P.pool(I_memset(ubuf, 0.0), w=[tub])
        sbT = self.AL(T, BF16)
        tsb = Tok()
        sg_r = Ring([self.AL(512) for _ in range(2)])
        c1 = self.AL(TL)
        tc1 = Tok()
        osc = self.AL(T, BF16)
        tosc = Tok()
        for ch in range(4):
            for tg, (s, n) in enumerate(TGS):
                pss = [self.PS.next() for _ in range(3)]
                for i in range(3):
                    for kc in range(8):
                        P.pe(I_mm(pss[i][0][:, 0:n], ws[i][:, kc, ch * 128:(ch + 1) * 128], self.hT3[:, kc, s:s + n],
                                  start=(kc == 0), stop=(kc == 7)), r=[tw[i], self.t_h[tg]], w=[pss[i][1]])
                off = 1 + s if tg < 4 else 2051
                sg, tsg = sg_r.next()
                P.act(I_acopy(sbT[:, s:s + n], pss[0][0][:, 0:n]), r=[pss[0][1]], w=[tsb])
                P.act(I_acopy(sg[:, 0:n], pss[1][0][:, 0:n]), r=[pss[1][1]], w=[tsg])
                P.dve(I_tt(ubuf[:, off:off + n], pss[2][0][:, 0:n], sg[:, 0:n], ALU.mult), r=[pss[2][1], tsg], w=[tub])
            for (o0, u0, n) in ((0, 0, TL), (TL, 2050, TC)):
                P.dve(I_ts(c1[:, 0:n], ubuf[:, u0:u0 + n], cw[:, ch:ch + 1], ALU.mult), r=[tub, tcw], w=[tc1])
                P.dve(I_stt(c1[:, 0:n], ubuf[:, u0 + 1:u0 + 1 + n], cw[:, 4 + ch:5 + ch], c1[:, 0:n], ALU.mult, ALU.add), r=[tub, tcw], w=[tc1])
                P.dve(I_stt(c1[:, 0:n], ubuf[:, u0 + 2:u0 + 2 + n], cw[:, 8 + ch:9 + ch], c1[:, 0:n], ALU.mult, ALU.add), r=[tub, tcw], w=[tc1])
                P.pool(I_tt(osc[:, o0:o0 + n], c1[:, 0:n], sbT[:, o0:o0 + n], ALU.mult), r=[tc1, tsb], w=[tosc])
            P.dma("sp", I_dma(self.brT_d[1, ch], osc), r=[tosc], w=[self.t_br[1]])
        P.barrier()
        A.release(m)

    def merge_phase(self, li):
        P, A = self.P, self.A
        m = A.mark()
        W = self.W["w_in"][li]
        wg = A.alloc(8 * 1024, BF16).rearrange("p (k c) -> p k c", k=8)
        wb = A.alloc(4 * 1024, BF16).rearrange("p (k c) -> p k c", k=4)
        twg, twb = Tok(), Tok()
        mg = A.alloc(8 * T, BF16).rearrange("p (k t) -> p k t", k=8)
        tmg = toks(5)
        br_r = Ring([A.alloc(4 * 512, BF16) for _ in range(2)])
        gs_r = Ring([A.alloc(512) for _ in range(2)])
        for n_ in range(3):
            self.load_w_bf16(wg, W[:, 4608 + n_ * 1024:4608 + (n_ + 1) * 1024], twg)
            self.load_w_bf16(wb, self.W["w_branch"][li, n_], twb, kc=4)
            for tg, (s, n) in enumerate(self.tgs(li)):
                br, tbr = br_r.next()
                br3 = br.rearrange("p (k t) -> p k t", k=4)
                P.dma("sp", I_dma(br3[:, :, 0:n], self.brT_d[n_, :, :, s:s + n].rearrange("k p t -> p k t")), r=[self.t_br[n_]], w=[tbr])
                for cc in range(8):
                    psg, ptg = self.PS.next()
                    for kc in range(8):
                        P.pe(I_mm(psg[:, 0:n], wg[:, kc, cc * 128:(cc + 1) * 128], self.hT3[:, kc, s:s + n], start=(kc == 0), stop=(kc == 7)),
                             r=[twg, self.t_h[tg]], w=[ptg])
                    gs, tgs = gs_r.next()
                    P.act(I_act(gs[:, 0:n], psg[:, 0:n], AF.Sigmoid), r=[ptg], w=[tgs])
                    psp, ptp = self.PS.next()
                    for kc in range(4):
                        P.pe(I_mm(psp[:, 0:n], wb[:, kc, cc * 128:(cc + 1) * 128], br3[:, kc, 0:n], start=(kc == 0), stop=(kc == 3)),
                             r=[twb, tbr], w=[ptp])
                    if n_ == 0:
                        P.dve(I_tt(mg[:, cc, s:s + n], psp[:, 0:n], gs[:, 0:n], ALU.mult), r=[ptp, tgs], w=[tmg[tg]])
                    else:
                        P.dve(I_tt(gs[:, 0:n], psp[:, 0:n], gs[:, 0:n], ALU.mult), r=[ptp, tgs], w=[tgs])
                        P.pool(I_tt(mg[:, cc, s:s + n], mg[:, cc, s:s + n], gs[:, 0:n], ALU.add), r=[tmg[tg], tgs], w=[tmg[tg]])
        wo = wg
        self.load_w_bf16(wo, self.W["w_out"][li], twg)
        for tg, (s, n) in enumerate(self.tgs(li)):
            col = 0 if tg < 4 else 1
            for cc in range(8):
                ps, pt = self.PS.next()
                for kc in range(8):
                    P.pe(I_mm(ps[:, 0:n], wo[:, kc, cc * 128:(cc + 1) * 128], mg[:, kc, s:s + n], start=(kc == 0), stop=(kc == 7)),
                         r=[twg, tmg[tg]], w=[pt])
                P.dve(I_stt(self.xT3[:, cc, s:s + n], ps[:, 0:n], self.eff4[:, 2, cc, col:col + 1], self.xT3[:, cc, s:s + n], ALU.mult, ALU.add),
                      r=[pt, self.t_eff, self.t_x[tg]], w=[self.t_x[tg]])
        P.barrier()
        A.release(m)

    def final_phase(self):
        P, A = self.P, self.A
        m = A.mark()
        sq = A.alloc(8 * 512)
        sq3 = sq.rearrange("p (k t) -> p k t", k=8)
        tsq = Tok()
        rstd_r = Ring([A.alloc(512) for _ in range(2)])
        y_r = Ring([A.alloc(8 * 512) for _ in range(2)])
        o_r = Ring([A.alloc(1024) for _ in range(3)])
        for tg, (s, n) in enumerate(TGS[:4]):
            P.act(I_act(sq3[:, :, 0:n], self.xT3[:, :, s:s + n], AF.Square), r=[self.t_x[tg]], w=[tsq])
            ps, pt = self.PS.next()
            for k in range(8):
                P.pe(I_mm(ps[:, 0:n], self.ones, sq3[:, k, 0:n], start=(k == 0), stop=(k == 7)), r=[tsq, self.t_c], w=[pt])
            rstd, trs = rstd_r.next()
            self.rsqrt(rstd[:, 0:n], ps[:, 0:n], 1.0 / D, 1e-6, [pt], trs)
            y, ty = y_r.next()
            y3 = y.rearrange("p (k t) -> p k t", k=8)
            for k in range(8):
                P.dve(I_stt(y3[:, k, 0:n], self.xT3[:, k, s:s + n], self.gvec[:, 16 + k:17 + k], rstd[:, 0:n], ALU.mult, ALU.mult),
                      r=[self.t_x[tg], self.t_gvec, trs], w=[ty])
            for j in range(n // 128):
                ti = s // 128 + j
                ob, tob = o_r.next()
                for half in range(2):
                    ps, pt = self.PS.next()
                    for q in range(4):
                        k = half * 4 + q
                        P.pe(I_tr(ps[:, q * 128:(q + 1) * 128], y3[:, k, j * 128:(j + 1) * 128], self.ident), r=[ty, self.t_c], w=[pt])
                    P.act(I_acopy(ob[:, half * 512:(half + 1) * 512], ps), r=[pt], w=[tob])
                to = Tok()
                P.dma("sp", I_dma(self.out_d[ti * 128:(ti + 1) * 128, :], ob), r=[tob], w=[to])
                self.out_toks.append(to)
        P.barrier()
        A.release(m)

    def rwkv_phase(self, li):
        P, A = self.P, self.A
        m = A.mark()
        W = self.W["w_in"][li]
        DS = 0.606531
        self.B.off = 0
        B = self.B
        AL = self.AL
        a512 = lambda dt=F32: AL(512, dt)
        wrkv = [A.alloc(8 * 512, BF16).rearrange("p (k c) -> p k c", k=8) for _ in range(3)]
        tw = toks(3)
        for i in range(3):
            self.load_w_bf16(wrkv[i], W[:, 3072 + i * 512:3584 + i * 512], tw[i])
        lo1 = A.alloc(8 * 416, BF16).rearrange("p (k c) -> p k c", k=8)
        tlo = Tok()
        for d in range(2):
            self.load_w_bf16(lo1[:, :, d * 64:(d + 1) * 64], self.W["rw_w1"][li, d], tlo)
            self.load_w_bf16(lo1[:, :, 128 + d * 64:128 + (d + 1) * 64], self.W["rw_a1"][li, d], tlo)
        self.load_w_bf16(lo1[:, :, 256:416], self.W["rw_g1"][li], tlo)
        lo2 = A.alloc(6 * 512, BF16).rearrange("p (k c) -> p k c", k=6)
        for d in range(2):
            P.dma("pool", I_dma(lo2[0:64, d, :], self.W["rw_w2"][li, d]), w=[tlo])
            P.dma("pool", I_dma(lo2[0:64, 2 + d, :], self.W["rw_a2"][li, d]), w=[tlo])
        P.dma("pool", I_dma(lo2[:, 4, :], self.W["rw_g2"][li, 0:128, :]), w=[tlo])
        P.dma("pool", I_dma(lo2[0:32, 5, :], self.W["rw_g2"][li, 128:160, :]), w=[tlo])
        brow = A.alloc(4 * 512, BF16)
        P.dma("pool", I_dma(brow[0:1, 0:1024], self.W["rw_w0"][li:li + 1].rearrange("o a b -> o (a b)")), w=[tlo])
        P.dma("pool", I_dma(brow[0:1, 1024:2048], self.W["rw_a0"][li:li + 1].rearrange("o a b -> o (a b)")), w=[tlo])
        rows = A.alloc(5 * 512, BF16)
        trow = Tok()
        for i, nm in enumerate(["rw_k_k", "rw_k_a", "rw_lnx_g", "rw_lnx_b"]):
            P.dma("pool", I_dma(rows[0:1, i * 512:(i + 1) * 512], self.W[nm][li:li + 1, :]), w=[trow])
        P.dma("pool", I_dma(rows[0:1, 2048:2560], self.W["rw_r_k"][li:li + 1].rearrange("o a b -> o (a b)")), w=[trow])
        bc = [A.alloc(512) for _ in range(5)]
        tbc = Tok()
        for i in range(5):
            self.bcast_rows(bc[i], rows[:, i * 512:(i + 1) * 512], 512, trow, tbc, ones=self.ones_b)
        kk_bc, ka_bc, lg_bc, lb_bc, rk_bc = bc
        A.release(A.mark())
        msk = {}
        tmk = Tok()
        for d, (nS, nI, nST) in enumerate((("m_f_strict", "m_f_incl", "m_r_strict"), ("m_r_strict", "m_r_incl", "m_f_strict"))):
            mc1, mc2, mn = A.alloc(256, BF16), A.alloc(256, BF16), A.alloc(128, BF16)
            tri = A.alloc(128)
            P.dma("pool", I_dma(mc2[:, 0:128], self.Cd[nS]), w=[tmk])
            P.dma("pool", I_dma(mc2[:, 128:256], self.Cd[nI]), w=[tmk])
            P.dma("pool", I_dma(mn, self.Cd[nST]), w=[tmk])
            P.dma("sp", I_dma(tri, self.Cd[nI]), w=[tmk])
            P.dve(I_ts(mc1[:, 0:128], mc2[:, 0:128], -1.0, ALU.mult), r=[tmk], w=[tmk])
            P.dve(I_copy(mc1[:, 128:256], mc2[:, 128:256]), r=[tmk], w=[tmk])
            P.dve(I_ts(mn, mn, -1.0, ALU.mult), r=[tmk], w=[tmk])
            P.dve(I_ts(tri, tri, -DS, ALU.mult), r=[tmk], w=[tmk])
            msk[d] = (mc1, mc2, mn, tri)
        allneg = A.alloc(128)
        P.dve(I_ts(allneg, self.ones, -DS, ALU.mult), r=[self.t_c], w=[tmk])
        Mst = [[A.alloc(512) for _ in range(2)] for _ in range(2)]
        tM = [toks(2) for _ in range(2)]
        for d in range(2):
            P.pool(I_memset(Mst[d][0], 0.0), w=[tM[d][0]])
        mpar = [0, 0]
        r_s, k_s, v_s, sig, a_s, kx, kk, b_s, kE, tA, tB, cum_s, cumC_s = [a512() for _ in range(13)]
        tk_ = {n: Tok() for n in "r k v sig a kx kk b kE tA tB cum cumC l1 la e1 e2 e3 e4 kt bt k2 rt kh bh vb ktb rtb fT pc dg".split()}
        l1 = AL(128, BF16)
        la = AL(128, BF16)
        e1, e2, e3, e4 = [a512(BF16) for _ in range(4)]
        kt_, bt_, k2_, rt_ = [a512(BF16) for _ in range(4)]
        kh_, bh_, vb_ = [a512(BF16) for _ in range(3)]
        red8 = A.alloc(16)
        tred = Tok()
        pcb = a512()
        dg = a512()
        fT = [AL(4 * 128, BF16).rearrange("p (q t) -> p q t", q=4) for _ in range(4)]
        tfT = toks(4)
        hb = []
        for h in range(8):
            d_ = {}
            for nm, sz in (("X0", 256), ("X1", 256), ("P0", 128), ("P1", 128), ("A1", 256), ("A2", 256), ("KAV", 128), ("WU", 128)):
                d_[nm] = AL(sz, BF16)
            d_["t"] = {nm: Tok() for nm in ("X0", "X1", "P0", "P1", "A1", "A2", "KAV", "WU")}
            hb.append(d_)
        PhiT = AL(512)
        Gall = AL(512)
        QT = AL(8 * 128).rearrange("p (h t) -> p h t", h=8)
        Y0s = a512()
        yout = AL(1024)
        tPhi, tG, tQT, tY0, tyo = Tok(), Tok(), Tok(), Tok(), Tok()
        tys = [[Tok() for _ in range(NT)] for _ in range(2)]
        order = [[16, 17] + list(range(16)), [17, 16] + list(range(15, -1, -1))]
        nsteps = NT if self.stop != f"rw1_{li}" else 3
        for step in range(nsteps):
            for d in range(2):
                i = order[d][step]
                mc1, mc2, mn, tri = msk[d]
                tgi = min(i // 4, 4)
                th = self.t_h[tgi]
                cols = slice(i * 128, (i + 1) * 128)
                for q, (dst, tn) in enumerate(((r_s, "r"), (k_s, "k"), (v_s, "v"))):
                    ps, pt = self.PS.next()
                    for kc in range(8):
                        P.pe(I_mm(ps, self.hT3[:, kc, cols], wrkv[q][:, kc, :], start=(kc == 0), stop=(kc == 7)), r=[tw[q], th], w=[pt])
                    P.act(I_acopy(dst, ps), r=[pt], w=[tk_[tn]])
                P.pool(I_copy(vb_, v_s), r=[tk_["v"]], w=[tk_["vb"]])
                for (c0, dst, tn, fn) in ((d * 64, l1, "l1", AF.Tanh), (128 + d * 64, la, "la", AF.Copy)):
                    ps, pt = self.PS.next()
                    for kc in range(8):
                        P.pe(I_mm(ps[0:64, 0:128], lo1[:, kc, c0:c0 + 64], self.hT3[:, kc, cols], start=(kc == 0), stop=(kc == 7)), r=[tlo, th], w=[pt])
                    P.act(I_act(dst[0:64, :], ps[0:64, 0:128], fn), r=[pt], w=[tk_[tn]])
                for (src, tn_s, wi, bi, dst, tn) in ((l1, "l1", d, d, sig, "sig"), (la, "la", 2 + d, 2 + d, a_s, "a")):
                    ps, pt = self.PS.next()
                    P.pe(I_mm(ps, src[0:64, :], lo2[0:64, wi, :], start=True, stop=False), r=[tk_[tn_s], tlo], w=[pt])
                    P.pe(I_mm(ps, self.ones_b[0:1, 0:128], brow[0:1, bi * 512:(bi + 1) * 512], start=False, stop=True), r=[tlo, self.t_c], w=[pt])
                    P.act(I_act(dst, ps, AF.Sigmoid), r=[pt], w=[tk_[tn]])
                P.pool(I_tt(kx, k_s, kk_bc, ALU.mult), r=[tk_["k"], tbc], w=[tk_["kx"]])
                P.pool(I_tt(tA, kx, kx, ALU.mult), r=[tk_["kx"]], w=[tk_["tA"]])
                P.dve(I_red(red8[:, 0:8], tA.rearrange("p (h j) -> p h j", h=8)), r=[tk_["tA"]], w=[tred])
                P.dve(I_ts(red8[:, 0:8], red8[:, 0:8], 1e-24, ALU.max), r=[tred], w=[tred])
                self.rsqrt(red8[:, 0:8], red8[:, 0:8], 1.0, 1e-24, [tred], tred)
                P.dve(I_tt(kk.rearrange("p (h j) -> p h j", h=8), kx.rearrange("p (h j) -> p h j", h=8),
                           red8[:, 0:8].unsqueeze(2).to_broadcast([128, 8, 64]), ALU.mult), r=[tk_["kx"], tred], w=[tk_["kk"]])
                P.pool(I_tt(b_s, kk, a_s, ALU.mult), r=[tk_["kk"], tk_["a"]], w=[tk_["b"]])
                P.dve(I_stt(tB, a_s, -1.0, ka_bc, ALU.add, ALU.mult), r=[tk_["a"], tbc], w=[tk_["tB"]])
                P.dve(I_stt(kE, tB, 1.0, k_s, ALU.add, ALU.mult), r=[tk_["tB"], tk_["k"]], w=[tk_["kE"]])
                P.pool(I_tt(tA, r_s, kE, ALU.mult), r=[tk_["r"], tk_["kE"], tred], w=[tk_["tA"]])
                P.pool(I_tt(tA, tA, rk_bc, ALU.mult), r=[tk_["tA"], tbc], w=[tk_["tA"]])
                P.dve(I_red(red8[:, 8:16], tA.rearrange("p (h j) -> p h j", h=8)), r=[tk_["tA"]], w=[tred])
                P.dve(I_tt(yout[:, 512:1024].rearrange("p (h j) -> p h j", h=8), v_s.rearrange("p (h j) -> p h j", h=8),
                           red8[:, 8:16].unsqueeze(2).to_broadcast([128, 8, 64]), ALU.mult), r=[tk_["v"], tred], w=[tyo])
                psc, ptc = self.PS.next()
                psC, ptC = self.PS.next()
                P.pe(I_mm(psc, tri, sig), r=[tmk, tk_["sig"]], w=[ptc])
                P.pe(I_mm(psC, allneg, sig), r=[tmk, tk_["sig"]], w=[ptC])
                P.act(I_acopy(cum_s, psc), r=[ptc], w=[tk_["cum"]])
                P.act(I_acopy(cumC_s, psC), r=[ptC], w=[tk_["cumC"]])
                P.dve(I_stt(tB, sig, DS, cum_s, ALU.mult, ALU.add), r=[tk_["sig"], tk_["cum"], tk_["kE"]], w=[tk_["tB"]])
                P.act(I_act(e1, tB, AF.Exp), r=[tk_["tB"]], w=[tk_["e1"]])
                P.act(I_act(e2, cum_s, AF.Exp, scale=-1.0), r=[tk_["cum"]], w=[tk_["e2"]])
                P.act(I_act(e3, cum_s, AF.Exp), r=[tk_["cum"]], w=[tk_["e3"]])
                P.dve(I_tt(tA, cumC_s, cum_s, ALU.subtract), r=[tk_["cumC"], tk_["cum"], tred], w=[tk_["tA"]])
                P.act(I_act(e4, tA, AF.Exp), r=[tk_["tA"]], w=[tk_["e4"]])
                P.act(I_act(pcb, cumC_s, AF.Exp), r=[tk_["cumC"]], w=[tk_["pc"]])
                P.pool(I_tt(dg[0:64, :].rearrange("p (h j) -> p h j", h=8), pcb[0:64, :].rearrange("p (h j) -> p h j", h=8),
                            self.ident[0:64, 0:64].unsqueeze(1).to_broadcast([64, 8, 64]), ALU.mult), r=[tk_["pc"], self.t_c], w=[tk_["dg"]])
                P.dve(I_tt(kt_, kk, e1, ALU.mult), r=[tk_["kk"], tk_["e1"]], w=[tk_["kt"]])
                P.pool(I_tt(bt_, b_s, e2, ALU.mult), r=[tk_["b"], tk_["e2"]], w=[tk_["bt"]])
                P.dve(I_tt(k2_, kE, e2, ALU.mult), r=[tk_["kE"], tk_["e2"]], w=[tk_["k2"]])
                P.pool(I_tt(rt_, r_s, e3, ALU.mult), r=[tk_["r"], tk_["e3"]], w=[tk_["rt"]])
                P.dve(I_tt(kh_, kE, e4, ALU.mult), r=[tk_["kE"], tk_["e4"]], w=[tk_["kh"]])
                P.pool(I_tt(bh_, b_s, e4, ALU.mult), r=[tk_["b"], tk_["e4"]], w=[tk_["bh"]])
                for g in range(4):
                    ps, pt = self.PS.next()
                    psb = ps.bitcast(BF16)
                    for q, (src, tn) in enumerate(((kt_, "kt"), (rt_, "rt"), (bt_, "bt"), (k2_, "k2"))):
                        P.pe(I_tr(psb[:, q * 128:(q + 1) * 128], src[:, g * 128:(g + 1) * 128], self.ident_b), r=[tk_[tn], self.t_c], w=[pt])
                    P.op("act" if g % 2 == 0 else "dve", (I_acopy if g % 2 == 0 else I_copy)(fT[g].rearrange("p q t -> p (q t)"), psb[:, 0:512]), r=[pt], w=[tfT[g]])
                for h in range(8):
                    g, R_ = h // 2, slice((h % 2) * 64, (h % 2) * 64 + 64)
                    H = hb[h]
                    f = fT[g]
                    ps1, pt1 = self.PS.next()
                    P.pe(I_mm(ps1[:, 0:256], f[R_, 2, :], f[R_, 0:2, :].rearrange("p q t -> p (q t)")), r=[tfT[g]], w=[pt1])
                    P.dve(I_tt(H["A1"], ps1[:, 0:256], mc1, ALU.mult), r=[pt1, tmk], w=[H["t"]["A1"]])
                    ps2, pt2 = self.PS.next()
                    P.pe(I_mm(ps2[:, 0:256], f[R_, 3, :], f[R_, 0:2, :].rearrange("p q t -> p (q t)")), r=[tfT[g]], w=[pt2])
                    P.dve(I_tt(H["A2"], ps2[:, 0:256], mc2, ALU.mult), r=[pt2, tmk], w=[H["t"]["A2"]])
                    ps3, pt3 = self.PS.next()
                    P.pe(I_mm(ps3[:, 0:128], f[R_, 0, :], f[R_, 2, :]), r=[tfT[g]], w=[pt3])
                    P.dve(I_tt(H["X0"][:, 0:128], ps3[:, 0:128], mn, ALU.mult), r=[pt3, tmk], w=[H["t"]["X0"]])
                    P.pool(I_copy(H["X0"][:, 128:256], H["A1"][:, 0:128]), r=[H["t"]["A1"]], w=[H["t"]["X0"]])
                    P.pool(I_tt(H["P0"], H["A1"][:, 0:128], self.ident_b, ALU.add), r=[H["t"]["A1"], self.t_c], w=[H["t"]["P0"]])
                cur = 0
                for lvl in range(1, 7):
                    nxt = 1 - cur
                    for h in range(8):
                        H = hb[h]
                        XNc, XNn = H[f"X{cur}"], H[f"X{nxt}"]
                        tXc, tXn = H["t"][f"X{cur}"], H["t"][f"X{nxt}"]
                        Nc, Xc = XNc[:, 0:128], XNc[:, 128:256]
                        psA, ptA = self.PS.next()
                        P.pe(I_mm(psA[:, 0:128], Xc, Nc), r=[tXc], w=[ptA])
                        w_ = 128
                        if lvl < 6:
                            P.pe(I_mm(psA[:, 128:256], Nc, Xc), r=[tXc], w=[ptA])
                            w_ = 256
                        if h % 2 == 0:
                            P.act(I_acopy(XNn[:, 0:w_], psA[:, 0:w_]), r=[ptA], w=[tXn])
                        else:
                            P.dve(I_copy(XNn[:, 0:w_], psA[:, 0:w_]), r=[ptA], w=[tXn])
                    for h in range(8):
                        H = hb[h]
                        XNn, Pc, Pn = H[f"X{nxt}"], H[f"P{cur}"], H[f"P{nxt}"]
                        tXn, tPc, tPn = H["t"][f"X{nxt}"], H["t"][f"P{cur}"], H["t"][f"P{nxt}"]
                        psP, ptP = self.PS.next()
                        P.pe(I_mm(psP[:, 0:128], XNn[:, 0:128], Pc), r=[tXn, tPc], w=[ptP])
                        P.dve(I_tt(Pn, psP[:, 0:128], Pc, ALU.add), r=[ptP, tPc], w=[tPn])
                    cur = nxt
                for h in range(8):
                    hc = slice(h * 64, (h + 1) * 64)
                    H = hb[h]
                    P.pool(I_copy(H["KAV"][:, 0:64], kt_[:, hc]), r=[tk_["kt"]], w=[H["t"]["KAV"]])
                    ps, pt = self.PS.next()
                    P.pe(I_mm(ps[:, 0:64], H["A2"][:, 0:128], vb_[:, hc]), r=[H["t"]["A2"], tk_["vb"]], w=[pt])
                    P.act(I_acopy(H["KAV"][:, 64:128], ps[:, 0:64]), r=[pt], w=[H["t"]["KAV"]])
                for h in range(8):
                    H = hb[h]
                    TT, tTT = H[f"P{cur}"], H["t"][f"P{cur}"]
                    ps, pt = self.PS.next()
                    P.pe(I_mm(ps[:, 0:128], TT, H["KAV"]), r=[tTT, H["t"]["KAV"]], w=[pt])
                    P.dve(I_ts(H["WU"], ps[:, 0:128], -1.0, ALU.mult), r=[pt], w=[H["t"]["WU"]])
                psY0, ptY0 = self.PSacc.next()
                psPhi, ptPhi = self.PSacc.next()
                psG, ptG = self.PSacc.next()
                for h in range(8):
                    hc = slice(h * 64, (h + 1) * 64)
                    H = hb[h]
                    tWU = H["t"]["WU"]
                    ps, pt = self.PS.next()
                    P.pe(I_mm(ps[0:64, 0:128], H["WU"][:, 0:64], H["A1"][:, 128:256], start=True, stop=False), r=[tWU, H["t"]["A1"]], w=[pt])
                    P.pe(I_mm(ps[0:64, 0:128], rt_[:, hc], self.ident_b, start=False, stop=True), r=[tk_["rt"], self.t_c], w=[pt])
                    P.act(I_acopy(QT[0:64, h, :], ps[0:64, 0:128]), r=[pt], w=[tQT])
                    P.pe(I_mm(psY0[:, hc], H["A1"][:, 128:256], H["WU"][:, 64:128], start=True, stop=False), r=[tWU, H["t"]["A1"]], w=[ptY0])
                    P.pe(I_mm(psY0[:, hc], H["A2"][:, 128:256], vb_[:, hc], start=False, stop=True), r=[H["t"]["A2"], tk_["vb"]], w=[ptY0])
                    P.pe(I_mm(psPhi[0:64, hc], H["WU"][:, 0:64], bh_[:, hc]), r=[tWU, tk_["bh"]], w=[ptPhi])
                    P.pe(I_mm(psG[0:64, hc], bh_[:, hc], H["WU"][:, 64:128], start=True, stop=False), r=[tWU, tk_["bh"]], w=[ptG])
                    P.pe(I_mm(psG[0:64, hc], kh_[:, hc], vb_[:, hc], start=False, stop=True), r=[tk_["kh"], tk_["vb"]], w=[ptG])
                P.dve(I_copy(Y0s, psY0), r=[ptY0], w=[tY0])
                P.dve(I_tt(PhiT[0:64, :], psPhi[0:64, :], dg[0:64, :], ALU.add), r=[ptPhi, tk_["dg"]], w=[tPhi])
                P.act(I_acopy(Gall[0:64, :], psG[0:64, :]), r=[ptG], w=[tG])
                pc_, pn_ = mpar[d], 1 - mpar[d]
                Mc, Mn = Mst[d][pc_], Mst[d][pn_]
                tMc, tMn = tM[d][pc_], tM[d][pn_]
                psY, ptY = self.PSacc.next()
                for h in range(8):
                    hc = slice(h * 64, (h + 1) * 64)
                    P.pe(I_mm(psY[:, hc], QT[0:64, h, :], Mc[0:64, hc]), r=[tQT, tMc], w=[ptY])
                P.dve(I_tt(yout[:, 0:512], psY, Y0s, ALU.add), r=[ptY, tY0], w=[tyo])
                psM, ptM = self.PS.next()
                for h in range(8):
                    hc = slice(h * 64, (h + 1) * 64)
                    P.pe(I_mm(psM[0:64, hc], PhiT[0:64, hc], Mc[0:64, hc]), r=[tPhi, tMc], w=[ptM])
                P.dve(I_tt(Mn[0:64, :], psM[0:64, :], Gall[0:64, :], ALU.add), r=[ptM, tG], w=[tMn])
                mpar[d] = pn_
                P.dma("sp", I_dma(self.yscr_d[d, i], yout), r=[tyo], w=[tys[d][i]])
        P.barrier()
        B.off = 0
        ya_r = Ring([AL(1024) for _ in range(2)])
        yb_r = Ring([AL(1024) for _ in range(2)])
        gs1 = AL(128, BF16)
        gs2 = AL(128, BF16)
        tgs = Tok()
        stg_r = Ring([AL(512, BF16) for _ in range(2)])
        yc = a512()
        tyc = Tok()
        for i in range(NT):
            if nsteps < NT and i not in (16, 17):
                continue
            cols = slice(i * 128, (i + 1) * 128)
            th = self.t_h[min(i // 4, 4)]
            ya, tya = ya_r.next()
            yb, tyb = yb_r.next()
            P.dma("sp", I_dma(ya, self.yscr_d[0, i]), r=[tys[0][i]], w=[tya])
            P.dma("act", I_dma(yb, self.yscr_d[1, i]), r=[tys[1][i]], w=[tyb])
            P.pool(I_tt(ya, ya, yb, ALU.add), r=[tya, tyb], w=[tya])
            y3 = ya[:, 0:512].rearrange("p (h j) -> p h j", h=8)
            P.dve(I_red(red8[:, 0:8], y3), r=[tya], w=[tred])
            P.dve(I_ts(red8[:, 0:8], red8[:, 0:8], 1.0 / 64, ALU.mult), r=[tred], w=[tred])
            P.dve(I_tt(yc.rearrange("p (h j) -> p h j", h=8), y3, red8[:, 0:8].unsqueeze(2).to_broadcast([128, 8, 64]), ALU.subtract),
                  r=[tya, tred], w=[tyc])
            P.pool(I_tt(yb[:, 0:512], yc, yc, ALU.mult), r=[tyc], w=[tyb])
            P.dve(I_red(red8[:, 8:16], yb[:, 0:512].rearrange("p (h j) -> p h j", h=8)), r=[tyb], w=[tred])
            self.rsqrt(red8[:, 8:16], red8[:, 8:16], 1.0 / 64, 64e-5, [tred], tred)
            P.dve(I_tt(yc.rearrange("p (h j) -> p h j", h=8), yc.rearrange("p (h j) -> p h j", h=8),
                       red8[:, 8:16].unsqueeze(2).to_broadcast([128, 8, 64]), ALU.mult), r=[tyc, tred], w=[tyc])
            P.pool(I_tt(yc, yc, lg_bc, ALU.mult), r=[tyc, tbc], w=[tyc])
            P.pool(I_tt(yc, yc, lb_bc, ALU.add), r=[tyc, tbc], w=[tyc])
            P.pool(I_tt(yc, yc, ya[:, 512:1024], ALU.add), r=[tyc, tya], w=[tyc])
            ps, pt = self.PS.next()
            for kc in range(8):
                P.pe(I_mm(ps[:, 0:128], lo1[:, kc, 256:384], self.hT3[:, kc, cols], start=(kc == 0), stop=(kc == 7)), r=[tlo, th], w=[pt])
            P.act(I_act(gs1, ps[:, 0:128], AF.Sigmoid), r=[pt], w=[tgs])
            ps, pt = self.PS.next()
            for kc in range(8):
                P.pe(I_mm(ps[0:32, 0:128], lo1[:, kc, 384:416], self.hT3[:, kc, cols], start=(kc == 0), stop=(kc == 7)), r=[tlo, th], w=[pt])
            P.act(I_act(gs2[0:32, :], ps[0:32, 0:128], AF.Sigmoid), r=[pt], w=[tgs])
            ps, pt = self.PS.next()
            P.pe(I_mm(ps, gs1, lo2[:, 4, :], start=True, stop=False), r=[tgs, tlo], w=[pt])
            P.pe(I_mm(ps, gs2[0:32, :], lo2[0:32, 5, :], start=False, stop=True), r=[tgs, tlo], w=[pt])
            P.dve(I_tt(yc, yc, ps, ALU.mult), r=[tyc, pt], w=[tyc])
            ps, pt = self.PS.next()
            for g in range(4):
                P.pe(I_tr(ps[:, g * 128:(g + 1) * 128], yc[:, g * 128:(g + 1) * 128], self.ident), r=[tyc, self.t_c], w=[pt])
            stg, tst = stg_r.next()
            P.act(I_acopy(stg, ps), r=[pt], w=[tst])
            P.dma("sp", I_dma(self.brT_d[2, :, :, cols].rearrange("k p t -> p k t"), stg.rearrange("p (k t) -> p k t", k=4)),
                  r=[tst], w=[self.t_br[2]])
        P.barrier()
        A.release(m)

    def moe_router_setup(self, li):
        P, A = self.P, self.A
        self.wr = A.alloc(8 * 32).rearrange("p (k c) -> p k c", k=8)
        self.t_wr = Tok()
        P.dma("sp", I_dma(self.wr, self.W["router_w"][li].rearrange("(k p) c -> p k c", p=128)), w=[self.t_wr])
        self.rbrow = A.alloc(32)
        P.dma("sp", I_dma(self.rbrow[0:1, :], self.W["router_b"][li:li + 1, :]), w=[self.t_wr])
        self.GT = A.alloc(T)
        self.t_GT = Tok()
        self.rt_r = Ring([A.alloc(128) for _ in range(2)])

    def router_cb(self, tg, s, n, h2f3, th2f):
        P = self.P
        for j in range(n // 128):
            c0 = s + j * 128
            ps, pt = self.PS.next()
            for kc in range(8):
                P.pe(I_mm(ps[:, 0:32], h2f3[:, kc, j * 128:(j + 1) * 128], self.wr[:, kc, :], start=(kc == 0), stop=False),
                     r=[th2f, self.t_wr], w=[pt])
            P.pe(I_mm(ps[:, 0:32], self.ones[0:1, 0:128], self.rbrow[0:1, 0:32], start=False, stop=True), r=[self.t_wr, self.t_c], w=[pt])
            wk, twk = self.rt_r.next()
            lg, t8, msk, e_, sm = wk[:, 0:32], wk[:, 32:40], wk[:, 40:72], wk[:, 72:104], wk[:, 104:108]
            P.dve(I_copy(lg, ps[:, 0:32]), r=[pt], w=[twk])
            P.dve(lambda e, o=t8, i=lg: e.max(out=o, in_=i), r=[twk], w=[twk])
            P.dve(I_ts(msk, lg, t8[:, 3:4], ALU.is_ge), r=[twk], w=[twk])
            P.dve(I_ts(sm[:, 0:1], t8[:, 0:1], -1.0, ALU.mult), r=[twk], w=[twk])
            P.act(I_act(e_, lg, AF.Exp, bias=sm[:, 0:1]), r=[twk], w=[twk])
            P.dve(I_tt(e_, e_, msk, ALU.mult), r=[twk], w=[twk])
            P.dve(I_red(sm[:, 1:2], e_), r=[twk], w=[twk])
            P.dve(lambda e, o=sm[:, 2:3], i=sm[:, 1:2]: e.reciprocal(out=o, in_=i), r=[twk], w=[twk])
            P.dve(I_ts(e_, e_, sm[:, 2:3], ALU.mult), r=[twk], w=[twk])
            ps2, pt2 = self.PS.next()
            P.pe(I_tr(ps2[0:32, 0:128], e_, self.ident), r=[twk, self.t_c], w=[pt2])
            P.dve(I_copy(self.GT[0:32, c0:c0 + 128], ps2[0:32, 0:128]), r=[pt2], w=[self.t_GT])

    def moe_phase(self, li):
        P, A = self.P, self.A
        m = A.mark()
        b1T = A.alloc(16 * 32).rearrange("p (c e) -> p c e", c=16)
        tb1T = Tok()
        m2 = A.mark()
        self.moe_router_setup(li)
        self.norm_phase(1, li, router=self.router_cb)
        self.dbg(f"GT{li}", self.GT[0:32, :], self.t_GT, [32, T])
        tgt = Tok()
        P.dma("sp", I_dma(self.gt_d, self.GT[0:32, :]), r=[self.t_GT], w=[tgt])
        b1s = A.alloc(2048)
        tb1 = Tok()
        P.dma("act", I_dma(b1s[0:32, :], self.W["exp_b1"][li]), w=[tb1])
        for c in range(16):
            ps, pt = self.PS.next()
            P.pe(I_tr(ps[:, 0:32], b1s[0:32, c * 128:(c + 1) * 128], self.ident[0:32, 0:32]), r=[tb1, self.t_c], w=[pt])
            P.dve(I_copy(b1T[:, c, :], ps[:, 0:32]), r=[pt], w=[tb1T])
        b2s = A.alloc(1024)
        tb2 = Tok()
        P.dma("act", I_dma(b2s[0:32, :], self.W["exp_b2"][li]), w=[tb2])
        for tg, (s, n) in enumerate(self.tgs(li)):
            col = 0 if tg < 4 else 1
            for cc in range(8):
                ps, pt = self.PS.next()
                P.pe(I_mm(ps[:, 0:n], b2s[0:32, cc * 128:(cc + 1) * 128], self.GT[0:32, s:s + n]), r=[tb2, self.t_GT], w=[pt])
                P.dve(I_stt(self.xT3[:, cc, s:s + n], ps[:, 0:n], self.eff4[:, 5, cc, col:col + 1], self.xT3[:, cc, s:s + n], ALU.mult, ALU.add),
                      r=[pt, self.t_eff, self.t_x[tg]], w=[self.t_x[tg]])
        P.barrier()
        A.release(m2)
        w1_r = Ring([A.alloc(8 * 256, BF16) for _ in range(4)])
        w2_r = Ring([A.alloc(8 * 128, BF16) for _ in range(4)])
        gate_r = Ring([A.alloc(T, BF16) for _ in range(2)])
        aT3 = A.alloc(8 * T, BF16).rearrange("p (c t) -> p c t", c=8)
        taT = toks(5)
        tmp_r = Ring([A.alloc(512) for _ in range(2)])
        g_r = Ring([A.alloc(512) for _ in range(3)])
        s_r = Ring([A.alloc(512, BF16) for _ in range(2)])
        l_r = Ring([A.alloc(512, BF16) for _ in range(2)])
        a_r = Ring([A.alloc(512, BF16) for _ in range(2)])
        W1, W2 = self.W["exp_w1"][li], self.W["exp_w2"][li]
        n_exp = 32 if self.stop != f"moe1_{li}" else self.n_exp_dbg
        loads, comps = [], []
        bufmap = {}

        def mk_gate(e):
            def f():
                gate, tgate = gate_r.next()
                P.dma("pool", I_dma(gate, self.gt_d[e:e + 1, :].to_broadcast([128, T])), r=[tgt], w=[tgate])
                bufmap[("g", e)] = (gate, tgate)
            return f

        def mk_w1(e, c):
            def f():
                wp, twp = w1_r.next()
                wp3 = wp.rearrange("p (k c) -> p k c", k=8)
                P.dma("pool", I_dma(wp3[:, :, 0:128], W1[e][:, c * 128:(c + 1) * 128].rearrange("(k p) c -> p k c", p=128)), w=[twp])
                P.dma("pool", I_dma(wp3[:, :, 128:256], W1[e][:, 1024 + c * 128:1024 + (c + 1) * 128].rearrange("(k p) c -> p k c", p=128)), w=[twp])
                bufmap[("w1", e, c)] = (wp3, twp)
            return f

        def mk_w2(e, cc):
            def f():
                w2p, tw2 = w2_r.next()
                w2p3 = w2p.rearrange("p (k c) -> p k c", k=8)
                P.dma("pool", I_dma(w2p3, W2[e][:, cc * 128:(cc + 1) * 128].rearrange("(k p) c -> p k c", p=128)), w=[tw2])
                bufmap[("w2", e, cc)] = (w2p3, tw2)
            return f

        def mk_hid(e, c):
            def f():
                wp3, twp = bufmap[("w1", e, c)]
                for tg, (s, n) in enumerate(self.tgs(li)):
                    psg, ptg = self.PS.next()
                    psl, ptl = self.PS.next()
                    for kc in range(8):
                        P.pe(I_mm(psg[:, 0:n], wp3[:, kc, 0:128], self.hT3[:, kc, s:s + n], start=(kc == 0), stop=(kc == 7)),
                             r=[twp, self.t_h[tg]], w=[ptg])
                    for kc in range(8):
                        P.pe(I_mm(psl[:, 0:n], wp3[:, kc, 128:256], self.hT3[:, kc, s:s + n], start=(kc == 0), stop=(kc == 7)),
                             r=[twp, self.t_h[tg]], w=[ptl])
                    g_, tg_ = g_r.next()
                    s_, ts_ = s_r.next()
                    l_, tl_ = l_r.next()
                    a_, ta_ = a_r.next()
                    P.dve(I_ts(g_[:, 0:n], psg[:, 0:n], b1T[:, c, e:e + 1], ALU.add, 7.0, ALU.min), r=[ptg, tb1T], w=[tg_])
                    P.act(I_act(s_[:, 0:n], g_[:, 0:n], AF.Sigmoid, scale=1.702), r=[tg_], w=[ts_])
                    P.act(I_act(l_[:, 0:n], psl[:, 0:n], AF.Identity, bias=b1T[:, 8 + c, e:e + 1]), r=[ptl, tb1T], w=[tl_])
                    P.dve(I_ts(l_[:, 0:n], l_[:, 0:n], 7.0, ALU.min, -7.0, ALU.max), r=[tl_], w=[tl_])
                    P.dve(I_tt(a_[:, 0:n], g_[:, 0:n], s_[:, 0:n], ALU.mult), r=[tg_, ts_], w=[ta_])
                    P.dve(I_stt(aT3[:, c, s:s + n], l_[:, 0:n], 1.0, a_[:, 0:n], ALU.add, ALU.mult), r=[ta_, tl_], w=[taT[tg]])
            return f

        def mk_y(e, cc):
            def f():
                gate, tgate = bufmap[("g", e)]
                w2p3, tw2 = bufmap[("w2", e, cc)]
                for tg, (s, n) in enumerate(self.tgs(li)):
                    col = 0 if tg < 4 else 1
                    ps, pt = self.PS.next()
                    for c in range(8):
                        P.pe(I_mm(ps[:, 0:n], w2p3[:, c, :], aT3[:, c, s:s + n], start=(c == 0), stop=(c == 7)), r=[tw2, taT[tg]], w=[pt])
                    tmp, ttmp = tmp_r.next()
                    P.dve(I_tt(tmp[:, 0:n], ps[:, 0:n], gate[:, s:s + n], ALU.mult), r=[pt, tgate], w=[ttmp])
                    P.dve(I_stt(self.xT3[:, cc, s:s + n], tmp[:, 0:n], self.eff4[:, 5, cc, col:col + 1], self.xT3[:, cc, s:s + n], ALU.mult, ALU.add),
                          r=[ttmp, self.t_eff, self.t_x[tg]], w=[self.t_x[tg]])
            return f

        for e in range(n_exp):
            loads.append(mk_gate(e))
            for c in range(8):
                loads.append(mk_w1(e, c))
                comps.append((len(loads) - 1, mk_hid(e, c)))
            for cc in range(8):
                loads.append(mk_w2(e, cc))
                comps.append((len(loads) - 1, mk_y(e, cc)))
        LOOK = 3
        ptr = 0
        for need, f in comps:
            tgt_ptr = min(len(loads), need + 1 + LOOK)
            while ptr < tgt_ptr:
                loads[ptr]()
                ptr += 1
            f()
        P.barrier()
        A.release(m)

    def build(self):
        P = self.P
        self.setup()
        self.load_x()
        for li in range(self.n_layers):
            self.mod_phase(li)
            self.norm_phase(0, li)
            self.dbg(f"hT{li}", self.hT, self.t_h, [128, 8 * T], BF16)
            self.dbg(f"eff{li}", self.eff, self.t_eff, [128, 96])
            if self.stop == f"norm1_{li}":
                break
            self.spill_x()
            if "attn" not in self.skip:
                self.attn_phase(li)
            if self.stop in (f"attn_{li}", f"attnproj_{li}"):
                break
            if "conv" not in self.skip:
                self.conv_phase(li)
            if self.stop == f"conv_{li}":
                break
            if "rw" not in self.skip:
                self.rwkv_phase(li)
            if self.stop in (f"rw_{li}", f"rw1_{li}"):
                break
            self.restore_x()
            self.merge_phase(li)
            self.dbg(f"xmid{li}", self.xT, self.t_x, [128, 8 * T])
            if self.stop == f"merge_{li}":
                break
            self.moe_phase(li)
            self.dbg(f"xout{li}", self.xT, self.t_x, [128, 8 * T])
            if self.stop in (f"moe_{li}", f"moe1_{li}"):
                break
        if self.stop is None:
            self.final_phase()
        if self.dbg_br:
            t = Tok()
            d = self.nc.dram_tensor("dbg_brT", [3, 4, 128, T], BF16, kind="ExternalOutput").ap()
            P.dma("sp", I_dma(d, self.brT_d), r=self.t_br, w=[t])
            self.out_toks.append(t)
            self.dbg_out["brT"] = "dbg_brT"
        P.op("sp", None, r=self.out_toks)
        P.emit(self.nc, self.st)
        self.st.close()
        return self.nc


_CONSTS = None


def make_in_maps(inputs):
    global _CONSTS
    if _CONSTS is None:
        _CONSTS = make_consts()
    shared = {n: np.ascontiguousarray(np.asarray(inputs[n], dtype=np.float32)) for n, _ in WEIGHT_SPECS}
    for n in CONST_NAMES:
        shared["k_" + n] = _CONSTS[n]
    x = np.asarray(inputs["x"], dtype=np.float32)
    ctx = np.asarray(inputs["ctx"], dtype=np.float32)
    c = np.asarray(inputs["c"], dtype=np.float32)
    c_ctx = np.asarray(inputs["c_ctx"], dtype=np.float32)
    maps = []
    for b in range(8):
        m = dict(shared)
        m["x"] = np.ascontiguousarray(x[b])
        m["ctx"] = np.ascontiguousarray(ctx[b])
        m["c2"] = np.ascontiguousarray(np.stack([c[b], c_ctx], 0))
        maps.append(m)
    return maps


def kernel(**inputs):
    mk = MK()
    nc = mk.build()
    maps = make_in_maps(inputs)
    res = run_bass_kernel_spmd(nc, maps, core_ids=list(range(8)))
    return np.stack([np.asarray(res.results[b]["out"], dtype=np.float32) for b in range(8)], 0)
```
